# Optimizing a Trainium2 kernel written in Bass

```python
import math
import jax, jax.numpy as jnp
from jax import lax
import numpy as np

D_MODEL = 1024
BATCH = 4
SEQ = 8192
DEPTH = 2

GRID_W = 64
CTX_LEN = 256
EPS = 1e-6

F_GROUPS = 4
F_GDIM = 64
F_WIDTH = F_GROUPS * F_GDIM
DA_HEADS = 6
DA_DIM = 32
DA_VDIM = 2 * DA_DIM
DA_WIDTH = DA_HEADS * DA_VDIM
M_HEADS = 4
M_DIM = 96
M_WIDTH = M_HEADS * M_DIM
M_CHUNK = 64
M_CONV = 3
MIX_WIDTH = F_WIDTH + DA_WIDTH + M_WIDTH
N_GATES = 4 * M_HEADS
Q_BLOCK = 128
ROPE_THETA = 10000.0
N_EXPERTS = 16
EC_CAPACITY = 2
D_FF_EXPERT = 2 * D_MODEL
ADA_CHUNKS = 6

OFF_F = 0
OFF_DQ = OFF_F + F_WIDTH
OFF_MO = OFF_DQ + 2 * DA_HEADS * DA_DIM
OFF_MQ = OFF_MO + M_WIDTH
OFF_MK = OFF_MQ + M_WIDTH
OFF_DK = OFF_MK + M_WIDTH
OFF_DV = OFF_DK + 2 * DA_HEADS * DA_DIM
OFF_MV = OFF_DV + DA_HEADS * DA_VDIM
OFF_G = OFF_MV + M_WIDTH
PROJ_WIDTH = OFF_G + N_GATES

kernel_name = 'hybrid_fourier_diffattn_mlstm_ecmoe_dit'


def rmsnorm(x, g):
    xf = x.astype(jnp.float32)
    r = lax.rsqrt(jnp.mean(xf * xf, axis=-1, keepdims=True) + EPS)
    return (xf * r).astype(x.dtype) * g


def adaln(cvec, w, b):
    return jnp.split(jax.nn.silu(cvec) @ w + b, ADA_CHUNKS, axis=-1)


def cols(p, off, width, base=0):
    return p[..., off - base: off - base + width]


def axial_rope_tables(rows):
    t_row = jnp.repeat(jnp.arange(rows), GRID_W)
    t_col = jnp.tile(jnp.arange(GRID_W), rows)
    nf = DA_DIM // 4
    inv = ROPE_THETA ** (-jnp.arange(nf, dtype=jnp.float32) / nf)
    ar = t_row[:, None].astype(jnp.float32) * inv
    ac = t_col[:, None].astype(jnp.float32) * inv
    ang = jnp.concatenate([ar, ar, ac, ac], axis=-1)
    return jnp.cos(ang), jnp.sin(ang)


def rope_2d(x, cos, sin):
    xs = x.reshape(x.shape[:-1] + (2, 2, DA_DIM // 4))
    rot = jnp.concatenate([-xs[..., 1:, :], xs[..., :1, :]], axis=-2).reshape(x.shape)
    c = cos.astype(x.dtype)[:, None, None, :]
    s = sin.astype(x.dtype)[:, None, None, :]
    return x * c + rot * s


def fourier_mix(p, w):
    B, N, _ = p.shape
    z = jnp.fft.fft2(p.reshape(B, N, F_GROUPS, F_GDIM).astype(jnp.float32), axes=(1, 3), norm='ortho').real
    return jnp.einsum('bngc,gcd->bngd', z.astype(p.dtype), w).reshape(B, N, F_WIDTH)


def diff_attend(q, k, v, lam):
    s = jnp.einsum('bqhcd,bkhcd->bhcqk', q, k).astype(jnp.float32) * (DA_DIM ** -0.5)
    a = jax.nn.softmax(s, axis=-1)
    a = a[:, :, 0] - lam * a[:, :, 1]
    return jnp.einsum('bhqk,bkhv->bqhv', a.astype(v.dtype), v)


def centred_conv(x, w, b):
    pad = M_CONV // 2
    n = x.shape[1]
    xp = jnp.pad(x, ((0, 0), (pad, pad), (0, 0)))
    out = b
    for j in range(M_CONV):
        out = out + xp[:, j:j + n] * w[j]
    return out


def heads(a):
    B, N, _ = a.shape
    return a.reshape(B, N, M_HEADS, M_DIM).transpose(0, 2, 1, 3).astype(jnp.float32)


def gate_logs(p, gate_b):
    B, N, _ = p.shape
    g = (p.astype(jnp.float32) + gate_b.astype(jnp.float32)).reshape(B, N, 2, 2, M_HEADS)
    g = jnp.transpose(g, (2, 3, 0, 4, 1))
    return g[0, 0], jax.nn.log_sigmoid(g[0, 1]), g[1, 0], jax.nn.log_sigmoid(g[1, 1])


def mlstm_zero_state(B):
    return (jnp.zeros((B, M_HEADS, M_DIM, M_DIM), jnp.float32),
            jnp.zeros((B, M_HEADS, M_DIM), jnp.float32),
            jnp.zeros((B, M_HEADS), jnp.float32))


def mlstm_state_update(state, k, v, li, lf):
    C, n, m = state
    b = jnp.cumsum(lf, axis=-1)
    b_last = b[..., -1]
    w_log = b_last[..., None] - b + li
    m_new = jnp.maximum(b_last + m, jnp.max(w_log, axis=-1))
    decay = jnp.exp(b_last + m - m_new)
    w = jnp.exp(w_log - m_new[..., None])
    C_new = decay[..., None, None] * C + jnp.einsum('bhlv,bhlk->bhvk', v * w[..., None], k)
    n_new = decay[..., None] * n + jnp.einsum('bhl,bhlk->bhk', w, k)
    return (C_new, n_new, m_new)


def mlstm_chunk(state, chunk):
    q, k, v, li, lf = chunk
    C, n, m = state
    L = q.shape[2]
    b = jnp.cumsum(lf, axis=-1)
    seen = jnp.tril(jnp.ones((L, L), dtype=bool))
    d_log = jnp.where(seen, b[..., :, None] - b[..., None, :] + li[..., None, :], -jnp.inf)
    inter_log = b + m[..., None]
    m_t = jnp.maximum(inter_log, jnp.max(d_log, axis=-1))
    w_inter = jnp.exp(inter_log - m_t)
    s = jnp.einsum('bhld,bhsd->bhls', q, k) * jnp.exp(d_log - m_t[..., None])
    num = w_inter[..., None] * jnp.einsum('bhvk,bhlk->bhlv', C, q) + jnp.einsum('bhls,bhsv->bhlv', s, v)
    den = w_inter * jnp.einsum('bhk,bhlk->bhl', n, q) + jnp.sum(s, axis=-1)
    h = num / jnp.maximum(jnp.abs(den), jnp.exp(-m_t))[..., None]
    return mlstm_state_update(state, k, v, li, lf), h


def mlstm_scan(q, k, v, li, lf, state):
    B, H, N, d = q.shape
    nc = N // M_CHUNK

    def to_chunks(a):
        return jnp.moveaxis(a.reshape((B, H, nc, M_CHUNK) + a.shape[3:]), 2, 0)

    state, h = lax.scan(mlstm_chunk, state, tuple(to_chunks(a) for a in (q, k, v, li, lf)))
    return jnp.moveaxis(h, 0, 2).reshape(B, H, N, d), state


def flip(a):
    return jnp.flip(a, axis=2)


def mlstm_out(h, o, g):
    B, H, N, d = h.shape
    hn = rmsnorm(jnp.transpose(h, (0, 2, 1, 3)).astype(o.dtype), g.reshape(H, d))
    return hn.reshape(B, N, M_WIDTH) * jax.nn.sigmoid(o)


def token_mixers(hl, hc, w_in, four_w, conv_w, conv_b, gate_b, m_norm_g, d_lam, d_norm_g,
                 lam_init, cos, sin, ctx_out):
    B, N, _ = hl.shape
    Bc, NC, _ = hc.shape
    base = 0 if ctx_out else OFF_MK
    pl = hl @ w_in
    pc = hc @ w_in[:, base:]
    qshape = (DA_HEADS, 2, DA_DIM)

    lam = (jnp.exp(jnp.sum(d_lam[0] * d_lam[1])) - jnp.exp(jnp.sum(d_lam[2] * d_lam[3]))
           + lam_init).astype(jnp.float32)
    q_l = rope_2d(pl[..., OFF_DQ:OFF_MO].reshape((B, N) + qshape), cos, sin)
    k_l = rope_2d(pl[..., OFF_DK:OFF_DV].reshape((B, N) + qshape), cos, sin)
    v_l = pl[..., OFF_DV:OFF_MV].reshape(B, N, DA_HEADS, DA_VDIM)
    k_c = cols(pc, OFF_DK, 2 * DA_HEADS * DA_DIM, base).reshape((Bc, NC) + qshape)
    v_c = cols(pc, OFF_DV, DA_WIDTH, base).reshape(Bc, NC, DA_HEADS, DA_VDIM)
    k_all = jnp.concatenate([k_c, k_l], axis=1)
    v_all = jnp.concatenate([v_c, v_l], axis=1)
    nb = N // Q_BLOCK
    q_blocks = jnp.moveaxis(q_l.reshape((B, nb, Q_BLOCK) + qshape), 1, 0)
    o_l = lax.map(lambda qb: diff_attend(qb, k_all, v_all, lam), q_blocks)
    o_l = jnp.moveaxis(o_l, 0, 1).reshape(B, N, DA_HEADS, DA_VDIM)
    da_l = (rmsnorm(o_l, d_norm_g) * (1.0 - lam_init)).reshape(B, N, DA_WIDTH)

    q_m = heads(jax.nn.silu(centred_conv(pl[..., OFF_MQ:OFF_MK], conv_w[:, :M_WIDTH], conv_b[:M_WIDTH])))
    k_m = heads(jax.nn.silu(centred_conv(pl[..., OFF_MK:OFF_DK], conv_w[:, M_WIDTH:], conv_b[M_WIDTH:]))) * (M_DIM ** -0.5)
    v_m = heads(pl[..., OFF_MV:OFF_G])
    li_f, lf_f, li_b, lf_b = gate_logs(pl[..., OFF_G:], gate_b)
    k_mc = heads(jax.nn.silu(centred_conv(cols(pc, OFF_MK, M_WIDTH, base), conv_w[:, M_WIDTH:], conv_b[M_WIDTH:]))) * (M_DIM ** -0.5)
    v_mc = heads(cols(pc, OFF_MV, M_WIDTH, base))
    lic_f, lfc_f, lic_b, lfc_b = gate_logs(cols(pc, OFF_G, N_GATES, base), gate_b)
    zero = mlstm_zero_state(Bc)
    if ctx_out:
        q_mc = heads(jax.nn.silu(centred_conv(pc[..., OFF_MQ:OFF_MK], conv_w[:, :M_WIDTH], conv_b[:M_WIDTH])))
        h_cf, st_f = mlstm_scan(q_mc, k_mc, v_mc, lic_f, lfc_f, zero)
        h_cb, st_b = mlstm_scan(flip(q_mc), flip(k_mc), flip(v_mc), flip(lic_b), flip(lfc_b), zero)
        m_c = mlstm_out(h_cf + flip(h_cb), pc[..., OFF_MO:OFF_MQ], m_norm_g)
    else:
        st_f = mlstm_state_update(zero, k_mc, v_mc, lic_f, lfc_f)
        st_b = mlstm_state_update(zero, flip(k_mc), flip(v_mc), flip(lic_b), flip(lfc_b))
    h_lf, _ = mlstm_scan(q_m, k_m, v_m, li_f, lf_f, st_f)
    h_lb, _ = mlstm_scan(flip(q_m), flip(k_m), flip(v_m), flip(li_b), flip(lf_b), st_b)
    m_l = mlstm_out(h_lf + flip(h_lb), pl[..., OFF_MO:OFF_MQ], m_norm_g)

    f_l = fourier_mix(pl[..., OFF_F:OFF_DQ], four_w)
    mix_l = jnp.concatenate([f_l, da_l, m_l], axis=-1)

    if ctx_out:
        q_c = pc[..., OFF_DQ:OFF_MO].reshape((Bc, NC) + qshape)
        o_c = diff_attend(q_c, k_c, v_c, lam)
        da_c = (rmsnorm(o_c, d_norm_g) * (1.0 - lam_init)).reshape(Bc, NC, DA_WIDTH)
        f_c = fourier_mix(pc[..., OFF_F:OFF_DQ], four_w)
        mix_c = jnp.concatenate([f_c, da_c, m_c], axis=-1)
    else:
        mix_c = None
    return mix_l, mix_c


def ec_moe(h, w_r, w1, w3, w2):
    B, N, D = h.shape
    cap = EC_CAPACITY * N // N_EXPERTS
    probs = jax.nn.softmax((h @ w_r).astype(jnp.float32), axis=-1)
    gate, idx = lax.top_k(jnp.swapaxes(probs, 1, 2), cap)
    xs = jax.vmap(lambda hb, ib: hb[ib])(h, idx)
    hid = jax.nn.silu(jnp.einsum('becd,edf->becf', xs, w1)) * jnp.einsum('becd,edf->becf', xs, w3)
    y = jnp.einsum('becf,efd->becd', hid, w2) * gate[..., None].astype(h.dtype)
    return jax.vmap(lambda ib, yb: jnp.zeros((N, D), h.dtype).at[ib.reshape(-1)].add(yb.reshape(-1, D)))(idx, y)


def setup_inputs(seed: int = 0) -> dict:
    key = jax.random.key(seed)
    ks = jax.random.split(key, 24)
    nrm = jax.random.normal
    f32 = jnp.float32
    D = D_MODEL
    gate_noise = 0.1 * nrm(ks[12], (DEPTH, 2, 2, M_HEADS), f32)
    gate_offset = jnp.array([0.0, 3.0], f32)[None, None, :, None]
    return {
        'x': nrm(ks[0], (BATCH, SEQ, D), f32),
        'c': nrm(ks[1], (BATCH, D), f32),
        'ctx': nrm(ks[2], (BATCH, CTX_LEN, D), f32),
        'c_ctx': nrm(ks[3], (D,), f32),
        'ada_w': nrm(ks[4], (DEPTH, D, ADA_CHUNKS * D), f32) * (0.5 * D ** -0.5),
        'ada_b': 0.02 * nrm(ks[5], (DEPTH, ADA_CHUNKS * D), f32),
        'norm1_g': 1.0 + 0.02 * nrm(ks[6], (DEPTH, D), f32),
        'norm2_g': 1.0 + 0.02 * nrm(ks[7], (DEPTH, D), f32),
        'w_in': nrm(ks[8], (DEPTH, D, PROJ_WIDTH), f32) * D ** -0.5,
        'four_w': nrm(ks[9], (DEPTH, F_GROUPS, F_GDIM, F_GDIM), f32) * F_GDIM ** -0.5,
        'm_conv_w': nrm(ks[10], (DEPTH, M_CONV, 2 * M_WIDTH), f32) * M_CONV ** -0.5,
        'm_conv_b': 0.02 * nrm(ks[11], (DEPTH, 2 * M_WIDTH), f32),
        'm_gate_b': (gate_noise + gate_offset).reshape(DEPTH, N_GATES),
        'm_norm_g': 1.0 + 0.02 * nrm(ks[13], (DEPTH, M_WIDTH), f32),
        'd_lam': 0.1 * nrm(ks[14], (DEPTH, 4, DA_DIM), f32),
        'd_norm_g': 1.0 + 0.02 * nrm(ks[15], (DEPTH, DA_VDIM), f32),
        'w_out': nrm(ks[16], (DEPTH, MIX_WIDTH, D), f32) * MIX_WIDTH ** -0.5,
        'router_w': nrm(ks[17], (DEPTH, D, N_EXPERTS), f32) * D ** -0.5,
        'exp_w1': nrm(ks[18], (DEPTH, N_EXPERTS, D, D_FF_EXPERT), f32) * D ** -0.5,
        'exp_w3': nrm(ks[19], (DEPTH, N_EXPERTS, D, D_FF_EXPERT), f32) * D ** -0.5,
        'exp_w2': nrm(ks[20], (DEPTH, N_EXPERTS, D_FF_EXPERT, D), f32) * D_FF_EXPERT ** -0.5,
        'final_g': 1.0 + 0.02 * nrm(ks[21], (D,), f32),
    }


def reference(x, c, ctx, c_ctx, ada_w, ada_b, norm1_g, norm2_g, w_in, four_w, m_conv_w, m_conv_b,
              m_gate_b, m_norm_g, d_lam, d_norm_g, w_out, router_w, exp_w1, exp_w3, exp_w2, final_g):
    B, S, _ = x.shape
    ROWS = S // GRID_W
    cos, sin = axial_rope_tables(ROWS)
    xl, xc = x, ctx
    for layer in range(DEPTH):
        ctx_out = layer < DEPTH - 1
        lam_init = 0.8 - 0.6 * math.exp(-0.3 * layer)
        sh1, sc1, gt1, sh2, sc2, gt2 = [m[:, None, :] for m in adaln(c, ada_w[layer], ada_b[layer])]
        csh1, csc1, cgt1, csh2, csc2, cgt2 = adaln(c_ctx, ada_w[layer], ada_b[layer])
        hl = rmsnorm(xl, norm1_g[layer]) * (1.0 + sc1) + sh1
        hc = rmsnorm(xc, norm1_g[layer]) * (1.0 + csc1) + csh1
        mix_l, mix_c = token_mixers(hl, hc, w_in[layer], four_w[layer], m_conv_w[layer], m_conv_b[layer],
                                    m_gate_b[layer], m_norm_g[layer], d_lam[layer], d_norm_g[layer],
                                    lam_init, cos, sin, ctx_out)
        xl = xl + gt1 * (mix_l @ w_out[layer])
        hl = rmsnorm(xl, norm2_g[layer]) * (1.0 + sc2) + sh2
        xl = xl + gt2 * ec_moe(hl, router_w[layer], exp_w1[layer], exp_w3[layer], exp_w2[layer])
        if ctx_out:
            xc = xc + cgt1 * (mix_c @ w_out[layer])
            hc = rmsnorm(xc, norm2_g[layer]) * (1.0 + csc2) + csh2
            xc = xc + cgt2 * ec_moe(hc, router_w[layer], exp_w1[layer], exp_w3[layer], exp_w2[layer])
    return rmsnorm(xl, final_g)
```

```python
import math
import numpy as np
import concourse.bass as bass
import concourse.mybir as mybir
from concourse.bass_utils import run_bass_kernel_spmd
from contextlib import ExitStack

F32 = mybir.dt.float32
I32 = mybir.dt.int32
U32 = mybir.dt.uint32
AF = mybir.ActivationFunctionType
ALU = mybir.AluOpType
AX = mybir.AxisListType

D = 1024
B = 4
S = 8192
NCTX = 256
NCORE = 8
TL = S // 2
EPS = 1e-6
PW = 2960
NEXP = 16

SEM_ROT = 12000
N_DMA_SEM = 40


class T:
    __slots__ = ("a", "w", "r", "name")

    def __init__(self, a, name=""):
        self.a = a
        self.w = {}
        self.r = {}
        self.name = name

    def __getitem__(self, idx):
        return self.a[idx]

    def sub(self, idx, name=""):
        return T(self.a[idx], name)


def _merge(d, stamp):
    k = id(stamp[0])
    if k not in d or d[k][1] < stamp[1]:
        d[k] = stamp


class KB:
    def __init__(self):
        self.nc = bass.Bass("TRN2", target_bir_lowering=False)
        nc = self.nc
        self.es = ExitStack()
        self.eng = {"pe": nc.tensor, "act": nc.scalar, "dve": nc.vector,
                    "pool": nc.gpsimd, "sp": nc.sync}
        self.cur = {}
        self.owner = {}
        self.waited = {e: {} for e in self.eng}
        self.nsem = 0
        self.dma_pool = []
        self.dma_rr = 0
        self.nname = 0
        self.out_stamps = []
        self.n_wait = 0
        self.n_op = 0
        self.rr = 0

    def new_sem(self):
        self.nsem += 1
        return self.es.enter_context(self.nc.semaphore("s%d" % self.nsem))

    def _nm(self, p):
        self.nname += 1
        return "%s%d" % (p, self.nname)

    def sb(self, shape, dt=F32, name=None):
        name = name or self._nm("sb")
        return T(self.nc.alloc_sbuf_tensor(name, list(shape), dt).ap(), name)

    def ps(self, shape, dt=F32, name=None):
        name = name or self._nm("ps")
        g = self.nc.psum_tensor(name, list(shape), dt)
        return T(self.es.enter_context(g).ap(), name)

    def dram(self, name, shape, dt=F32, kind="Internal"):
        return T(self.nc.dram_tensor(name, list(shape), dt, kind=kind).ap(), name)

    def inp(self, name, shape, dt=F32):
        return self.dram(name, shape, dt, kind="ExternalInput")

    def outp(self, name, shape, dt=F32):
        return self.dram(name, shape, dt, kind="ExternalOutput")

    def _stamp_new(self, e):
        c = self.cur.get(e)
        if c is None or c[1] >= SEM_ROT:
            c = [self.new_sem(), 0]
            self.owner[id(c[0])] = e
            self.cur[e] = c
        c[1] += 1
        return (c[0], c[1])

    def _wait(self, e, stamps):
        eng = self.eng[e]
        wd = self.waited[e]
        need = {}
        for (sem, val) in stamps:
            k = id(sem)
            if e == "pe" and self.owner.get(k) == "pe":
                continue
            if wd.get(k, 0) >= val:
                continue
            if k not in need or need[k][1] < val:
                need[k] = (sem, val)
        for k, (sem, val) in need.items():
            eng.wait_ge(sem, val)
            wd[k] = val
            self.n_wait += 1

    def _deps(self, reads, writes):
        st = []
        for t in reads:
            st.extend(t.w.values())
        for t in writes:
            st.extend(t.w.values())
            st.extend(t.r.values())
        return st

    def _record(self, stamp, reads, writes):
        for t in reads:
            _merge(t.r, stamp)
        for t in writes:
            t.w = {id(stamp[0]): stamp}
            t.r = {}

    def op(self, e, ins_fn, reads=(), writes=()):
        self._wait(e, self._deps(reads, writes))
        ins = ins_fn(self.eng[e])
        stamp = self._stamp_new(e)
        ins.then_inc(stamp[0], 1)
        self._record(stamp, reads, writes)
        self.n_op += 1
        return ins

    def dma(self, e, out_ap, in_ap, reads=(), writes=(), is_output=False, fn=None, acc=(), **kw):
        if len(self.dma_pool) < N_DMA_SEM:
            ent = [self.new_sem(), 0]
            self.dma_pool.append(ent)
        else:
            ent = self.dma_pool[self.dma_rr % len(self.dma_pool)]
            self.dma_rr += 1
        st = self._deps(reads, writes)
        if ent[1] > 0:
            st.append((ent[0], ent[1]))
        self._wait(e, st)
        eng = self.eng[e]
        if fn is None:
            ins = eng.dma_start(out=out_ap, in_=in_ap, **kw)
        else:
            ins = fn(eng)
        ent[1] += 16
        stamp = (ent[0], ent[1])
        ins.then_inc(ent[0], 16)
        self._record(stamp, reads, writes)
        for t in acc:
            _merge(t.w, stamp)
        if is_output:
            self.out_stamps.append(stamp)
        self.n_op += 1
        return ins

    def finish(self):
        self._wait("sp", self.out_stamps)
        return self.nc

    def evac(self, out_t, out_ap, in_t, in_ap):
        self.rr += 1
        if self.rr % 2:
            self.op("act", lambda e: e.copy(out=out_ap, in_=in_ap), reads=[in_t], writes=[out_t])
        else:
            self.op("dve", lambda e: e.tensor_copy(out=out_ap, in_=in_ap), reads=[in_t], writes=[out_t])


def run(kb, in_maps):
    nc = kb.finish()
    res = run_bass_kernel_spmd(nc, in_maps, core_ids=list(range(NCORE)))
    return res.results


NJ1 = 30
PWX = 3728


def build_p1(TT_L, TT_C):
    k = KB()
    TT = TT_L + TT_C
    xT = k.inp("xT", [D, TT])
    cv = k.inp("cv", [128, 8, 2])
    adaw = k.inp("adaw", [12, 128, 8, 512])
    adab = k.inp("adab", [128, 48])
    g1 = k.inp("g1", [128, 8])
    w = k.inp("w", [NJ1, 128, 8, 128])
    modo = k.outp("modT", [128, 48, 2])
    plT = k.outp("plT", [NJ1 * 128, TT])

    ones = k.sb([128, 128])
    k.op("dve", lambda e: e.memset(ones[:], 1.0), writes=[ones])
    cvs = k.sb([128, 8, 2])
    k.dma("sp", cvs[:], cv[:], reads=[cv], writes=[cvs])
    scv = k.sb([128, 8, 2])
    k.op("act", lambda e: e.activation(out=scv[:], in_=cvs[:], func=AF.Silu), reads=[cvs], writes=[scv])
    adabs = k.sb([128, 48])
    k.dma("sp", adabs[:], adab[:], reads=[adab], writes=[adabs])
    g1s = k.sb([128, 8])
    k.dma("sp", g1s[:], g1[:], reads=[g1], writes=[g1s])
    modT = k.sb([128, 48, 2])
    abuf = [k.sb([128, 8, 512]) for _ in range(2)]
    mps = [k.ps([128, 2]) for _ in range(2)]
    for g in range(12):
        ab = abuf[g % 2]
        k.dma("sp", ab[:], adaw[g], reads=[adaw], writes=[ab])
        for jj in range(4):
            j = 4 * g + jj
            mp = mps[j % 2]
            for kc in range(8):
                k.op("pe", lambda e, kc=kc, jj=jj, mp=mp, ab=ab: e.matmul(
                    mp[:], ab[:, kc, jj * 128:(jj + 1) * 128], scv[:, kc, :], start=(kc == 0), stop=(kc == 7)),
                    reads=[ab, scv], writes=[mp])
            k.op("dve", lambda e, j=j, mp=mp: e.tensor_scalar(
                out=modT[:, j, :], in0=mp[:], scalar1=adabs[:, j:j + 1], scalar2=None, op0=ALU.add),
                reads=[mp, adabs], writes=[modT])
    k.dma("pool", modo[:], modT[:], reads=[modT], writes=[modo], is_output=True)
    a1 = k.sb([128, 8, 2])
    k.op("dve", lambda e: e.tensor_scalar(out=a1[:], in0=modT[:, 8:16, :], scalar1=1.0, scalar2=None, op0=ALU.add),
         reads=[modT], writes=[a1])
    k.op("dve", lambda e: e.tensor_tensor(out=a1[:], in0=a1[:], in1=g1s[:].unsqueeze(2).broadcast_to([128, 8, 2]),
                                          op=ALU.mult), reads=[a1, g1s], writes=[a1])
    epsb = k.sb([128, 1])
    k.op("dve", lambda e: e.memset(epsb[:], EPS), writes=[epsb])

    chunks = [(i * 512, 512, 0) for i in range(TT_L // 512)]
    if TT_C:
        chunks.append((TT_L, TT_C, 1))
    xbuf = [k.sb([128, 8, 512]) for _ in range(2)]
    sq = k.sb([128, 8, 512])
    hT = [k.sb([128, 8, 512]) for _ in range(2)]
    ssp = k.ps([128, 512])
    rs = k.sb([128, 512])
    wbuf = [k.sb([128, 8, 128]) for _ in range(3)]
    pps = [k.ps([128, 512]) for _ in range(3)]
    obuf = [k.sb([128, 512]) for _ in range(3)]
    xv = xT.a.rearrange("(f p) t -> p f t", p=128)
    it = 0
    for ci, (t0, n, col) in enumerate(chunks):
        xb = xbuf[ci % 2]
        h = hT[ci % 2]
        k.dma("sp", xb[:, :, :n], xv[:, :, t0:t0 + n], reads=[xT], writes=[xb])
        k.op("act", lambda e, xb=xb, n=n: e.activation(out=sq[:, :, :n], in_=xb[:, :, :n], func=AF.Square),
             reads=[xb], writes=[sq])
        for f in range(8):
            k.op("pe", lambda e, f=f, n=n: e.matmul(ssp[:, :n], ones[:], sq[:, f, :n], start=(f == 0), stop=(f == 7)),
                 reads=[ones, sq], writes=[ssp])
        k.op("act", lambda e, n=n: e.activation(out=rs[:, :n], in_=ssp[:, :n], func=AF.Sqrt, scale=1.0 / D, bias=epsb[:]),
             reads=[ssp, epsb], writes=[rs])
        k.op("dve", lambda e, n=n: e.reciprocal(out=rs[:, :n], in_=rs[:, :n]), reads=[rs], writes=[rs])
        for f in range(8):
            k.op("dve", lambda e, f=f, n=n, h=h, xb=xb, col=col: e.scalar_tensor_tensor(
                out=h[:, f, :n], in0=xb[:, f, :n], scalar=a1[:, f, col:col + 1], in1=rs[:, :n],
                op0=ALU.mult, op1=ALU.mult), reads=[xb, a1, rs], writes=[h])
            k.op("pool", lambda e, f=f, n=n, h=h, col=col: e.tensor_scalar(
                out=h[:, f, :n], in0=h[:, f, :n], scalar1=modT[:, f, col:col + 1], scalar2=None, op0=ALU.add),
                reads=[h, modT], writes=[h])
        for j in range(NJ1):
            wb = wbuf[it % 3]
            pp = pps[it % 3]
            ob = obuf[it % 3]
            it += 1
            k.dma("sp", wb[:], w[j], reads=[w], writes=[wb])
            for f in range(8):
                k.op("pe", lambda e, f=f, n=n, wb=wb, pp=pp, h=h: e.matmul(
                    pp[:, :n], wb[:, f, :], h[:, f, :n], start=(f == 0), stop=(f == 7)),
                    reads=[wb, h], writes=[pp])
            k.evac(ob, ob[:, :n], pp, pp[:, :n])
            k.dma("pool", plT[j * 128:(j + 1) * 128, t0:t0 + n], ob[:, :n], reads=[ob], writes=[plT], is_output=True)
    return k


OFF_F, OFF_DQ, OFF_MO, OFF_MQ, OFF_MK, OFF_DK, OFF_DV, OFF_MV, OFF_G = 0, 256, 640, 1024, 1408, 1792, 2176, 2560, 2944


def _swap_perm():
    p = np.zeros(32, np.int64)
    for a in range(2):
        for pp in range(2):
            for j in range(8):
                p[a * 16 + pp * 8 + j] = a * 16 + (1 - pp) * 8 + j
    return np.concatenate([g * 32 + p for g in range(12)])


R_F, R_DQ, R_DQS, R_DK, R_DKS, R_MQ, R_MK, R_DV, R_MV, R_MO, R_G = (
    0, 256, 640, 1024, 1408, 1792, 2176, 2560, 2944, 3328, 3712)


def w_in_cols():
    sw = _swap_perm()
    ar = np.arange
    return np.concatenate([
        ar(OFF_F, OFF_F + 256), ar(OFF_DQ, OFF_DQ + 384), OFF_DQ + sw, ar(OFF_DK, OFF_DK + 384), OFF_DK + sw,
        ar(OFF_MQ, OFF_MQ + 384), ar(OFF_MK, OFF_MK + 384), ar(OFF_DV, OFF_DV + 384), ar(OFF_MV, OFF_MV + 384),
        ar(OFF_MO, OFF_MO + 384), ar(OFF_G, OFF_G + 16)])


def chunk_w(wm, nj):
    K_, n = wm.shape
    out = np.zeros((K_, nj * 128), np.float32)
    out[:, :n] = wm
    return np.ascontiguousarray(out.reshape(K_ // 128, 128, nj, 128).transpose(2, 1, 0, 3))


def vecT(v, nchunk):
    return np.ascontiguousarray(v.reshape(nchunk, 128).T)


def host_p1_inputs(inp, layer, xT_cores):
    adaw = np.ascontiguousarray(inp["ada_w"][layer].reshape(8, 128, 12, 512).transpose(2, 1, 0, 3))
    adab = vecT(inp["ada_b"][layer], 48)
    g1 = vecT(inp["norm1_g"][layer], 8)
    w = chunk_w(inp["w_in"][layer][:, w_in_cols()], NJ1)
    maps = []
    for c in range(NCORE):
        b = c // 2
        cv = np.stack([inp["c"][b], inp["c_ctx"]], axis=-1)
        cv = np.ascontiguousarray(cv.reshape(8, 128, 2).transpose(1, 0, 2))
        maps.append({"xT": xT_cores[c], "cv": cv, "adaw": adaw, "adab": adab, "g1": g1, "w": w})
    return maps


def build_p2a(NQ, NK, lam_init, ctx_q):
    k = KB()
    qT = k.inp("qT", [384, NQ]); qsT = k.inp("qsT", [384, NQ])
    kT = k.inp("kT", [384, NK]); ksT = k.inp("ksT", [384, NK])
    cosk = k.inp("cosk", [128, NK]); sink = k.inp("sink", [128, NK])
    cosq = k.inp("cosq", [128, NQ]); sinq = k.inp("sinq", [128, NQ])
    v = k.inp("v", [NK, 384])
    dlam = k.inp("dlam", [128]); dng = k.inp("dng", [64])
    da = k.outp("da", [NQ, 384])
    if ctx_q:
        qcT = k.inp("qcT", [384, 128])
        dac = k.outp("dac", [128, 384])
    NKT = NK // 128
    scale = 32 ** -0.5

    dl = k.sb([128, 128])
    k.dma("sp", dl[:], dlam.a.partition_broadcast(128), reads=[dlam], writes=[dl])
    pr = k.sb([128, 2, 32]); s2 = k.sb([128, 2]); lam = k.sb([128, 1]); nlam = k.sb([128, 1])
    dlv = dl.a.rearrange("p (a b c) -> p a b c", a=2, b=2)
    k.op("dve", lambda e: e.tensor_tensor(out=pr[:], in0=dlv[:, :, 0, :], in1=dlv[:, :, 1, :], op=ALU.mult),
         reads=[dl], writes=[pr])
    k.op("dve", lambda e: e.tensor_reduce(out=s2[:], in_=pr[:], op=ALU.add, axis=AX.X), reads=[pr], writes=[s2])
    k.op("act", lambda e: e.activation(out=s2[:], in_=s2[:], func=AF.Exp), reads=[s2], writes=[s2])
    k.op("dve", lambda e: e.tensor_tensor(out=lam[:], in0=s2[:, 0:1], in1=s2[:, 1:2], op=ALU.subtract),
         reads=[s2], writes=[lam])
    k.op("dve", lambda e: e.tensor_scalar(out=nlam[:], in0=lam[:], scalar1=-1.0, scalar2=-lam_init,
                                          op0=ALU.mult, op1=ALU.add), reads=[lam], writes=[nlam])
    gbc = k.sb([128, 64])
    k.dma("sp", gbc[:], dng.a.partition_broadcast(128), reads=[dng], writes=[gbc])
    k.op("dve", lambda e: e.tensor_scalar(out=gbc[:], in0=gbc[:], scalar1=1.0 - lam_init, scalar2=None, op0=ALU.mult),
         reads=[gbc], writes=[gbc])
    epsb = k.sb([128, 1])
    k.op("dve", lambda e: e.memset(epsb[:], EPS), writes=[epsb])

    kr = k.sb([64, NK])
    vaug = k.sb([128, NKT, 65])
    ta = [k.sb([64, 512]) for _ in range(2)]
    tb = [k.sb([64, 512]) for _ in range(2)]
    tcs = [k.sb([64, 512]) for _ in range(2)]
    tsn = [k.sb([64, 512]) for _ in range(2)]
    qr = [k.sb([64, 512]) for _ in range(2)]
    Sps = [[k.ps([128, 512]) for _ in range(2)] for _ in range(2)]
    Psb = [[k.sb([128, 512]) for _ in range(2)] for _ in range(2)]
    accp = [k.ps([128, 512]) for _ in range(2)]
    ot = [k.sb([128, 4, 64]) for _ in range(2)]
    o1 = k.sb([128, 4, 64]); o2 = k.sb([128, 4, 64]); osq = k.sb([128, 4, 64])
    r0 = k.sb([128, 4]); r1 = k.sb([128, 4]); ss = k.sb([128, 4])
    vv = v.a.rearrange("(t p) c -> p t c", p=128)
    dav = da.a.rearrange("(s p) c -> p s c", p=128)
    state = {"i": 0, "q": 0, "o": 0}

    def rope(dst_t, dst_ap, src, ssrc, ct, st, r0_, c0, n):
        i = state["i"] % 2
        state["i"] += 1
        a, b_, c_, s_ = ta[i], tb[i], tcs[i], tsn[i]
        k.dma("sp", a[:, :n], src[r0_:r0_ + 64, c0:c0 + n], reads=[src], writes=[a])
        k.dma("sp", b_[:, :n], ssrc[r0_:r0_ + 64, c0:c0 + n], reads=[ssrc], writes=[b_])
        k.dma("sp", c_[:, :n], ct[0:64, c0:c0 + n], reads=[ct], writes=[c_])
        k.dma("sp", s_[:, :n], st[0:64, c0:c0 + n], reads=[st], writes=[s_])
        k.op("pool", lambda e: e.tensor_tensor(out=a[:, :n], in0=a[:, :n], in1=c_[:, :n], op=ALU.mult),
             reads=[a, c_], writes=[a])
        k.op("pool", lambda e: e.tensor_tensor(out=b_[:, :n], in0=b_[:, :n], in1=s_[:, :n], op=ALU.mult),
             reads=[b_, s_], writes=[b_])
        k.op("pool", lambda e: e.tensor_tensor(out=dst_ap, in0=a[:, :n], in1=b_[:, :n], op=ALU.add),
             reads=[a, b_], writes=[dst_t])

    def attend(qt, nq, nkt, out_dram_t, out_ap):
        nsub = nq // 128
        for kt in range(nkt):
            for m in range(2):
                buf = kt % 2
                sp_, pb = Sps[m][buf], Psb[m][buf]
                k.op("pe", lambda e: e.matmul(sp_[:, :nq], kr[32 * m:32 * m + 32, kt * 128:(kt + 1) * 128],
                                              qt[32 * m:32 * m + 32, :nq], start=True, stop=True),
                     reads=[kr, qt], writes=[sp_])
                k.op("act", lambda e: e.activation(out=pb[:, :nq], in_=sp_[:, :nq], func=AF.Exp, scale=scale),
                     reads=[sp_], writes=[pb])
                for sub in range(nsub):
                    k.op("pe", lambda e: e.matmul(accp[m][:, sub * 65:(sub + 1) * 65], pb[:, sub * 128:(sub + 1) * 128],
                                                  vaug[:, kt, :], start=(kt == 0 and sub == 0),
                                                  stop=(kt == nkt - 1), skip_group_check=True),
                         reads=[pb, vaug], writes=[accp[m]])
        a0 = accp[0].a[:, 0:nsub * 65].rearrange("p (s c) -> p s c", c=65)
        a1 = accp[1].a[:, 0:nsub * 65].rearrange("p (s c) -> p s c", c=65)
        o = ot[state["o"] % 2]
        state["o"] += 1
        k.op("dve", lambda e: e.reciprocal(out=r0[:, :nsub], in_=a0[:, :, 64]), reads=[accp[0]], writes=[r0])
        k.op("dve", lambda e: e.reciprocal(out=r1[:, :nsub], in_=a1[:, :, 64]), reads=[accp[1]], writes=[r1])
        k.op("dve", lambda e: e.tensor_scalar(out=r1[:, :nsub], in0=r1[:, :nsub], scalar1=nlam[:], scalar2=None,
                                              op0=ALU.mult), reads=[r1, nlam], writes=[r1])
        k.op("dve", lambda e: e.tensor_tensor(out=o1[:, :nsub, :], in0=a0[:, :, 0:64],
                                              in1=r0[:, :nsub].unsqueeze(2).broadcast_to([128, nsub, 64]), op=ALU.mult),
             reads=[accp[0], r0], writes=[o1])
        k.op("dve", lambda e: e.tensor_tensor(out=o2[:, :nsub, :], in0=a1[:, :, 0:64],
                                              in1=r1[:, :nsub].unsqueeze(2).broadcast_to([128, nsub, 64]), op=ALU.mult),
             reads=[accp[1], r1], writes=[o2])
        k.op("dve", lambda e: e.tensor_tensor(out=o1[:, :nsub, :], in0=o1[:, :nsub, :], in1=o2[:, :nsub, :], op=ALU.add),
             reads=[o1, o2], writes=[o1])
        k.op("pool", lambda e: e.tensor_tensor(out=osq[:, :nsub, :], in0=o1[:, :nsub, :], in1=o1[:, :nsub, :], op=ALU.mult),
             reads=[o1], writes=[osq])
        k.op("dve", lambda e: e.tensor_reduce(out=ss[:, :nsub], in_=osq[:, :nsub, :], op=ALU.add, axis=AX.X),
             reads=[osq], writes=[ss])
        k.op("act", lambda e: e.activation(out=ss[:, :nsub], in_=ss[:, :nsub], func=AF.Sqrt, scale=1.0 / 64, bias=epsb[:]),
             reads=[ss, epsb], writes=[ss])
        k.op("dve", lambda e: e.reciprocal(out=ss[:, :nsub], in_=ss[:, :nsub]), reads=[ss], writes=[ss])
        k.op("dve", lambda e: e.tensor_tensor(out=o1[:, :nsub, :], in0=o1[:, :nsub, :],
                                              in1=ss[:, :nsub].unsqueeze(2).broadcast_to([128, nsub, 64]), op=ALU.mult),
             reads=[o1, ss], writes=[o1])
        k.op("dve", lambda e: e.tensor_tensor(out=o[:, :nsub, :], in0=o1[:, :nsub, :],
                                              in1=gbc[:].unsqueeze(1).broadcast_to([128, nsub, 64]), op=ALU.mult),
             reads=[o1, gbc], writes=[o])
        k.dma("pool", out_ap, o[:, :nsub, :], reads=[o], writes=[out_dram_t], is_output=True)

    for head in range(6):
        for c0 in range(0, NK, 512):
            n = min(512, NK - c0)
            rope(kr, kr[:, c0:c0 + n], kT, ksT, cosk, sink, head * 64, c0, n)
        k.op("dve", lambda e: e.memset(vaug[:], 1.0), writes=[vaug])
        for t0 in range(0, NKT, 11):
            t1 = min(NKT, t0 + 11)
            k.dma("sp", vaug[:, t0:t1, 0:64], vv[:, t0:t1, head * 64:(head + 1) * 64], reads=[v], writes=[vaug])
        for qb in range(NQ // 512):
            qt = qr[state["q"] % 2]
            state["q"] += 1
            rope(qt, qt[:, :512], qT, qsT, cosq, sinq, head * 64, qb * 512, 512)
            attend(qt, 512, NKT, da, dav[:, qb * 4:(qb + 1) * 4, head * 64:(head + 1) * 64])
        if ctx_q:
            qt = qr[state["q"] % 2]
            state["q"] += 1
            k.dma("sp", qt[:, :128], qcT[head * 64:(head + 1) * 64, :], reads=[qcT], writes=[qt])
            attend(qt, 128, NCTX // 128, dac,
                   dac.a.rearrange("(s p) c -> p s c", p=128)[:, :, head * 64:(head + 1) * 64])
    return k


def rope_tables(nrows_lat):
    n = nrows_lat * 64
    t_row = np.repeat(np.arange(nrows_lat), 64).astype(np.float32)
    t_col = np.tile(np.arange(64), nrows_lat).astype(np.float32)
    inv = (np.float32(10000.0) ** (-np.arange(8, dtype=np.float32) / np.float32(8))).astype(np.float32)
    ar = t_row[:, None] * inv
    ac = t_col[:, None] * inv
    ang = np.concatenate([ar, ar, ac, ac], -1)
    cos = np.cos(ang).astype(np.float32)
    sin = np.sin(ang).astype(np.float32)
    sgn = np.tile(np.concatenate([-np.ones(8), np.ones(8)]), 2).astype(np.float32)
    ssin = sin * sgn
    cosk = np.concatenate([np.ones((NCTX, 32), np.float32), cos], 0).T
    sink = np.concatenate([np.zeros((NCTX, 32), np.float32), ssin], 0).T
    return (np.ascontiguousarray(np.tile(cosk, (4, 1))), np.ascontiguousarray(np.tile(sink, (4, 1))))


def host_p2a_inputs(inp, layer, plT, ctx_q):
    cosk, sink = rope_tables(S // 64)
    maps = []
    for c in range(NCORE):
        b, h = c // 2, c % 2
        pa, pb = plT[2 * b], plT[2 * b + 1]

        def allk(r0, n):
            return np.ascontiguousarray(np.concatenate([pa[r0:r0 + n, TL:], pa[r0:r0 + n, :TL], pb[r0:r0 + n, :TL]], 1))
        me = plT[c]
        q0 = NCTX + h * TL
        m = {"qT": np.ascontiguousarray(me[R_DQ:R_DQ + 384, :TL]), "qsT": np.ascontiguousarray(me[R_DQS:R_DQS + 384, :TL]),
             "kT": allk(R_DK, 384), "ksT": allk(R_DKS, 384), "cosk": cosk, "sink": sink,
             "cosq": np.ascontiguousarray(cosk[:, q0:q0 + TL]), "sinq": np.ascontiguousarray(sink[:, q0:q0 + TL]),
             "v": np.ascontiguousarray(allk(R_DV, 384).T),
             "dlam": np.ascontiguousarray(inp["d_lam"][layer].reshape(128)), "dng": inp["d_norm_g"][layer]}
        if ctx_q:
            m["qcT"] = np.ascontiguousarray(me[R_DQ:R_DQ + 384, TL + h * 128:TL + (h + 1) * 128])
        maps.append(m)
    return maps


def build_p2c(ctx_out):
    k = KB()
    xfT = k.inp("xfT", [128, S])
    fwblk = k.inp("fwblk", [128, 128])
    ccb = k.inp("ccb", [128, 256])
    cs128 = k.inp("cs128", [128, 512])
    tw = k.inp("tw", [64, 256])
    cs64 = k.inp("cs64", [64, 128])
    fo = k.outp("fo", [64, 128, 128])
    if ctx_out:
        xcT = k.inp("xcT", [128, NCTX])
        dftc = k.inp("dftc", [128, 2, 512])
        foc = k.outp("foc", [NCTX, 128])

    def load(t, shape=None):
        s = k.sb(list(t.a.shape))
        k.dma("sp", s[:], t[:], reads=[t], writes=[s])
        return s
    fws = load(fwblk); ccbs = load(ccb); cs128s = load(cs128); tws = load(tw); cs64s = load(cs64)
    AB = k.sb([128, 256])
    yps = [k.ps([128, 512]) for _ in range(2)]
    pab = yps[0]
    for i in range(2):
        k.op("pe", lambda e: e.matmul(pab[:, i * 128:(i + 1) * 128], ccbs[:, i * 128:(i + 1) * 128], fws[:],
                                      start=True, stop=True), reads=[ccbs, fws], writes=[pab])
    k.op("dve", lambda e: e.tensor_copy(out=AB[:], in_=pab[:, 0:256]), reads=[pab], writes=[AB])

    xs = k.sb([128, S])
    for i in range(4):
        k.dma("sp", xs[:, i * 2048:(i + 1) * 2048], xfT[:, i * 2048:(i + 1) * 2048], reads=[xfT], writes=[xs])
    Y = k.sb([128, 64, 256])
    xv = xs.a.rearrange("p (n2 n1) -> p n1 n2", n1=64)
    for i in range(32):
        yp = yps[i % 2]
        for j in range(2):
            n1 = 2 * i + j
            k.op("pe", lambda e: e.matmul(yp[:, j * 256:(j + 1) * 256], xv[:, n1, :], AB[:], start=True, stop=True),
                 reads=[xs, AB], writes=[yp])
        k.evac(Y, Y[:, 2 * i:2 * i + 2, :].rearrange("p a b -> p (a b)"), yp, yp[:])
    zps = [k.ps([64, 512]) for _ in range(4)]
    zr = [k.sb([64, 4, 128]) for _ in range(2)]
    zi = [k.sb([64, 4, 128]) for _ in range(2)]
    t1 = k.sb([64, 2, 128]); t2 = k.sb([64, 2, 128])
    ops_ = [k.ps([64, 512]) for _ in range(2)]
    ob = [k.sb([64, 512]) for _ in range(2)]
    tcb = tws.a[:, 0:128].unsqueeze(1).broadcast_to([64, 2, 128])
    tsb = tws.a[:, 128:256].unsqueeze(1).broadcast_to([64, 2, 128])
    for blk in range(32):
        zrb, zib = zr[blk % 2], zi[blk % 2]
        for hb in range(2):
            zp = zps[(2 * blk + hb) % 4]
            for j in range(2):
                d = blk * 4 + hb * 2 + j
                k.op("pe", lambda e: e.matmul(zp[:, j * 256:(j + 1) * 256], Y[:, :, d], cs128s[:, 0:256],
                                              start=True, stop=False), reads=[Y, cs128s], writes=[zp])
                k.op("pe", lambda e: e.matmul(zp[:, j * 256:(j + 1) * 256], Y[:, :, 128 + d], cs128s[:, 256:512],
                                              start=False, stop=True), reads=[Y, cs128s], writes=[zp])
            zv = zp.a.rearrange("p (d c m) -> p d c m", d=2, c=2)
            sl = slice(hb * 2, hb * 2 + 2)
            k.op("dve", lambda e: e.tensor_tensor(out=t1[:], in0=zv[:, :, 0, :], in1=tcb, op=ALU.mult),
                 reads=[zp, tws], writes=[t1])
            k.op("dve", lambda e: e.tensor_tensor(out=t2[:], in0=zv[:, :, 1, :], in1=tsb, op=ALU.mult),
                 reads=[zp, tws], writes=[t2])
            k.op("pool", lambda e: e.tensor_tensor(out=zrb[:, sl, :], in0=t1[:], in1=t2[:], op=ALU.subtract),
                 reads=[t1, t2], writes=[zrb])
            k.op("dve", lambda e: e.tensor_tensor(out=t1[:], in0=zv[:, :, 0, :], in1=tsb, op=ALU.mult),
                 reads=[zp, tws], writes=[t1])
            k.op("dve", lambda e: e.tensor_tensor(out=t2[:], in0=zv[:, :, 1, :], in1=tcb, op=ALU.mult),
                 reads=[zp, tws], writes=[t2])
            k.op("pool", lambda e: e.tensor_tensor(out=zib[:, sl, :], in0=t1[:], in1=t2[:], op=ALU.add),
                 reads=[t1, t2], writes=[zib])
        op_ = ops_[blk % 2]
        o = ob[blk % 2]
        k.op("pe", lambda e: e.matmul(op_[:], cs64s[:, 0:64], zrb[:].rearrange("p a b -> p (a b)"), start=True, stop=False),
             reads=[cs64s, zrb], writes=[op_])
        k.op("pe", lambda e: e.matmul(op_[:], cs64s[:, 64:128], zib[:].rearrange("p a b -> p (a b)"), start=False, stop=True),
             reads=[cs64s, zib], writes=[op_])
        k.evac(o, o[:], op_, op_[:])
        k.dma("pool", fo[:, blk * 4:(blk + 1) * 4, :], o[:].rearrange("p (a b) -> p a b", a=4), reads=[o], writes=[fo],
              is_output=True)
    if ctx_out:
        xcs = load(xcT); dfs = load(dftc)
        Yc = k.sb([128, 2, 256])
        for t in range(2):
            yp = yps[t % 2]
            k.op("pe", lambda e: e.matmul(yp[:, 0:256], xcs[:, t * 128:(t + 1) * 128], AB[:], start=True, stop=True),
                 reads=[xcs, AB], writes=[yp])
            k.evac(Yc, Yc[:, t, :], yp, yp[:, 0:256])
        for mt in range(2):
            op_ = ops_[mt % 2]
            pv = yps[mt % 2].sub((slice(None), slice(0, 128))) if False else yps[mt % 2]
            i = 0
            for t in range(2):
                for c in range(2):
                    k.op("pe", lambda e: e.matmul(pv[:, 0:128], dfs[:, t, c * 256 + mt * 128:c * 256 + (mt + 1) * 128],
                                                  Yc[:, t, c * 128:(c + 1) * 128], start=(i == 0), stop=(i == 3)),
                         reads=[dfs, Yc], writes=[pv])
                    i += 1
            oc = k.sb([128, 128])
            k.evac(oc, oc[:], pv, pv[:, 0:128])
            k.dma("pool", foc[mt * 128:(mt + 1) * 128, :], oc[:], reads=[oc], writes=[foc], is_output=True)
    return k


def fourier_tables():
    def cs(n, m, N):
        ang = 2.0 * np.pi * np.outer(np.arange(n), np.arange(m)) / N
        return np.cos(ang), np.sin(ang)
    c64, s64 = cs(64, 64, 64)
    z = np.zeros((64, 64))
    ccb = np.concatenate([np.block([[c64, z], [z, c64]]), np.block([[s64, z], [z, s64]])], 1)
    c128, s128 = cs(128, 128, 128)
    cs128 = np.concatenate([c128, s128, -s128, c128], 1)
    tc, ts = cs(64, 128, S)
    sc = 1.0 / np.sqrt(64.0 * S)
    tw = np.concatenate([tc, ts], 1) * sc
    cs64 = np.concatenate([c64, -s64], 1)
    cn, sn = cs(NCTX, NCTX, NCTX)
    scc = 1.0 / np.sqrt(64.0 * NCTX)
    dft = np.concatenate([cn, -sn], 1) * scc
    dftc = dft.reshape(2, 128, 512).transpose(1, 0, 2)
    f = lambda a: np.ascontiguousarray(a.astype(np.float32))
    return {"ccb": f(ccb), "cs128": f(cs128), "tw": f(tw), "cs64": f(cs64), "dftc": f(dftc)}


def host_p2c_inputs(inp, layer, plT, ctx_out):
    tabs = fourier_tables()
    maps = []
    for c in range(NCORE):
        b, h = c // 2, c % 2
        pa, pb = plT[2 * b], plT[2 * b + 1]
        r0 = R_F + h * 128
        fw = inp["four_w"][layer]
        fwblk = np.zeros((128, 128), np.float32)
        fwblk[0:64, 0:64] = fw[2 * h]
        fwblk[64:128, 64:128] = fw[2 * h + 1]
        m = {"xfT": np.ascontiguousarray(np.concatenate([pa[r0:r0 + 128, :TL], pb[r0:r0 + 128, :TL]], 1)),
             "fwblk": fwblk, "ccb": tabs["ccb"], "cs128": tabs["cs128"], "tw": tabs["tw"], "cs64": tabs["cs64"]}
        if ctx_out:
            m["xcT"] = np.ascontiguousarray(pa[r0:r0 + 128, TL:])
            m["dftc"] = tabs["dftc"]
        maps.append(m)
    return maps


NT = (NCTX + S) // 128
XPW = NCTX + S + 4


def mcol(t):
    return t * 128 + (2 if t >= 2 else 0)


def build_p2b():
    k = KB()
    mqp = k.inp("mqp", [2, 96, XPW]); mkp = k.inp("mkp", [2, 96, XPW])
    cw = k.inp("cw", [2, 96, 8])
    mv = k.inp("mv", [NT * 128, 2, 96]); mo = k.inp("mo", [NT * 128, 2, 96])
    gt = k.inp("gt", [NT * 128, 2, 4]); gb = k.inp("gb", [8]); mng = k.inp("mng", [192])
    msk = k.inp("msk", [128, 6, 128])
    idn = k.inp("idn", [128, 128])
    out = k.outp("mout", [NT * 128, 2, 96])

    def load(t):
        s = k.sb(list(t.a.shape))
        k.dma("sp", s[:], t[:], reads=[t], writes=[s])
        return s
    msks = load(msk); ident = load(idn)
    ones = k.sb([128, 96])
    k.op("dve", lambda e: e.memset(ones[:], 1.0), writes=[ones])
    epsb = k.sb([128, 1])
    k.op("dve", lambda e: e.memset(epsb[:], EPS), writes=[epsb])
    gbs = k.sb([128, 8])
    k.dma("sp", gbs[:], gb.a.partition_broadcast(128), reads=[gb], writes=[gbs])
    mngs = k.sb([128, 192])
    k.dma("sp", mngs[:], mng.a.partition_broadcast(128), reads=[mng], writes=[mngs])

    qc = k.sb([96, NT * 128 + 2]); kc = k.sb([96, NT * 128 + 2])
    xin = [k.sb([96, 2050]) for _ in range(2)]
    tmp = [k.sb([96, 2048]) for _ in range(2)]
    cws = k.sb([96, 8])
    vaug = k.sb([128, NT, 97])
    G = k.sb([128, NT, 4]); EB = k.sb([128, NT, 2]); DEC = k.sb([96, NT, 2])
    Hsb = k.sb([128, NT, 96])
    Cst = [k.sb([96, 97]) for _ in range(2)]
    LU = [k.sb([128, 128]) for _ in range(2)]
    DT = [k.sb([128, 128]) for _ in range(2)]
    PT = [k.sb([128, 128]) for _ in range(2)]
    isb = [k.sb([128, 97]) for _ in range(2)]
    nd = [k.sb([128, 97]) for _ in range(2)]
    kw = [k.sb([128, 96]) for _ in range(2)]
    den = [k.sb([128, 1]) for _ in range(2)]
    osb = [k.sb([128, 96]) for _ in range(2)]
    yt = [k.sb([128, 96]) for _ in range(2)]
    hsq = k.sb([128, 96])
    ssn = [k.sb([128, 1]) for _ in range(2)]
    Bps = [k.ps([128, 512]) for _ in range(2)]
    Sps = [k.ps([128, 128]) for _ in range(2)]
    bigps = Bps
    ips = k.ps([128, 97]); nps = k.ps([128, 97]); kps = k.ps([128, 96]); dps = k.ps([96, 97])
    mvv = mv.a.rearrange("(t p) h c -> p t h c", p=128)
    mov = mo.a.rearrange("(t p) h c -> p t h c", p=128)
    gtv = gt.a.rearrange("(t p) h c -> p t h c", p=128)
    outv = out.a.rearrange("(t p) h c -> p t h c", p=128)
    W = NT * 128 + 2
    cnt = {"i": 0}

    for hd in range(2):
        k.dma("sp", cws[:], cw[hd], reads=[cw], writes=[cws])
        for (src, dst, o0, scl) in ((mqp, qc, 0, None), (mkp, kc, 4, 96 ** -0.5)):
            for c0 in range(0, W, 2048):
                n = min(2048, W - c0)
                i = cnt["i"] % 2
                cnt["i"] += 1
                xi, tm = xin[i], tmp[i]
                k.dma("sp", xi[:, :n + 2], src[hd, :, c0:c0 + n + 2], reads=[src], writes=[xi])
                k.op("dve", lambda e: e.tensor_scalar(out=tm[:, :n], in0=xi[:, 0:n], scalar1=cws[:, o0:o0 + 1],
                                                      scalar2=cws[:, o0 + 3:o0 + 4], op0=ALU.mult, op1=ALU.add),
                     reads=[xi, cws], writes=[tm])
                k.op("dve", lambda e: e.scalar_tensor_tensor(out=tm[:, :n], in0=xi[:, 1:n + 1], scalar=cws[:, o0 + 1:o0 + 2],
                                                             in1=tm[:, :n], op0=ALU.mult, op1=ALU.add),
                     reads=[xi, cws, tm], writes=[tm])
                k.op("dve", lambda e: e.scalar_tensor_tensor(out=tm[:, :n], in0=xi[:, 2:n + 2], scalar=cws[:, o0 + 2:o0 + 3],
                                                             in1=tm[:, :n], op0=ALU.mult, op1=ALU.add),
                     reads=[xi, cws, tm], writes=[tm])
                k.op("act", lambda e: e.activation(out=dst[:, c0:c0 + n], in_=tm[:, :n], func=AF.Silu),
                     reads=[tm], writes=[dst])
                if scl is not None:
                    k.op("pool", lambda e: e.tensor_scalar(out=dst[:, c0:c0 + n], in0=dst[:, c0:c0 + n], scalar1=scl,
                                                           scalar2=None, op0=ALU.mult), reads=[dst], writes=[dst])
        k.op("dve", lambda e: e.memset(vaug[:], 1.0), writes=[vaug])
        for t0 in range(0, NT, 11):
            k.dma("sp", vaug[:, t0:t0 + 11, 0:96], mvv[:, t0:t0 + 11, hd, :], reads=[mv], writes=[vaug])
        k.dma("sp", G[:], gtv[:, :, hd, :], reads=[gt], writes=[G])
        k.op("dve", lambda e: e.tensor_tensor(out=G[:], in0=G[:], in1=gbs[:, hd * 4:(hd + 1) * 4].unsqueeze(1).broadcast_to([128, NT, 4]),
                                              op=ALU.add), reads=[G, gbs], writes=[G])
        Gd = G.a.rearrange("p t (d y) -> p t d y", d=2)
        k.op("act", lambda e: e.activation(out=Gd[:, :, :, 1], in_=Gd[:, :, :, 1], func=AF.Exp, scale=-1.0), reads=[G], writes=[G])
        k.op("act", lambda e: e.activation(out=Gd[:, :, :, 1], in_=Gd[:, :, :, 1], func=AF.Ln, bias=1.0), reads=[G], writes=[G])
        k.op("dve", lambda e: e.tensor_scalar(out=Gd[:, :, :, 1], in0=Gd[:, :, :, 1], scalar1=-1.0, scalar2=None, op0=ALU.mult),
             reads=[G], writes=[G])
        G2 = G.a.rearrange("p t c -> p (t c)")
        for dr in range(2):
            k.op("pe", lambda e: e.matmul(bigps[dr][:, 0:NT * 4], msks[:, 1 + 3 * dr, :], G2, start=True, stop=True),
                 reads=[msks, G], writes=[bigps[dr]])
            bv = bigps[dr].a[:, 0:NT * 4].rearrange("p (t c) -> p t c", c=4)
            k.op("act", lambda e: e.activation(out=EB[:, :, dr], in_=bv[:, :, 2 * dr + 1], func=AF.Exp),
                 reads=[bigps[dr]], writes=[EB])
        k.op("pe", lambda e: e.matmul(bigps[0][0:96, 0:NT * 4], ones[:], G2, start=True, stop=True),
             reads=[ones, G, EB], writes=[bigps[0]])
        bv = bigps[0].a[0:96, 0:NT * 4].rearrange("p (t d y) -> p t d y", d=2, y=2)
        k.op("act", lambda e: e.activation(out=DEC[:], in_=bv[:, :, :, 1], func=AF.Exp), reads=[bigps[0]], writes=[DEC])

        for dr in range(2):
            order = list(range(NT)) if dr == 0 else [1, 0] + list(range(NT - 1, 1, -1))
            U, TR, MK = msks.a[:, 3 * dr, :], msks.a[:, 3 * dr + 1, :], msks.a[:, 3 * dr + 2, :]
            last = 127 if dr == 0 else 0
            cs = Cst[0]
            k.op("dve", lambda e: e.memset(cs[:], 0.0), writes=[cs])
            for si, t in enumerate(order):
                b2 = si % 2
                c0 = mcol(t)
                lf = G.a[:, t, 2 * dr + 1:2 * dr + 2]
                li = G.a[:, t, 2 * dr:2 * dr + 1]
                lu, dt_, pt, bp, sp_ = LU[b2], DT[b2], PT[b2], Bps[b2], Sps[b2]
                k.op("pool", lambda e: e.tensor_scalar(out=lu[:], in0=U, scalar1=lf, scalar2=None, op0=ALU.mult),
                     reads=[msks, G], writes=[lu])
                k.op("pe", lambda e: e.matmul(bp[:, 0:128], lu[:], TR, start=True, stop=False), reads=[lu, msks], writes=[bp])
                k.op("pe", lambda e: e.matmul(bp[:, 0:128], ident[:], MK, start=False, stop=True), reads=[ident, msks], writes=[bp])
                k.op("act", lambda e: e.activation(out=dt_[:], in_=bp[:, 0:128], func=AF.Exp, bias=li), reads=[bp, G], writes=[dt_])
                k.op("pe", lambda e: e.matmul(sp_[:], kc[:, c0:c0 + 128], qc[:, c0:c0 + 128], start=True, stop=True),
                     reads=[kc, qc], writes=[sp_])
                k.op("dve", lambda e: e.tensor_tensor(out=pt[:], in0=sp_[:], in1=dt_[:], op=ALU.mult),
                     reads=[sp_, dt_], writes=[pt])
                k.op("pe", lambda e: e.matmul(ips[:], pt[:], vaug[:, t, :], start=True, stop=True), reads=[pt, vaug], writes=[ips])
                k.op("pe", lambda e: e.matmul(nps[:], qc[:, c0:c0 + 128], cs[:], start=True, stop=True),
                     reads=[qc, cs], writes=[nps])
                ib, ndb, dn = isb[b2], nd[b2], den[b2]
                k.op("pool" if False else "dve", lambda e: e.tensor_copy(out=ib[:], in_=ips[:]), reads=[ips], writes=[ib])
                k.op("dve", lambda e: e.scalar_tensor_tensor(out=ndb[:], in0=nps[:], scalar=EB[:, t, dr:dr + 1], in1=ib[:],
                                                             op0=ALU.mult, op1=ALU.add), reads=[nps, EB, ib], writes=[ndb])
                k.op("dve", lambda e: e.tensor_scalar(out=dn[:], in0=ndb[:, 96:97], scalar1=1.0, scalar2=None, op0=ALU.max),
                     reads=[ndb], writes=[dn])
                k.op("dve", lambda e: e.scalar_tensor_tensor(out=dn[:], in0=ndb[:, 96:97], scalar=-1.0, in1=dn[:],
                                                             op0=ALU.mult, op1=ALU.max), reads=[ndb, dn], writes=[dn])
                k.op("dve", lambda e: e.reciprocal(out=dn[:], in_=dn[:]), reads=[dn], writes=[dn])
                if dr == 0:
                    k.op("dve", lambda e: e.tensor_scalar(out=Hsb[:, t, :], in0=ndb[:, 0:96], scalar1=dn[:], scalar2=None,
                                                          op0=ALU.mult), reads=[ndb, dn], writes=[Hsb])
                else:
                    y, ob_, s1 = yt[b2], osb[b2], ssn[b2]
                    k.dma("sp", ob_[:], mov[:, t, hd, :], reads=[mo], writes=[ob_])
                    k.op("dve", lambda e: e.scalar_tensor_tensor(out=y[:], in0=ndb[:, 0:96], scalar=dn[:], in1=Hsb[:, t, :],
                                                                 op0=ALU.mult, op1=ALU.add), reads=[ndb, dn, Hsb], writes=[y])
                    k.op("dve", lambda e: e.tensor_tensor(out=hsq[:], in0=y[:], in1=y[:], op=ALU.mult), reads=[y], writes=[hsq])
                    k.op("dve", lambda e: e.tensor_reduce(out=s1[:], in_=hsq[:], op=ALU.add, axis=AX.X), reads=[hsq], writes=[s1])
                    k.op("act", lambda e: e.activation(out=s1[:], in_=s1[:], func=AF.Ln, scale=1.0 / 96, bias=epsb[:]),
                         reads=[s1, epsb], writes=[s1])
                    k.op("act", lambda e: e.activation(out=s1[:], in_=s1[:], func=AF.Exp, scale=-0.5), reads=[s1], writes=[s1])
                    k.op("dve", lambda e: e.scalar_tensor_tensor(out=y[:], in0=y[:], scalar=s1[:], in1=mngs[:, hd * 96:(hd + 1) * 96],
                                                                 op0=ALU.mult, op1=ALU.mult), reads=[y, s1, mngs], writes=[y])
                    k.op("act", lambda e: e.activation(out=ob_[:], in_=ob_[:], func=AF.Exp, scale=-1.0), reads=[ob_], writes=[ob_])
                    k.op("pool", lambda e: e.tensor_scalar(out=ob_[:], in0=ob_[:], scalar1=1.0, scalar2=None, op0=ALU.add),
                         reads=[ob_], writes=[ob_])
                    k.op("dve", lambda e: e.reciprocal(out=ob_[:], in_=ob_[:]), reads=[ob_], writes=[ob_])
                    k.op("dve", lambda e: e.tensor_tensor(out=y[:], in0=y[:], in1=ob_[:], op=ALU.mult), reads=[y, ob_], writes=[y])
                    k.dma("pool", outv[:, t, hd, :], y[:], reads=[y], writes=[out], is_output=True)
                kwb = kw[b2]
                k.op("pe", lambda e: e.transpose(kps[:], kc[:, c0:c0 + 128], ident[0:96, 0:96]), reads=[kc, ident], writes=[kps])
                k.op("dve", lambda e: e.tensor_scalar(out=kwb[:], in0=kps[:], scalar1=dt_[:, last:last + 1], scalar2=None,
                                                      op0=ALU.mult), reads=[kps, dt_], writes=[kwb])
                k.op("pe", lambda e: e.matmul(dps[:], kwb[:], vaug[:, t, :], start=True, stop=True), reads=[kwb, vaug], writes=[dps])
                cn = Cst[(si + 1) % 2]
                k.op("dve", lambda e: e.scalar_tensor_tensor(out=cn[:], in0=cs[:], scalar=DEC[:, t, dr:dr + 1], in1=dps[:],
                                                             op0=ALU.mult, op1=ALU.add), reads=[cs, DEC, dps], writes=[cn])
                cs = cn
    return k


def mlstm_masks():
    j = np.arange(128)[:, None]
    s = np.arange(128)[None, :]
    UF = (j > s); TRF = (j <= s)
    MF = np.where(s < j, -30000.0, 0.0)
    UB = (j < s); TRB = (j >= s)
    MB = np.where(j < s, -30000.0, 0.0)
    m = np.stack([UF, TRF, MF, UB, TRB, MB], 1).astype(np.float32)
    return np.ascontiguousarray(m)


def host_p2b_inputs(inp, layer, plT):
    msk = mlstm_masks()
    idn = np.eye(128, dtype=np.float32)
    cwl, cbl = inp["m_conv_w"][layer], inp["m_conv_b"][layer]
    maps = []
    for c in range(NCORE):
        b, h = c // 2, c % 2
        pa, pb = plT[2 * b], plT[2 * b + 1]

        def seq(r0, n):
            return np.concatenate([pa[r0:r0 + n, TL:], pa[r0:r0 + n, :TL], pb[r0:r0 + n, :TL]], 1)

        def padded(r0):
            o = np.zeros((96, XPW), np.float32)
            o[:, 1:1 + NCTX] = pa[r0:r0 + 96, TL:]
            o[:, 3 + NCTX:3 + NCTX + TL] = pa[r0:r0 + 96, :TL]
            o[:, 3 + NCTX + TL:3 + NCTX + 2 * TL] = pb[r0:r0 + 96, :TL]
            return o
        heads = [2 * h, 2 * h + 1]
        mqp = np.stack([padded(R_MQ + hh * 96) for hh in heads])
        mkp = np.stack([padded(R_MK + hh * 96) for hh in heads])
        mv = np.ascontiguousarray(np.stack([seq(R_MV + hh * 96, 96).T for hh in heads], 1))
        mo = np.ascontiguousarray(np.stack([seq(R_MO + hh * 96, 96).T for hh in heads], 1))
        gall = seq(R_G, 16).T
        gcols = [[hh, 4 + hh, 8 + hh, 12 + hh] for hh in heads]
        gt = np.ascontiguousarray(np.stack([gall[:, gc] for gc in gcols], 1))
        gbv = np.ascontiguousarray(np.stack([inp["m_gate_b"][layer][gc] for gc in gcols]).reshape(8))
        cw = np.zeros((2, 96, 8), np.float32)
        for i, hh in enumerate(heads):
            cw[i, :, 0:3] = cwl[:, hh * 96:(hh + 1) * 96].T
            cw[i, :, 3] = cbl[hh * 96:(hh + 1) * 96]
            cw[i, :, 4:7] = cwl[:, 384 + hh * 96:384 + (hh + 1) * 96].T
            cw[i, :, 7] = cbl[384 + hh * 96:384 + (hh + 1) * 96]
        mng = np.ascontiguousarray(inp["m_norm_g"][layer][heads[0] * 96:(heads[1] + 1) * 96])
        maps.append({"mqp": mqp, "mkp": mkp, "cw": cw, "mv": mv, "mo": mo, "gt": gt, "gb": gbv, "mng": mng,
                     "msk": msk, "idn": idn})
    return maps


def build_p3(TT_L, TT_C):
    k = KB()
    TT = TT_L + TT_C
    mixT = k.inp("mixT", [D, TT]); xT = k.inp("xT", [D, TT])
    modi = k.inp("modT", [128, 48, 2])
    wo = k.inp("wo", [8, 128, 8, 128]); g2 = k.inp("g2", [128, 8]); wr = k.inp("wr", [128, 8, 16])
    x1o = k.outp("x1T", [D, TT]); h2o = k.outp("h2T", [D, TT]); pro = k.outp("probs", [TT, NEXP])

    def load(t):
        s = k.sb(list(t.a.shape))
        k.dma("sp", s[:], t[:], reads=[t], writes=[s])
        return s
    modT = load(modi); g2s = load(g2); wrs = load(wr)
    wos = k.sb([128, 8, 8, 128])
    for j in range(8):
        k.dma("sp", wos[:, j], wo[j], reads=[wo], writes=[wos])
    ones = k.sb([128, 128])
    k.op("dve", lambda e: e.memset(ones[:], 1.0), writes=[ones])
    epsb = k.sb([128, 1])
    k.op("dve", lambda e: e.memset(epsb[:], EPS), writes=[epsb])
    a2 = k.sb([128, 8, 2])
    k.op("dve", lambda e: e.tensor_scalar(out=a2[:], in0=modT[:, 32:40, :], scalar1=1.0, scalar2=None, op0=ALU.add),
         reads=[modT], writes=[a2])
    k.op("dve", lambda e: e.tensor_tensor(out=a2[:], in0=a2[:], in1=g2s[:].unsqueeze(2).broadcast_to([128, 8, 2]),
                                          op=ALU.mult), reads=[a2, g2s], writes=[a2])
    chunks = [(i * 512, 512, 0) for i in range(TT_L // 512)]
    if TT_C:
        chunks.append((TT_L, TT_C, 1))
    mb = [k.sb([128, 8, 512]) for _ in range(2)]
    xb = [k.sb([128, 8, 512]) for _ in range(2)]
    x1 = [k.sb([128, 8, 512]) for _ in range(2)]
    h2 = [k.sb([128, 8, 512]) for _ in range(2)]
    sq = k.sb([128, 8, 512])
    pps = [k.ps([128, 512]) for _ in range(2)]
    ssp = k.ps([128, 512])
    rs = k.sb([128, 512])
    lps = [k.ps([128, 16]) for _ in range(2)]
    mx = k.sb([128, 1]); sm = k.sb([128, 1]); ex = k.sb([128, 16])
    pb = [k.sb([128, 16]) for _ in range(2)]
    mv = mixT.a.rearrange("(f p) t -> p f t", p=128)
    xv = xT.a.rearrange("(f p) t -> p f t", p=128)
    x1v = x1o.a.rearrange("(f p) t -> p f t", p=128)
    h2v = h2o.a.rearrange("(f p) t -> p f t", p=128)
    it = 0
    for ci, (t0, n, col) in enumerate(chunks):
        m_, x_, x1_, h_ = mb[ci % 2], xb[ci % 2], x1[ci % 2], h2[ci % 2]
        k.dma("sp", m_[:, :, :n], mv[:, :, t0:t0 + n], reads=[mixT], writes=[m_])
        k.dma("sp", x_[:, :, :n], xv[:, :, t0:t0 + n], reads=[xT], writes=[x_])
        for j in range(8):
            pp = pps[it % 2]
            it += 1
            for f in range(8):
                k.op("pe", lambda e: e.matmul(pp[:, :n], wos[:, j, f, :], m_[:, f, :n], start=(f == 0), stop=(f == 7)),
                     reads=[wos, m_], writes=[pp])
            k.op("dve", lambda e: e.scalar_tensor_tensor(out=x1_[:, j, :n], in0=pp[:, :n], scalar=modT[:, 16 + j, col:col + 1],
                                                         in1=x_[:, j, :n], op0=ALU.mult, op1=ALU.add),
                 reads=[pp, modT, x_], writes=[x1_])
        k.dma("pool", x1v[:, :, t0:t0 + n], x1_[:, :, :n], reads=[x1_], writes=[x1o], is_output=True)
        k.op("act", lambda e: e.activation(out=sq[:, :, :n], in_=x1_[:, :, :n], func=AF.Square), reads=[x1_], writes=[sq])
        for f in range(8):
            k.op("pe", lambda e: e.matmul(ssp[:, :n], ones[:], sq[:, f, :n], start=(f == 0), stop=(f == 7)),
                 reads=[ones, sq], writes=[ssp])
        k.op("act", lambda e: e.activation(out=rs[:, :n], in_=ssp[:, :n], func=AF.Sqrt, scale=1.0 / D, bias=epsb[:]),
             reads=[ssp, epsb], writes=[rs])
        k.op("dve", lambda e: e.reciprocal(out=rs[:, :n], in_=rs[:, :n]), reads=[rs], writes=[rs])
        for f in range(8):
            k.op("dve", lambda e: e.scalar_tensor_tensor(out=h_[:, f, :n], in0=x1_[:, f, :n], scalar=a2[:, f, col:col + 1],
                                                         in1=rs[:, :n], op0=ALU.mult, op1=ALU.mult),
                 reads=[x1_, a2, rs], writes=[h_])
            k.op("pool", lambda e: e.tensor_scalar(out=h_[:, f, :n], in0=h_[:, f, :n], scalar1=modT[:, 24 + f, col:col + 1],
                                                   scalar2=None, op0=ALU.add), reads=[h_, modT], writes=[h_])
        k.dma("pool", h2v[:, :, t0:t0 + n], h_[:, :, :n], reads=[h_], writes=[h2o], is_output=True)
        for st in range(n // 128):
            lp = lps[st % 2]
            p_ = pb[st % 2]
            for f in range(8):
                k.op("pe", lambda e: e.matmul(lp[:], h_[:, f, st * 128:(st + 1) * 128], wrs[:, f, :], start=(f == 0), stop=(f == 7)),
                     reads=[h_, wrs], writes=[lp])
            k.op("dve", lambda e: e.tensor_reduce(out=mx[:], in_=lp[:], op=ALU.max, axis=AX.X, negate=True),
                 reads=[lp], writes=[mx])
            k.op("act", lambda e: e.activation(out=ex[:], in_=lp[:], func=AF.Exp, bias=mx[:], accum_out=sm[:]),
                 reads=[lp, mx], writes=[ex, sm])
            k.op("dve", lambda e: e.reciprocal(out=sm[:], in_=sm[:]), reads=[sm], writes=[sm])
            k.op("dve", lambda e: e.tensor_scalar(out=p_[:], in0=ex[:], scalar1=sm[:], scalar2=None, op0=ALU.mult),
                 reads=[ex, sm], writes=[p_])
            k.dma("pool", pro[t0 + st * 128:t0 + (st + 1) * 128, :], p_[:], reads=[p_], writes=[pro], is_output=True)
    return k


def host_p3_inputs(inp, layer, mixT, xT, modT):
    wo = chunk_w(inp["w_out"][layer], 8)
    g2 = vecT(inp["norm2_g"][layer], 8)
    wr = np.ascontiguousarray(inp["router_w"][layer].reshape(8, 128, 16).transpose(1, 0, 2))
    return [{"mixT": mixT[c], "xT": xT[c], "modT": modT[c], "wo": wo, "g2": g2, "wr": wr} for c in range(NCORE)]


def assemble_mixT(fo, foc, da, dac, mout, ctx_out):
    out = []
    for c in range(NCORE):
        b, h = c // 2, c % 2
        f_lat = np.concatenate([fo[2 * b + g].transpose(0, 2, 1).reshape(S, 128) for g in range(2)], 1)
        m_all = np.concatenate([mout[2 * b + g].reshape(NT * 128, 192) for g in range(2)], 1)
        sl = slice(h * TL, (h + 1) * TL)
        mix = np.concatenate([f_lat[sl], da[c], m_all[NCTX:][sl]], 1)
        if ctx_out:
            f_c = np.concatenate([foc[2 * b + g] for g in range(2)], 1)
            da_c = np.concatenate([dac[2 * b], dac[2 * b + 1]], 0)
            mixc = np.concatenate([f_c, da_c, m_all[:NCTX]], 1)
            mix = np.concatenate([mix, mixc], 0)
        out.append(np.ascontiguousarray(mix.T))
    return out


NBIS = 30


def build_p4(NL, NC_):
    k = KB()
    capL, capC = 2 * NL // NEXP, 2 * NC_ // NEXP
    NSLOT = capL + (128 if NC_ else 0)
    NST = NSLOT // 128
    h2 = k.inp("h2", [NL, D]); pr = k.inp("pr", [NL, 8])
    w1c = k.inp("w1c", [8, 16, 128, 8, 128]); w3c = k.inp("w3c", [8, 16, 128, 8, 128])
    w2c = k.inp("w2c", [8, 4, 128, 16, 256])
    lst = k.inp("lst", [128, 128]); idn = k.inp("idn", [128, 128])
    part = k.outp("part", [NL, D])
    if NC_:
        h2c = k.inp("h2c", [NC_, D]); prc = k.inp("prc", [NC_, 8])
        partc = k.outp("partc", [NC_, D])
    xs = [k.dram("xs%d" % e, [NSLOT, D]) for e in range(8)]
    ys = [k.dram("ys%d" % e, [NSLOT, D]) for e in range(8)]

    def load(t):
        s = k.sb(list(t.a.shape))
        k.dma("sp", s[:], t[:], reads=[t], writes=[s])
        return s
    lsts = load(lst); ident = load(idn)
    ones = k.sb([128, 128])
    k.op("dve", lambda e: e.memset(ones[:], 1.0), writes=[ones])
    banks = [k.ps([128, 512]) for _ in range(8)]
    breg = k.nc.gpsimd.alloc_register("bchk")
    k.nc.gpsimd.reg_mov(breg, NSLOT - 1)

    def route(prt, N, cap, base):
        ntl = N // 128
        prs = k.sb([128, ntl, 8]); cmp_ = k.sb([128, ntl, 8]); mask = k.sb([128, ntl, 8]); gate = k.sb([128, ntl, 8])
        off = k.sb([128, ntl, 8]); idxf = k.sb([128, ntl, 8]); idx = k.sb([128, ntl, 8], I32)
        lo = k.sb([128, 8]); mid = k.sb([128, 8]); cpart = k.sb([128, 8]); sel = k.sb([128, 8])
        k.dma("sp", prs[:], prt.a.rearrange("(t p) e -> p t e", p=128), reads=[prt], writes=[prs])
        k.op("dve", lambda e: e.memset(lo[:], 0.0), writes=[lo])
        tot = banks[6]
        for it in range(NBIS):
            w = 2.0 ** -(it + 1)
            k.op("dve", lambda e: e.tensor_scalar(out=mid[:], in0=lo[:], scalar1=w, scalar2=None, op0=ALU.add),
                 reads=[lo], writes=[mid])
            k.op("dve", lambda e: e.tensor_tensor(out=cmp_[:], in0=prs[:], in1=mid[:].unsqueeze(1).broadcast_to([128, ntl, 8]),
                                                  op=ALU.is_gt), reads=[prs, mid], writes=[cmp_])
            k.op("dve", lambda e: e.tensor_reduce(out=cpart[:], in_=cmp_[:].rearrange("p t e -> p e t"), op=ALU.add, axis=AX.X),
                 reads=[cmp_], writes=[cpart])
            k.op("pe", lambda e: e.matmul(tot[:, 0:8], ones[:], cpart[:], start=True, stop=True), reads=[ones, cpart], writes=[tot])
            k.op("dve", lambda e: e.tensor_scalar(out=sel[:], in0=tot[:, 0:8], scalar1=cap - 0.5, scalar2=w, op0=ALU.is_ge,
                                                  op1=ALU.mult), reads=[tot], writes=[sel])
            k.op("dve", lambda e: e.tensor_tensor(out=lo[:], in0=lo[:], in1=sel[:], op=ALU.add), reads=[lo, sel], writes=[lo])
        k.op("dve", lambda e: e.tensor_tensor(out=mask[:], in0=prs[:], in1=lo[:].unsqueeze(1).broadcast_to([128, ntl, 8]),
                                              op=ALU.is_gt), reads=[prs, lo], writes=[mask])
        k.op("dve", lambda e: e.tensor_tensor(out=gate[:], in0=prs[:], in1=mask[:], op=ALU.mult), reads=[prs, mask], writes=[gate])
        m2 = mask.a.rearrange("p t e -> p (t e)")
        pre, cnt = banks[6], banks[7]
        k.op("pe", lambda e: e.matmul(pre[:, 0:ntl * 8], lsts[:], m2, start=True, stop=True), reads=[lsts, mask], writes=[pre])
        k.op("pe", lambda e: e.matmul(cnt[:, 0:ntl * 8], ones[:], m2, start=True, stop=True), reads=[ones, mask], writes=[cnt])
        cv = cnt.a[:, 0:ntl * 8].rearrange("p (t e) -> p t e", e=8)
        pv = pre.a[:, 0:ntl * 8].rearrange("p (t e) -> p t e", e=8)
        k.op("dve", lambda e: e.memset(off[:, 0, :], float(base)), writes=[off])
        for i in range(ntl - 1):
            k.op("dve", lambda e: e.tensor_tensor(out=off[:, i + 1, :], in0=cv[:, i, :], in1=off[:, i, :], op=ALU.add),
                 reads=[cnt, off], writes=[off])
        k.op("dve", lambda e: e.tensor_tensor(out=off[:], in0=pv, in1=off[:], op=ALU.add), reads=[pre, off], writes=[off])
        k.op("dve", lambda e: e.tensor_scalar(out=idxf[:], in0=mask[:], scalar1=-8192.0, scalar2=8192.0, op0=ALU.mult, op1=ALU.add),
             reads=[mask], writes=[idxf])
        k.op("dve", lambda e: e.tensor_tensor(out=idxf[:], in0=idxf[:], in1=off[:], op=ALU.add), reads=[idxf, off], writes=[idxf])
        k.op("dve", lambda e: e.tensor_copy(out=idx[:], in_=idxf[:]), reads=[idxf], writes=[idx])
        return idx, gate, ntl

    sets = [(h2, pr, NL, capL, 0, part)]
    if NC_:
        sets.append((h2c, prc, NC_, capC, capL, partc))
    stage = [k.sb([128, D]) for _ in range(2)]
    routed = []
    ns = 0
    for (ht, prt, N, cap, base, po) in sets:
        idx, gate, ntl = route(prt, N, cap, base)
        routed.append((idx, gate, ntl, po))
        for i in range(ntl):
            st_ = stage[ns % 2]
            ns += 1
            k.dma("sp", st_[:], ht[i * 128:(i + 1) * 128, :], reads=[ht], writes=[st_])
            for e_ in range(8):
                k.dma("pool", None, None, reads=[st_, idx], acc=[xs[e_]],
                      fn=lambda eng: eng.indirect_dma_start(
                          out=xs[e_][:, :], out_offset=bass.IndirectOffsetOnAxis(ap=idx[:, i, e_:e_ + 1], axis=0),
                          in_=st_[:, :], in_offset=None, bounds_check=breg, oob_is_err=False))

    xsT = k.sb([128, 8, NSLOT]); hid = k.sb([128, 16, NSLOT])
    w1b = [k.sb([128, 8, 128]) for _ in range(2)]; w3b = [k.sb([128, 8, 128]) for _ in range(2)]
    w2b = [k.sb([128, 16, 256]) for _ in range(1)]
    sil = [k.sb([128, 512]) for _ in range(2)]
    yb = [k.sb([128, 256]) for _ in range(2)]
    pieces = [(c0, min(512, NSLOT - c0)) for c0 in range(0, NSLOT, 512)]
    cnt = {"w": 0, "p": 0, "y": 0}
    for e_ in range(8):
        for st in range(NST):
            sg = stage[ns % 2]
            ns += 1
            k.dma("sp", sg[:], xs[e_][st * 128:(st + 1) * 128, :], reads=[xs[e_]], writes=[sg])
            for hf in range(2):
                tp = banks[hf]
                for j in range(4):
                    kc = hf * 4 + j
                    k.op("pe", lambda e: e.transpose(tp[:, j * 128:(j + 1) * 128], sg[:, kc * 128:(kc + 1) * 128], ident[:]),
                         reads=[sg, ident], writes=[tp])
                k.evac(xsT, xsT[:, hf * 4:hf * 4 + 4, st * 128:(st + 1) * 128], tp, tp[:].rearrange("p (a b) -> p a b", a=4))
        for fg in range(16):
            wb1, wb3 = w1b[cnt["w"] % 2], w3b[cnt["w"] % 2]
            cnt["w"] += 1
            k.dma("sp", wb1[:], w1c[e_, fg], reads=[w1c], writes=[wb1])
            k.dma("sp", wb3[:], w3c[e_, fg], reads=[w3c], writes=[wb3])
            for (c0, n) in pieces:
                p1, p3 = banks[2 + cnt["p"] % 2], banks[4 + cnt["p"] % 2]
                sl_ = sil[cnt["p"] % 2]
                cnt["p"] += 1
                for kc in range(8):
                    k.op("pe", lambda e: e.matmul(p1[:, :n], wb1[:, kc, :], xsT[:, kc, c0:c0 + n], start=(kc == 0), stop=(kc == 7)),
                         reads=[wb1, xsT], writes=[p1])
                for kc in range(8):
                    k.op("pe", lambda e: e.matmul(p3[:, :n], wb3[:, kc, :], xsT[:, kc, c0:c0 + n], start=(kc == 0), stop=(kc == 7)),
                         reads=[wb3, xsT], writes=[p3])
                k.op("act", lambda e: e.activation(out=sl_[:, :n], in_=p1[:, :n], func=AF.Silu), reads=[p1], writes=[sl_])
                k.op("dve", lambda e: e.tensor_tensor(out=hid[:, fg, c0:c0 + n], in0=p3[:, :n], in1=sl_[:, :n], op=ALU.mult),
                     reads=[p3, sl_], writes=[hid])
        for dq in range(4):
            wb2 = w2b[0]
            for q4 in range(4):
                k.dma("sp", wb2[:, q4 * 4:(q4 + 1) * 4, :], w2c[e_, dq, :, q4 * 4:(q4 + 1) * 4, :], reads=[w2c], writes=[wb2])
            for st in range(NST):
                po_ = banks[cnt["y"] % 2]
                y_ = yb[cnt["y"] % 2]
                cnt["y"] += 1
                for fg in range(16):
                    k.op("pe", lambda e: e.matmul(po_[:, 0:256], hid[:, fg, st * 128:(st + 1) * 128], wb2[:, fg, :],
                                                  start=(fg == 0), stop=(fg == 15)), reads=[hid, wb2], writes=[po_])
                k.evac(y_, y_[:], po_, po_[:, 0:256])
                k.dma("pool", ys[e_][st * 128:(st + 1) * 128, dq * 256:(dq + 1) * 256], y_[:], reads=[y_], acc=[ys[e_]])

    accb = [k.sb([128, D]) for _ in range(2)]
    for s_ in stage:
        k.op("dve", lambda e: e.memset(s_[:], 0.0), writes=[s_])
    na = 0
    for (idx, gate, ntl, po) in routed:
        for i in range(ntl):
            ac = accb[na % 2]
            na += 1
            for e_ in range(8):
                g_ = stage[ns % 2]
                ns += 1
                k.dma("pool", None, None, reads=[ys[e_], idx], writes=[g_],
                      fn=lambda eng: eng.indirect_dma_start(
                          out=g_[:, :], out_offset=None, in_=ys[e_][:, :],
                          in_offset=bass.IndirectOffsetOnAxis(ap=idx[:, i, e_:e_ + 1], axis=0),
                          bounds_check=breg, oob_is_err=False))
                if e_ == 0:
                    k.op("dve", lambda e: e.tensor_scalar(out=ac[:], in0=g_[:], scalar1=gate[:, i, 0:1], scalar2=None, op0=ALU.mult),
                         reads=[g_, gate], writes=[ac])
                else:
                    k.op("dve", lambda e: e.scalar_tensor_tensor(out=ac[:], in0=g_[:], scalar=gate[:, i, e_:e_ + 1], in1=ac[:],
                                                                 op0=ALU.mult, op1=ALU.add), reads=[g_, gate, ac], writes=[ac])
            k.dma("sp", po[i * 128:(i + 1) * 128, :], ac[:], reads=[ac], writes=[po], is_output=True)
    return k


def host_p4_inputs(inp, layer, h2T, probs, ctx_out):
    lst = np.ascontiguousarray(np.triu(np.ones((128, 128), np.float32), 1))
    idn = np.eye(128, dtype=np.float32)
    maps = []
    for c in range(NCORE):
        b, h = c // 2, c % 2
        es = slice(8 * h, 8 * h + 8)
        h2 = np.ascontiguousarray(np.concatenate([h2T[2 * b][:, :TL], h2T[2 * b + 1][:, :TL]], 1).T)
        pr = np.ascontiguousarray(np.concatenate([probs[2 * b][:TL], probs[2 * b + 1][:TL]], 0)[:, es])
        w1 = inp["exp_w1"][layer][es]; w3 = inp["exp_w3"][layer][es]; w2 = inp["exp_w2"][layer][es]
        w1c = np.ascontiguousarray(w1.reshape(8, 8, 128, 16, 128).transpose(0, 3, 2, 1, 4))
        w3c = np.ascontiguousarray(w3.reshape(8, 8, 128, 16, 128).transpose(0, 3, 2, 1, 4))
        w2c = np.ascontiguousarray(w2.reshape(8, 16, 128, 4, 256).transpose(0, 3, 2, 1, 4))
        m = {"h2": h2, "pr": pr, "w1c": w1c, "w3c": w3c, "w2c": w2c, "lst": lst, "idn": idn}
        if ctx_out:
            m["h2c"] = np.ascontiguousarray(h2T[2 * b][:, TL:].T)
            m["prc"] = np.ascontiguousarray(probs[2 * b][TL:][:, es])
        maps.append(m)
    return maps


def build_p5(TT_L, TT_C, final):
    k = KB()
    TT = TT_L + TT_C
    x1T = k.inp("x1T", [D, TT]); pa = k.inp("paT", [D, TT]); pb = k.inp("pbT", [D, TT])
    modi = k.inp("modT", [128, 48, 2])
    out = k.outp("x2T", [D, TT])
    modT = k.sb([128, 48, 2])
    k.dma("sp", modT[:], modi[:], reads=[modi], writes=[modT])
    if final:
        fg = k.inp("fg", [128, 8])
        fgs = k.sb([128, 8])
        k.dma("sp", fgs[:], fg[:], reads=[fg], writes=[fgs])
        ones = k.sb([128, 128])
        k.op("dve", lambda e: e.memset(ones[:], 1.0), writes=[ones])
        epsb = k.sb([128, 1])
        k.op("dve", lambda e: e.memset(epsb[:], EPS), writes=[epsb])
        sq = k.sb([128, 8, 512]); ssp = k.ps([128, 512]); rs = k.sb([128, 512])
    chunks = [(i * 512, 512, 0) for i in range(TT_L // 512)]
    if TT_C:
        chunks.append((TT_L, TT_C, 1))
    xb = [k.sb([128, 8, 512]) for _ in range(2)]
    ab = [k.sb([128, 8, 512]) for _ in range(2)]
    bb = [k.sb([128, 8, 512]) for _ in range(2)]
    v = lambda t: t.a.rearrange("(f p) t -> p f t", p=128)
    for ci, (t0, n, col) in enumerate(chunks):
        x_, a_, b_ = xb[ci % 2], ab[ci % 2], bb[ci % 2]
        k.dma("sp", x_[:, :, :n], v(x1T)[:, :, t0:t0 + n], reads=[x1T], writes=[x_])
        k.dma("sp", a_[:, :, :n], v(pa)[:, :, t0:t0 + n], reads=[pa], writes=[a_])
        k.dma("sp", b_[:, :, :n], v(pb)[:, :, t0:t0 + n], reads=[pb], writes=[b_])
        k.op("pool", lambda e: e.tensor_tensor(out=a_[:, :, :n], in0=a_[:, :, :n], in1=b_[:, :, :n], op=ALU.add),
             reads=[a_, b_], writes=[a_])
        for f in range(8):
            k.op("dve", lambda e: e.scalar_tensor_tensor(out=x_[:, f, :n], in0=a_[:, f, :n], scalar=modT[:, 40 + f, col:col + 1],
                                                         in1=x_[:, f, :n], op0=ALU.mult, op1=ALU.add),
                 reads=[a_, modT, x_], writes=[x_])
        if final:
            k.op("act", lambda e: e.activation(out=sq[:, :, :n], in_=x_[:, :, :n], func=AF.Square), reads=[x_], writes=[sq])
            for f in range(8):
                k.op("pe", lambda e: e.matmul(ssp[:, :n], ones[:], sq[:, f, :n], start=(f == 0), stop=(f == 7)),
                     reads=[ones, sq], writes=[ssp])
            k.op("act", lambda e: e.activation(out=rs[:, :n], in_=ssp[:, :n], func=AF.Sqrt, scale=1.0 / D, bias=epsb[:]),
                 reads=[ssp, epsb], writes=[rs])
            k.op("dve", lambda e: e.reciprocal(out=rs[:, :n], in_=rs[:, :n]), reads=[rs], writes=[rs])
            for f in range(8):
                k.op("dve", lambda e: e.scalar_tensor_tensor(out=x_[:, f, :n], in0=x_[:, f, :n], scalar=fgs[:, f:f + 1],
                                                             in1=rs[:, :n], op0=ALU.mult, op1=ALU.mult),
                     reads=[x_, fgs, rs], writes=[x_])
        k.dma("pool", v(out)[:, :, t0:t0 + n], x_[:, :, :n], reads=[x_], writes=[out], is_output=True)
    return k


def kernel(**inp):
    inp = {k_: np.asarray(v_) for k_, v_ in inp.items()}
    xT = []
    for c in range(NCORE):
        b, h = c // 2, c % 2
        xT.append(np.ascontiguousarray(np.concatenate([inp["x"][b, h * TL:(h + 1) * TL], inp["ctx"][b]], 0).T))
    nlayer = inp["w_in"].shape[0]
    outT = None
    for layer in range(nlayer):
        ctx_out = layer < nlayer - 1
        lam_init = 0.8 - 0.6 * math.exp(-0.3 * layer)
        r1 = run(build_p1(TL, NCTX), host_p1_inputs(inp, layer, xT))
        plT = [r["plT"] for r in r1]
        modT = [r["modT"] for r in r1]
        del r1
        r2a = run(build_p2a(TL, NCTX + S, lam_init, ctx_out), host_p2a_inputs(inp, layer, plT, ctx_out))
        r2b = run(build_p2b(), host_p2b_inputs(inp, layer, plT))
        r2c = run(build_p2c(ctx_out), host_p2c_inputs(inp, layer, plT, ctx_out))
        del plT
        mixT = assemble_mixT([r["fo"] for r in r2c], [r.get("foc") for r in r2c], [r["da"] for r in r2a],
                             [r.get("dac") for r in r2a], [r["mout"] for r in r2b], ctx_out)
        del r2a, r2b, r2c
        if ctx_out:
            xin, tc = xT, NCTX
        else:
            xin, tc = [np.ascontiguousarray(a[:, :TL]) for a in xT], 0
        r3 = run(build_p3(TL, tc), host_p3_inputs(inp, layer, mixT, xin, modT))
        del mixT
        x1T = [r["x1T"] for r in r3]
        r4 = run(build_p4(S, NCTX if ctx_out else 0),
                 host_p4_inputs(inp, layer, [r["h2T"] for r in r3], [r["probs"] for r in r3], ctx_out))
        del r3
        m5 = []
        for c in range(NCORE):
            b, h = c // 2, c % 2
            sl = slice(h * TL, (h + 1) * TL)
            pa, pb = r4[2 * b]["part"][sl], r4[2 * b + 1]["part"][sl]
            if ctx_out:
                pa = np.concatenate([pa, r4[2 * b]["partc"]], 0)
                pb = np.concatenate([pb, r4[2 * b + 1]["partc"]], 0)
            m = {"x1T": x1T[c], "paT": np.ascontiguousarray(pa.T), "pbT": np.ascontiguousarray(pb.T), "modT": modT[c]}
            if not ctx_out:
                m["fg"] = vecT(inp["final_g"], 8)
            m5.append(m)
        del r4
        r5 = run(build_p5(TL, tc, not ctx_out), m5)
        xT = [r["x2T"] for r in r5]
    out = np.zeros((B, S, D), np.float32)
    for c in range(NCORE):
        b, h = c // 2, c % 2
        out[b, h * TL:(h + 1) * TL] = xT[c].T
    return out
```

```python
import math
import numpy as np
import concourse.bass as bass
import concourse.mybir as mybir
from concourse.bass_utils import run_bass_kernel_spmd
from contextlib import ExitStack

F32 = mybir.dt.float32
I32 = mybir.dt.int32
U32 = mybir.dt.uint32
AF = mybir.ActivationFunctionType
ALU = mybir.AluOpType
AX = mybir.AxisListType

D = 1024
B = 4
S = 8192
NCTX = 256
NCORE = 8
TL = S // 2
EPS = 1e-6
PW = 2960
NEXP = 16

SEM_ROT = 12000
N_DMA_SEM = 32


class T:
    __slots__ = ("a", "w", "r", "name")

    def __init__(self, a, name=""):
        self.a = a
        self.w = {}
        self.r = {}
        self.name = name

    def __getitem__(self, idx):
        return self.a[idx]

    def sub(self, idx, name=""):
        return T(self.a[idx], name)


def _merge(d, stamp):
    k = id(stamp[0])
    if k not in d or d[k][1] < stamp[1]:
        d[k] = stamp


class KB:
    def __init__(self):
        self.nc = bass.Bass("TRN2", target_bir_lowering=False)
        nc = self.nc
        self.es = ExitStack()
        self.eng = {"pe": nc.tensor, "act": nc.scalar, "dve": nc.vector,
                    "pool": nc.gpsimd, "sp": nc.sync}
        self.cur = {}
        self.owner = {}
        self.waited = {e: {} for e in self.eng}
        self.nsem = 0
        self.dma_pool = []
        self.dma_rr = 0
        self.nname = 0
        self.out_stamps = []
        self.n_wait = 0
        self.n_op = 0
        self.rr = 0
        self.coll_stamps = []
        self.stk = self.es

    def new_sem(self):
        self.nsem += 1
        return self.es.enter_context(self.nc.semaphore("s%d" % self.nsem))

    def _nm(self, p):
        self.nname += 1
        return "%s%d" % (p, self.nname)

    def sb(self, shape, dt=F32, name=None):
        name = name or self._nm("sb")
        g = self.nc.sbuf_tensor(name, list(shape), dt)
        return T(self.stk.enter_context(g).ap(), name)

    def sbp(self, shape, dt=F32, name=None):
        name = name or self._nm("sbp")
        return T(self.nc.alloc_sbuf_tensor(name, list(shape), dt).ap(), name)

    def ps(self, shape, dt=F32, name=None):
        name = name or self._nm("ps")
        g = self.nc.psum_tensor(name, list(shape), dt)
        return T(self.stk.enter_context(g).ap(), name)

    def barrier(self):
        st = []
        for e, c in self.cur.items():
            st.append((c[0], c[1]))
        for ent in self.dma_pool:
            if ent[1] > 0:
                st.append((ent[0], ent[1]))
        st.extend(self.coll_stamps)
        for e in ("pe", "act", "dve", "pool", "sp"):
            self._wait(e, st)

    def stage_begin(self):
        self.stk = ExitStack()

    def stage_end(self):
        self.barrier()
        self.stk.close()

    def coll(self, kind, op, in_t, out_t, groups, in_ap=None, out_ap=None):
        self._wait("pool", self._deps([in_t], [out_t]))
        if not hasattr(self, "coll_pool"):
            self.coll_pool = [[self.new_sem(), 0] for _ in range(12)]
            self.coll_n = 0
        ent = self.coll_pool[self.coll_n % len(self.coll_pool)]
        self.coll_n += 1
        ia = in_t.a if in_ap is None else in_ap
        oa = out_t.a if out_ap is None else out_ap
        ins = self.nc.gpsimd.collective_compute(kind, op, replica_groups=groups, ins=[ia.opt()], outs=[oa.opt()])
        ent[1] += 1
        ins.then_inc(ent[0], 1)
        stamp = (ent[0], ent[1])
        self.coll_stamps = [st_ for st_ in self.coll_stamps if st_[0] is not ent[0]] + [stamp]
        _merge(in_t.r, stamp)
        _merge(out_t.w, stamp)

    def dram(self, name, shape, dt=F32, kind="Internal"):
        return T(self.nc.dram_tensor(name, list(shape), dt, kind=kind).ap(), name)

    def inp(self, name, shape, dt=F32):
        if not hasattr(self, "in_names"):
            self.in_names = set()
        self.in_names.add(name)
        return self.dram(name, shape, dt, kind="ExternalInput")

    def outp(self, name, shape, dt=F32):
        return self.dram(name, shape, dt, kind="ExternalOutput")

    def _stamp_new(self, e):
        c = self.cur.get(e)
        if c is None or c[1] >= SEM_ROT:
            c = [self.new_sem(), 0]
            self.owner[id(c[0])] = e
            self.cur[e] = c
        c[1] += 1
        return (c[0], c[1])

    def _wait(self, e, stamps):
        eng = self.eng[e]
        wd = self.waited[e]
        need = {}
        for (sem, val) in stamps:
            k = id(sem)
            if e == "pe" and self.owner.get(k) == "pe":
                continue
            if wd.get(k, 0) >= val:
                continue
            if k not in need or need[k][1] < val:
                need[k] = (sem, val)
        for k, (sem, val) in need.items():
            eng.wait_ge(sem, val)
            wd[k] = val
            self.n_wait += 1

    def _deps(self, reads, writes):
        st = []
        for t in reads:
            st.extend(t.w.values())
        for t in writes:
            st.extend(t.w.values())
            st.extend(t.r.values())
        return st

    def _record(self, stamp, reads, writes):
        for t in reads:
            _merge(t.r, stamp)
        for t in writes:
            t.w = {id(stamp[0]): stamp}
            t.r = {}

    def op(self, e, ins_fn, reads=(), writes=()):
        self._wait(e, self._deps(reads, writes))
        ins = ins_fn(self.eng[e])
        stamp = self._stamp_new(e)
        ins.then_inc(stamp[0], 1)
        self._record(stamp, reads, writes)
        self.n_op += 1
        return ins

    def dma(self, e, out_ap, in_ap, reads=(), writes=(), is_output=False, fn=None, acc=(), **kw):
        if len(self.dma_pool) < N_DMA_SEM:
            ent = [self.new_sem(), 0]
            self.dma_pool.append(ent)
        else:
            ent = self.dma_pool[self.dma_rr % len(self.dma_pool)]
            self.dma_rr += 1
        st = self._deps(reads, writes)
        if ent[1] > 0:
            st.append((ent[0], ent[1]))
        self._wait(e, st)
        eng = self.eng[e]
        if fn is None:
            ins = eng.dma_start(out=out_ap, in_=in_ap, **kw)
        else:
            ins = fn(eng)
        ent[1] += 16
        stamp = (ent[0], ent[1])
        ins.then_inc(ent[0], 16)
        self._record(stamp, reads, writes)
        for t in acc:
            _merge(t.w, stamp)
        if is_output:
            self.out_stamps.append(stamp)
        self.n_op += 1
        return ins

    def finish(self):
        self._wait("sp", self.out_stamps)
        return self.nc

    def evac(self, out_t, out_ap, in_t, in_ap):
        self.rr += 1
        if self.rr % 2:
            self.op("act", lambda e: e.copy(out=out_ap, in_=in_ap), reads=[in_t], writes=[out_t])
        else:
            self.op("dve", lambda e: e.tensor_copy(out=out_ap, in_=in_ap), reads=[in_t], writes=[out_t])


def run(kb, in_maps):
    nc = kb.finish()
    names = getattr(kb, "in_names", None)
    if names is not None:
        in_maps = [{n: m[n] for n in names} for m in in_maps]
    res = run_bass_kernel_spmd(nc, in_maps, core_ids=list(range(NCORE)))
    return res.results


RG = [[0, 1], [2, 3], [4, 5], [6, 7]]
NKALL = NCTX + S
NT = NKALL // 128
XPW = NKALL + 4
NFM = 11
TMW = 776
NBIS = 30


def mcol(t):
    return t * 128 + (2 if t >= 2 else 0)


def pcol(c):
    return c + 1 if c < NCTX else c + 3


class Ctx:
    pass


def st_proj(k, g, L, ctx_out):
    k.stage_begin()
    ones = k.sb([128, 128])
    k.op("dve", lambda e: e.memset(ones[:], 1.0), writes=[ones])
    epsb = k.sb([128, 1])
    k.op("dve", lambda e: e.memset(epsb[:], EPS), writes=[epsb])
    zer = k.sb([96, 2])
    k.op("dve", lambda e: e.memset(zer[:], 0.0), writes=[zer])
    for tp in (g.MQP, g.MKP):
        for hd in range(2):
            for c0 in (0, NCTX + 1, XPW - 1):
                n = 2 if c0 == NCTX + 1 else 1
                k.dma("sp", tp[hd, :, c0:c0 + n], zer[:, 0:n], reads=[zer], acc=[tp], allow_slow_non_contiguous=True)
    cvs = k.sb([128, 8, 2])
    k.dma("sp", cvs[:], g.cv[:], reads=[g.cv], writes=[cvs])
    scv = k.sb([128, 8, 2])
    k.op("act", lambda e: e.activation(out=scv[:], in_=cvs[:], func=AF.Silu), reads=[cvs], writes=[scv])
    adabs = k.sb([128, 48])
    k.dma("sp", adabs[:], L.adab[:], reads=[L.adab], writes=[adabs])
    g1s = k.sb([128, 8])
    k.dma("sp", g1s[:], L.g1[:], reads=[L.g1], writes=[g1s])
    modT = g.modT
    abuf = [k.sb([128, 8, 512]) for _ in range(2)]
    mps = [k.ps([128, 2]) for _ in range(2)]
    for gi in range(12):
        ab = abuf[gi % 2]
        k.dma("sp", ab[:], L.adaw[gi], reads=[L.adaw], writes=[ab])
        for jj in range(4):
            j = 4 * gi + jj
            mp = mps[j % 2]
            for kc in range(8):
                k.op("pe", lambda e: e.matmul(mp[:], ab[:, kc, jj * 128:(jj + 1) * 128], scv[:, kc, :],
                                              start=(kc == 0), stop=(kc == 7)), reads=[ab, scv], writes=[mp])
            k.op("dve", lambda e: e.tensor_scalar(out=modT[:, j, :], in0=mp[:], scalar1=adabs[:, j:j + 1], scalar2=None,
                                                  op0=ALU.add), reads=[mp, adabs], writes=[modT])
    a1 = k.sb([128, 8, 2])
    k.op("dve", lambda e: e.tensor_scalar(out=a1[:], in0=modT[:, 8:16, :], scalar1=1.0, scalar2=None, op0=ALU.add),
         reads=[modT], writes=[a1])
    k.op("dve", lambda e: e.tensor_tensor(out=a1[:], in0=a1[:], in1=g1s[:].unsqueeze(2).broadcast_to([128, 8, 2]),
                                          op=ALU.mult), reads=[a1, g1s], writes=[a1])
    wtm = k.sb([128, 8, TMW])
    k.dma("sp", wtm[:], L.wAtm[:], reads=[L.wAtm], writes=[wtm])
    xbuf = [k.sb([128, 8, 512]) for _ in range(2)]
    sq = k.sb([128, 8, 512])
    hT = [k.sb([128, 8, 512]) for _ in range(2)]
    ssp = k.ps([128, 512])
    rs = k.sb([128, 512])
    wbuf = [k.sb([128, 8, 128]) for _ in range(2)]
    pps = [k.ps([128, 512]) for _ in range(2)]
    obuf = [k.sb([128, 512]) for _ in range(2)]
    tps = [k.ps([128, 512]) for _ in range(2)]
    tob = [k.sb([128, TMW]) for _ in range(2)]
    cnt = {"c": 0, "w": 0, "t": 0}

    def norm_chunk(src_t, src_ap, n, col):
        ci = cnt["c"]
        cnt["c"] += 1
        xb, h = xbuf[ci % 2], hT[ci % 2]
        if isinstance(src_ap, tuple):
            xv_, t0_ = src_ap
            k.dma("sp", xb[0:64, :, :n], xv_[0, :, :, t0_:t0_ + n], reads=[src_t], writes=[xb])
            k.dma("sp", xb[64:128, :, :n], xv_[1, :, :, t0_:t0_ + n], reads=[src_t], acc=[xb])
        else:
            k.dma("sp", xb[:, :, :n], src_ap, reads=[src_t], writes=[xb])
        k.op("act", lambda e: e.activation(out=sq[:, :, :n], in_=xb[:, :, :n], func=AF.Square), reads=[xb], writes=[sq])
        for f in range(8):
            k.op("pe", lambda e: e.matmul(ssp[:, :n], ones[:], sq[:, f, :n], start=(f == 0), stop=(f == 7)),
                 reads=[ones, sq], writes=[ssp])
        k.op("act", lambda e: e.activation(out=rs[:, :n], in_=ssp[:, :n], func=AF.Sqrt, scale=1.0 / D, bias=epsb[:]),
             reads=[ssp, epsb], writes=[rs])
        k.op("dve", lambda e: e.reciprocal(out=rs[:, :n], in_=rs[:, :n]), reads=[rs], writes=[rs])
        for f in range(8):
            k.op("dve", lambda e: e.scalar_tensor_tensor(out=h[:, f, :n], in0=xb[:, f, :n], scalar=a1[:, f, col:col + 1],
                                                         in1=rs[:, :n], op0=ALU.mult, op1=ALU.mult),
                 reads=[xb, a1, rs], writes=[h])
            k.op("pool", lambda e: e.tensor_scalar(out=h[:, f, :n], in0=h[:, f, :n], scalar1=modT[:, f, col:col + 1],
                                                   scalar2=None, op0=ALU.add), reads=[h, modT], writes=[h])
        return h

    def fm_group(h, n, wsrc, j, M, dst_t, dst_ap):
        i = cnt["w"] % 2
        cnt["w"] += 1
        wb, pp, ob = wbuf[i], pps[i], obuf[i]
        k.dma("sp", wb[:], wsrc[j], reads=[wsrc], writes=[wb])
        for f in range(8):
            k.op("pe", lambda e: e.matmul(pp[:M, :n], wb[:, f, :M], h[:, f, :n], start=(f == 0), stop=(f == 7)),
                 reads=[wb, h], writes=[pp])
        k.evac(ob, ob[:M, :n], pp, pp[:M, :n])
        k.dma("pool", dst_ap, ob[:M, :n], reads=[ob], acc=[dst_t])

    xav = [g.xall.a[:, r, :, :].rearrange("(f ph) w t -> ph w f t", ph=2) for r in range(2)]
    xcv = g.xc.a.rearrange("(f p) t -> p f t", p=128)
    xov = g.xown.a.rearrange("(f p) t -> p f t", p=128)
    chunks = [(g.xc, xcv[:, :, :], NCTX, 1, 0)]
    for r in range(2):
        for i in range(TL // 512):
            chunks.append((g.xall, (xav[r], i * 512), 512, 0, NCTX + r * TL + i * 512))
    for (src_t, src_ap, n, col, c0) in chunks:
        h = norm_chunk(src_t, src_ap, n, col)
        fm_group(h, n, L.wAfm, 0, 128, g.FT_, g.FT_[:, c0:c0 + n])
        for j in range(3):
            fm_group(h, n, L.wAfm, 1 + j, 128, g.KT_, g.KT_[j * 128:(j + 1) * 128, c0:c0 + n])
            fm_group(h, n, L.wAfm, 4 + j, 128, g.KST_, g.KST_[j * 128:(j + 1) * 128, c0:c0 + n])
        p0 = pcol(c0)
        for hd in range(2):
            fm_group(h, n, L.wAfm, 7 + hd, 96, g.MQP, g.MQP[hd, :, p0:p0 + n])
            fm_group(h, n, L.wAfm, 9 + hd, 96, g.MKP, g.MKP[hd, :, p0:p0 + n])
        for sub in range(n // 128):
            i = cnt["t"] % 2
            cnt["t"] += 1
            tp, to = tps[i], tob[i]
            for f in range(8):
                k.op("pe", lambda e: e.matmul(tp[:, 0:512], h[:, f, sub * 128:(sub + 1) * 128], wtm[:, f, 0:512],
                                              start=(f == 0), stop=(f == 7)), reads=[h, wtm], writes=[tp])
            k.evac(to, to[:, 0:512], tp, tp[:, 0:512])
            for f in range(8):
                k.op("pe", lambda e: e.matmul(tp[:, 0:TMW - 512], h[:, f, sub * 128:(sub + 1) * 128], wtm[:, f, 512:TMW],
                                              start=(f == 0), stop=(f == 7)), reads=[h, wtm], writes=[tp])
            k.evac(to, to[:, 512:TMW], tp, tp[:, 0:TMW - 512])
            k.dma("pool", g.TM[c0 + sub * 128:c0 + (sub + 1) * 128, :], to[:], reads=[to], acc=[g.TM])
    qchunks = [(g.xown, xov[:, :, i * 512:(i + 1) * 512], 512, 0, i * 512, False) for i in range(TL // 512)]
    if ctx_out:
        qchunks.append((g.xc, xcv[:, :, :], NCTX, 1, 0, True))
    for (src_t, src_ap, n, col, c0, isc) in qchunks:
        h = norm_chunk(src_t, src_ap, n, col)
        for j in range(3):
            if isc:
                fm_group(h, n, L.wQ, j, 128, g.QCT, g.QCT[j * 128:(j + 1) * 128, 0:n])
            else:
                fm_group(h, n, L.wQ, j, 128, g.QT_, g.QT_[j * 128:(j + 1) * 128, c0:c0 + n])
                fm_group(h, n, L.wQ, 3 + j, 128, g.QST_, g.QST_[j * 128:(j + 1) * 128, c0:c0 + n])
    k.stage_end()


def st_attn(k, g, L, lam_init, ctx_out):
    k.stage_begin()
    NQ, NK = TL, NKALL
    NKT = NK // 128
    scale = 32 ** -0.5
    dl = k.sb([128, 128])
    k.dma("sp", dl[:], L.dlam.a.partition_broadcast(128), reads=[L.dlam], writes=[dl])
    pr = k.sb([128, 2, 32]); s2 = k.sb([128, 2]); lam = k.sb([128, 1]); nlam = k.sb([128, 1])
    dlv = dl.a.rearrange("p (a b c) -> p a b c", a=2, b=2)
    k.op("dve", lambda e: e.tensor_tensor(out=pr[:], in0=dlv[:, :, 0, :], in1=dlv[:, :, 1, :], op=ALU.mult),
         reads=[dl], writes=[pr])
    k.op("dve", lambda e: e.tensor_reduce(out=s2[:], in_=pr[:], op=ALU.add, axis=AX.X), reads=[pr], writes=[s2])
    k.op("act", lambda e: e.activation(out=s2[:], in_=s2[:], func=AF.Exp), reads=[s2], writes=[s2])
    k.op("dve", lambda e: e.tensor_tensor(out=lam[:], in0=s2[:, 0:1], in1=s2[:, 1:2], op=ALU.subtract),
         reads=[s2], writes=[lam])
    k.op("dve", lambda e: e.tensor_scalar(out=nlam[:], in0=lam[:], scalar1=-1.0, scalar2=-lam_init,
                                          op0=ALU.mult, op1=ALU.add), reads=[lam], writes=[nlam])
    gbc = k.sb([128, 64])
    k.dma("sp", gbc[:], L.dng.a.partition_broadcast(128), reads=[L.dng], writes=[gbc])
    k.op("dve", lambda e: e.tensor_scalar(out=gbc[:], in0=gbc[:], scalar1=1.0 - lam_init, scalar2=None, op0=ALU.mult),
         reads=[gbc], writes=[gbc])
    epsb = k.sb([128, 1])
    k.op("dve", lambda e: e.memset(epsb[:], EPS), writes=[epsb])
    kr = k.sb([64, NK])
    vaug = k.sb([128, NKT, 65])
    ta = [k.sb([64, 512]) for _ in range(2)]
    tb = [k.sb([64, 512]) for _ in range(2)]
    tcs = [k.sb([64, 512]) for _ in range(2)]
    tsn = [k.sb([64, 512]) for _ in range(2)]
    qr = [k.sb([64, 512]) for _ in range(2)]
    Sps = [[k.ps([128, 512]) for _ in range(2)] for _ in range(2)]
    Psb = [[k.sb([128, 512]) for _ in range(2)] for _ in range(2)]
    accp = [k.ps([128, 512]) for _ in range(2)]
    ot = [k.sb([128, 4, 64]) for _ in range(2)]
    o1 = k.sb([128, 4, 64]); o2 = k.sb([128, 4, 64]); osq = k.sb([128, 4, 64])
    r0 = k.sb([128, 4]); r1 = k.sb([128, 4]); ss = k.sb([128, 4])
    vv = g.TM.a[:, 0:384].rearrange("(t p) c -> p t c", p=128)
    dav = g.DA.a.rearrange("(s p) c -> p s c", p=128)
    state = {"i": 0, "q": 0, "o": 0}

    def rope(dst_t, dst_ap, src, ssrc, ct, st, r0_, c0, n):
        i = state["i"] % 2
        state["i"] += 1
        a, b_, c_, s_ = ta[i], tb[i], tcs[i], tsn[i]
        k.dma("sp", a[:, :n], src[r0_:r0_ + 64, c0:c0 + n], reads=[src], writes=[a])
        k.dma("sp", b_[:, :n], ssrc[r0_:r0_ + 64, c0:c0 + n], reads=[ssrc], writes=[b_])
        k.dma("sp", c_[:, :n], ct[0:64, c0:c0 + n], reads=[ct], writes=[c_])
        k.dma("sp", s_[:, :n], st[0:64, c0:c0 + n], reads=[st], writes=[s_])
        k.op("pool", lambda e: e.tensor_tensor(out=a[:, :n], in0=a[:, :n], in1=c_[:, :n], op=ALU.mult),
             reads=[a, c_], writes=[a])
        k.op("pool", lambda e: e.tensor_tensor(out=b_[:, :n], in0=b_[:, :n], in1=s_[:, :n], op=ALU.mult),
             reads=[b_, s_], writes=[b_])
        k.op("pool", lambda e: e.tensor_tensor(out=dst_ap, in0=a[:, :n], in1=b_[:, :n], op=ALU.add),
             reads=[a, b_], writes=[dst_t])

    def attend(qt, nq, nkt, out_dram_t, out_ap):
        nsub = nq // 128
        for kt in range(nkt):
            for m in range(2):
                buf = kt % 2
                sp_, pb = Sps[m][buf], Psb[m][buf]
                k.op("pe", lambda e: e.matmul(sp_[:, :nq], kr[32 * m:32 * m + 32, kt * 128:(kt + 1) * 128],
                                              qt[32 * m:32 * m + 32, :nq], start=True, stop=True),
                     reads=[kr, qt], writes=[sp_])
                k.op("act", lambda e: e.activation(out=pb[:, :nq], in_=sp_[:, :nq], func=AF.Exp, scale=scale),
                     reads=[sp_], writes=[pb])
                for sub in range(nsub):
                    k.op("pe", lambda e: e.matmul(accp[m][:, sub * 65:(sub + 1) * 65], pb[:, sub * 128:(sub + 1) * 128],
                                                  vaug[:, kt, :], start=(kt == 0 and sub == 0),
                                                  stop=(kt == nkt - 1), skip_group_check=True),
                         reads=[pb, vaug], writes=[accp[m]])
        a0 = accp[0].a[:, 0:nsub * 65].rearrange("p (s c) -> p s c", c=65)
        a1 = accp[1].a[:, 0:nsub * 65].rearrange("p (s c) -> p s c", c=65)
        o = ot[state["o"] % 2]
        state["o"] += 1
        k.op("dve", lambda e: e.reciprocal(out=r0[:, :nsub], in_=a0[:, :, 64]), reads=[accp[0]], writes=[r0])
        k.op("dve", lambda e: e.reciprocal(out=r1[:, :nsub], in_=a1[:, :, 64]), reads=[accp[1]], writes=[r1])
        k.op("dve", lambda e: e.tensor_scalar(out=r1[:, :nsub], in0=r1[:, :nsub], scalar1=nlam[:], scalar2=None,
                                              op0=ALU.mult), reads=[r1, nlam], writes=[r1])
        k.op("dve", lambda e: e.tensor_tensor(out=o1[:, :nsub, :], in0=a0[:, :, 0:64],
                                              in1=r0[:, :nsub].unsqueeze(2).broadcast_to([128, nsub, 64]), op=ALU.mult),
             reads=[accp[0], r0], writes=[o1])
        k.op("dve", lambda e: e.tensor_tensor(out=o2[:, :nsub, :], in0=a1[:, :, 0:64],
                                              in1=r1[:, :nsub].unsqueeze(2).broadcast_to([128, nsub, 64]), op=ALU.mult),
             reads=[accp[1], r1], writes=[o2])
        k.op("dve", lambda e: e.tensor_tensor(out=o1[:, :nsub, :], in0=o1[:, :nsub, :], in1=o2[:, :nsub, :], op=ALU.add),
             reads=[o1, o2], writes=[o1])
        k.op("pool", lambda e: e.tensor_tensor(out=osq[:, :nsub, :], in0=o1[:, :nsub, :], in1=o1[:, :nsub, :], op=ALU.mult),
             reads=[o1], writes=[osq])
        k.op("dve", lambda e: e.tensor_reduce(out=ss[:, :nsub], in_=osq[:, :nsub, :], op=ALU.add, axis=AX.X),
             reads=[osq], writes=[ss])
        k.op("act", lambda e: e.activation(out=ss[:, :nsub], in_=ss[:, :nsub], func=AF.Sqrt, scale=1.0 / 64, bias=epsb[:]),
             reads=[ss, epsb], writes=[ss])
        k.op("dve", lambda e: e.reciprocal(out=ss[:, :nsub], in_=ss[:, :nsub]), reads=[ss], writes=[ss])
        k.op("dve", lambda e: e.tensor_tensor(out=o1[:, :nsub, :], in0=o1[:, :nsub, :],
                                              in1=ss[:, :nsub].unsqueeze(2).broadcast_to([128, nsub, 64]), op=ALU.mult),
             reads=[o1, ss], writes=[o1])
        k.op("dve", lambda e: e.tensor_tensor(out=o[:, :nsub, :], in0=o1[:, :nsub, :],
                                              in1=gbc[:].unsqueeze(1).broadcast_to([128, nsub, 64]), op=ALU.mult),
             reads=[o1, gbc], writes=[o])
        k.dma("pool", out_ap, o[:, :nsub, :], reads=[o], acc=[out_dram_t])

    for head in range(6):
        for c0 in range(0, NK, 512):
            n = min(512, NK - c0)
            rope(kr, kr[:, c0:c0 + n], g.KT_, g.KST_, g.cosk, g.sink, head * 64, c0, n)
        k.op("dve", lambda e: e.memset(vaug[:], 1.0), writes=[vaug])
        for t0 in range(0, NKT, 11):
            t1 = min(NKT, t0 + 11)
            k.dma("sp", vaug[:, t0:t1, 0:64], vv[:, t0:t1, head * 64:(head + 1) * 64], reads=[g.TM], writes=[vaug])
        for qb in range(NQ // 512):
            qt = qr[state["q"] % 2]
            state["q"] += 1
            rope(qt, qt[:, :512], g.QT_, g.QST_, g.cosq, g.sinq, head * 64, qb * 512, 512)
            attend(qt, 512, NKT, g.DA, dav[:, qb * 4:(qb + 1) * 4, head * 64:(head + 1) * 64])
        if ctx_out:
            qt = qr[state["q"] % 2]
            state["q"] += 1
            k.dma("sp", qt[:, :NCTX], g.QCT[head * 64:(head + 1) * 64, :], reads=[g.QCT], writes=[qt])
            attend(qt, NCTX, NCTX // 128, g.DAC,
                   g.DAC.a.rearrange("(s p) c -> p s c", p=128)[:, :, head * 64:(head + 1) * 64])
    k.stage_end()


def st_mlstm(k, g, L):
    k.stage_begin()

    def load(t):
        s = k.sb(list(t.a.shape))
        k.dma("sp", s[:], t[:], reads=[t], writes=[s])
        return s
    msks = load(g.msk); ident = load(g.idn)
    ones = k.sb([128, 96])
    k.op("dve", lambda e: e.memset(ones[:], 1.0), writes=[ones])
    epsb = k.sb([128, 1])
    k.op("dve", lambda e: e.memset(epsb[:], EPS), writes=[epsb])
    gbs = k.sb([128, 8])
    k.dma("sp", gbs[:], L.gb.a.partition_broadcast(128), reads=[L.gb], writes=[gbs])
    mngs = k.sb([128, 192])
    k.dma("sp", mngs[:], L.mng.a.partition_broadcast(128), reads=[L.mng], writes=[mngs])
    W = NT * 128 + 2
    qc = k.sb([96, W]); kc = k.sb([96, W])
    xin = [k.sb([96, 2050]) for _ in range(2)]
    tmp = [k.sb([96, 2048]) for _ in range(2)]
    cws = k.sb([96, 8])
    vaug = k.sb([128, NT, 97])
    G = k.sb([128, NT, 4]); EB = k.sb([128, NT, 2]); DEC = k.sb([96, NT, 2])
    Hsb = k.sb([128, NT, 96])
    Cst = [k.sb([96, 97]) for _ in range(2)]
    LU = [k.sb([128, 128]) for _ in range(2)]
    DT = [k.sb([128, 128]) for _ in range(2)]
    PT = [k.sb([128, 128]) for _ in range(2)]
    isb = [k.sb([128, 97]) for _ in range(2)]
    nd = [k.sb([128, 97]) for _ in range(2)]
    kw = [k.sb([128, 96]) for _ in range(2)]
    den = [k.sb([128, 1]) for _ in range(2)]
    osb = [k.sb([128, 96]) for _ in range(2)]
    yt = [k.sb([128, 96]) for _ in range(2)]
    hsq = k.sb([128, 96])
    ssn = [k.sb([128, 1]) for _ in range(2)]
    Bps = [k.ps([128, 512]) for _ in range(2)]
    Sps = [k.ps([128, 128]) for _ in range(2)]
    bigps = Bps
    ips = k.ps([128, 97]); nps = k.ps([128, 97]); kps = k.ps([128, 96]); dps = k.ps([96, 97])
    mvv = g.TM.a[:, 384:576].rearrange("(t p) (h c) -> p t h c", p=128, h=2)
    mov = g.TM.a[:, 576:768].rearrange("(t p) (h c) -> p t h c", p=128, h=2)
    gtv = g.TM.a[:, 768:776].rearrange("(t p) (h c) -> p t h c", p=128, h=2)
    cnt = {"i": 0}

    for hd in range(2):
        k.dma("sp", cws[:], L.cw[hd], reads=[L.cw], writes=[cws])
        for (src, dst, o0, scl) in ((g.MQP, qc, 0, None), (g.MKP, kc, 4, 96 ** -0.5)):
            for c0 in range(0, W, 2048):
                n = min(2048, W - c0)
                i = cnt["i"] % 2
                cnt["i"] += 1
                xi, tm = xin[i], tmp[i]
                k.dma("sp", xi[:, :n + 2], src[hd, :, c0:c0 + n + 2], reads=[src], writes=[xi])
                k.op("dve", lambda e: e.tensor_scalar(out=tm[:, :n], in0=xi[:, 0:n], scalar1=cws[:, o0:o0 + 1],
                                                      scalar2=cws[:, o0 + 3:o0 + 4], op0=ALU.mult, op1=ALU.add),
                     reads=[xi, cws], writes=[tm])
                k.op("dve", lambda e: e.scalar_tensor_tensor(out=tm[:, :n], in0=xi[:, 1:n + 1], scalar=cws[:, o0 + 1:o0 + 2],
                                                             in1=tm[:, :n], op0=ALU.mult, op1=ALU.add),
                     reads=[xi, cws, tm], writes=[tm])
                k.op("dve", lambda e: e.scalar_tensor_tensor(out=tm[:, :n], in0=xi[:, 2:n + 2], scalar=cws[:, o0 + 2:o0 + 3],
                                                             in1=tm[:, :n], op0=ALU.mult, op1=ALU.add),
                     reads=[xi, cws, tm], writes=[tm])
                k.op("act", lambda e: e.activation(out=dst[:, c0:c0 + n], in_=tm[:, :n], func=AF.Silu),
                     reads=[tm], writes=[dst])
                if scl is not None:
                    k.op("pool", lambda e: e.tensor_scalar(out=dst[:, c0:c0 + n], in0=dst[:, c0:c0 + n], scalar1=scl,
                                                           scalar2=None, op0=ALU.mult), reads=[dst], writes=[dst])
        k.op("dve", lambda e: e.memset(vaug[:], 1.0), writes=[vaug])
        for t0 in range(0, NT, 11):
            k.dma("sp", vaug[:, t0:t0 + 11, 0:96], mvv[:, t0:t0 + 11, hd, :], reads=[g.TM], writes=[vaug])
        k.dma("sp", G[:], gtv[:, :, hd, :], reads=[g.TM], writes=[G])
        k.op("dve", lambda e: e.tensor_tensor(out=G[:], in0=G[:], in1=gbs[:, hd * 4:(hd + 1) * 4].unsqueeze(1).broadcast_to([128, NT, 4]),
                                              op=ALU.add), reads=[G, gbs], writes=[G])
        Gd = G.a.rearrange("p t (d y) -> p t d y", d=2)
        k.op("act", lambda e: e.activation(out=Gd[:, :, :, 1], in_=Gd[:, :, :, 1], func=AF.Exp, scale=-1.0), reads=[G], writes=[G])
        k.op("act", lambda e: e.activation(out=Gd[:, :, :, 1], in_=Gd[:, :, :, 1], func=AF.Ln, bias=1.0), reads=[G], writes=[G])
        k.op("dve", lambda e: e.tensor_scalar(out=Gd[:, :, :, 1], in0=Gd[:, :, :, 1], scalar1=-1.0, scalar2=None, op0=ALU.mult),
             reads=[G], writes=[G])
        G2 = G.a.rearrange("p t c -> p (t c)")
        for dr in range(2):
            k.op("pe", lambda e: e.matmul(bigps[dr][:, 0:NT * 4], msks[:, 1 + 3 * dr, :], G2, start=True, stop=True),
                 reads=[msks, G], writes=[bigps[dr]])
            bv = bigps[dr].a[:, 0:NT * 4].rearrange("p (t c) -> p t c", c=4)
            k.op("act", lambda e: e.activation(out=EB[:, :, dr], in_=bv[:, :, 2 * dr + 1], func=AF.Exp),
                 reads=[bigps[dr]], writes=[EB])
        k.op("pe", lambda e: e.matmul(bigps[0][0:96, 0:NT * 4], ones[:], G2, start=True, stop=True),
             reads=[ones, G, EB], writes=[bigps[0]])
        bv = bigps[0].a[0:96, 0:NT * 4].rearrange("p (t d y) -> p t d y", d=2, y=2)
        k.op("act", lambda e: e.activation(out=DEC[:], in_=bv[:, :, :, 1], func=AF.Exp), reads=[bigps[0]], writes=[DEC])

        for dr in range(2):
            order = list(range(NT)) if dr == 0 else [1, 0] + list(range(NT - 1, 1, -1))
            U, TR, MK = msks.a[:, 3 * dr, :], msks.a[:, 3 * dr + 1, :], msks.a[:, 3 * dr + 2, :]
            last = 127 if dr == 0 else 0
            cs = Cst[0]
            k.op("dve", lambda e: e.memset(cs[:], 0.0), writes=[cs])
            for si, t in enumerate(order):
                b2 = si % 2
                c0 = mcol(t)
                lf = G.a[:, t, 2 * dr + 1:2 * dr + 2]
                li = G.a[:, t, 2 * dr:2 * dr + 1]
                lu, dt_, pt, bp, sp_ = LU[b2], DT[b2], PT[b2], Bps[b2], Sps[b2]
                k.op("pool", lambda e: e.tensor_scalar(out=lu[:], in0=U, scalar1=lf, scalar2=None, op0=ALU.mult),
                     reads=[msks, G], writes=[lu])
                k.op("pe", lambda e: e.matmul(bp[:, 0:128], lu[:], TR, start=True, stop=False), reads=[lu, msks], writes=[bp])
                k.op("pe", lambda e: e.matmul(bp[:, 0:128], ident[:], MK, start=False, stop=True), reads=[ident, msks], writes=[bp])
                k.op("act", lambda e: e.activation(out=dt_[:], in_=bp[:, 0:128], func=AF.Exp, bias=li), reads=[bp, G], writes=[dt_])
                k.op("pe", lambda e: e.matmul(sp_[:], kc[:, c0:c0 + 128], qc[:, c0:c0 + 128], start=True, stop=True),
                     reads=[kc, qc], writes=[sp_])
                k.op("dve", lambda e: e.tensor_tensor(out=pt[:], in0=sp_[:], in1=dt_[:], op=ALU.mult),
                     reads=[sp_, dt_], writes=[pt])
                k.op("pe", lambda e: e.matmul(ips[:], pt[:], vaug[:, t, :], start=True, stop=True), reads=[pt, vaug], writes=[ips])
                k.op("pe", lambda e: e.matmul(nps[:], qc[:, c0:c0 + 128], cs[:], start=True, stop=True),
                     reads=[qc, cs], writes=[nps])
                ib, ndb, dn = isb[b2], nd[b2], den[b2]
                k.op("dve", lambda e: e.tensor_copy(out=ib[:], in_=ips[:]), reads=[ips], writes=[ib])
                k.op("dve", lambda e: e.scalar_tensor_tensor(out=ndb[:], in0=nps[:], scalar=EB[:, t, dr:dr + 1], in1=ib[:],
                                                             op0=ALU.mult, op1=ALU.add), reads=[nps, EB, ib], writes=[ndb])
                k.op("dve", lambda e: e.tensor_scalar(out=dn[:], in0=ndb[:, 96:97], scalar1=1.0, scalar2=None, op0=ALU.max),
                     reads=[ndb], writes=[dn])
                k.op("dve", lambda e: e.scalar_tensor_tensor(out=dn[:], in0=ndb[:, 96:97], scalar=-1.0, in1=dn[:],
                                                             op0=ALU.mult, op1=ALU.max), reads=[ndb, dn], writes=[dn])
                k.op("dve", lambda e: e.reciprocal(out=dn[:], in_=dn[:]), reads=[dn], writes=[dn])
                if dr == 0:
                    k.op("dve", lambda e: e.tensor_scalar(out=Hsb[:, t, :], in0=ndb[:, 0:96], scalar1=dn[:], scalar2=None,
                                                          op0=ALU.mult), reads=[ndb, dn], writes=[Hsb])
                else:
                    y, ob_, s1 = yt[b2], osb[b2], ssn[b2]
                    k.dma("sp", ob_[:], mov[:, t, hd, :], reads=[g.TM], writes=[ob_])
                    k.op("dve", lambda e: e.scalar_tensor_tensor(out=y[:], in0=ndb[:, 0:96], scalar=dn[:], in1=Hsb[:, t, :],
                                                                 op0=ALU.mult, op1=ALU.add), reads=[ndb, dn, Hsb], writes=[y])
                    k.op("dve", lambda e: e.tensor_tensor(out=hsq[:], in0=y[:], in1=y[:], op=ALU.mult), reads=[y], writes=[hsq])
                    k.op("dve", lambda e: e.tensor_reduce(out=s1[:], in_=hsq[:], op=ALU.add, axis=AX.X), reads=[hsq], writes=[s1])
                    k.op("act", lambda e: e.activation(out=s1[:], in_=s1[:], func=AF.Ln, scale=1.0 / 96, bias=epsb[:]),
                         reads=[s1, epsb], writes=[s1])
                    k.op("act", lambda e: e.activation(out=s1[:], in_=s1[:], func=AF.Exp, scale=-0.5), reads=[s1], writes=[s1])
                    k.op("dve", lambda e: e.scalar_tensor_tensor(out=y[:], in0=y[:], scalar=s1[:], in1=mngs[:, hd * 96:(hd + 1) * 96],
                                                                 op0=ALU.mult, op1=ALU.mult), reads=[y, s1, mngs], writes=[y])
                    k.op("act", lambda e: e.activation(out=ob_[:], in_=ob_[:], func=AF.Exp, scale=-1.0), reads=[ob_], writes=[ob_])
                    k.op("pool", lambda e: e.tensor_scalar(out=ob_[:], in0=ob_[:], scalar1=1.0, scalar2=None, op0=ALU.add),
                         reads=[ob_], writes=[ob_])
                    k.op("dve", lambda e: e.reciprocal(out=ob_[:], in_=ob_[:]), reads=[ob_], writes=[ob_])
                    k.op("dve", lambda e: e.tensor_tensor(out=y[:], in0=y[:], in1=ob_[:], op=ALU.mult), reads=[y, ob_], writes=[y])
                    cc = 128 + hd * 96
                    if t < 2:
                        k.dma("pool", g.CMINE[t * 128:(t + 1) * 128, cc:cc + 96], y[:], reads=[y], acc=[g.CMINE])
                    else:
                        k.dma("pool", g.SEND[(t - 2) * 128:(t - 1) * 128, cc:cc + 96], y[:], reads=[y], acc=[g.SEND])
                kwb = kw[b2]
                k.op("pe", lambda e: e.transpose(kps[:], kc[:, c0:c0 + 128], ident[0:96, 0:96]), reads=[kc, ident], writes=[kps])
                k.op("dve", lambda e: e.tensor_scalar(out=kwb[:], in0=kps[:], scalar1=dt_[:, last:last + 1], scalar2=None,
                                                      op0=ALU.mult), reads=[kps, dt_], writes=[kwb])
                k.op("pe", lambda e: e.matmul(dps[:], kwb[:], vaug[:, t, :], start=True, stop=True), reads=[kwb, vaug], writes=[dps])
                cn = Cst[(si + 1) % 2]
                k.op("dve", lambda e: e.scalar_tensor_tensor(out=cn[:], in0=cs[:], scalar=DEC[:, t, dr:dr + 1], in1=dps[:],
                                                             op0=ALU.mult, op1=ALU.add), reads=[cs, DEC, dps], writes=[cn])
                cs = cn
    k.stage_end()


def st_fourier(k, g, L, ctx_out):
    k.stage_begin()

    def load(t):
        s = k.sb(list(t.a.shape))
        k.dma("sp", s[:], t[:], reads=[t], writes=[s])
        return s
    fws = load(L.fwblk); ccbs = load(g.ccb); cs128s = load(g.cs128); tws = load(g.tw); cs64s = load(g.cs64)
    AB = k.sb([128, 256])
    yps = [k.ps([128, 512]) for _ in range(2)]
    pab = yps[0]
    for i in range(2):
        k.op("pe", lambda e: e.matmul(pab[:, i * 128:(i + 1) * 128], ccbs[:, i * 128:(i + 1) * 128], fws[:],
                                      start=True, stop=True), reads=[ccbs, fws], writes=[pab])
    k.op("dve", lambda e: e.tensor_copy(out=AB[:], in_=pab[:, 0:256]), reads=[pab], writes=[AB])
    xs = k.sb([128, S])
    for i in range(4):
        k.dma("sp", xs[:, i * 2048:(i + 1) * 2048], g.FT_[:, NCTX + i * 2048:NCTX + (i + 1) * 2048], reads=[g.FT_], writes=[xs])
    Y = k.sb([128, 64, 256])
    xv = xs.a.rearrange("p (n2 n1) -> p n1 n2", n1=64)
    for i in range(32):
        yp = yps[i % 2]
        for j in range(2):
            n1 = 2 * i + j
            k.op("pe", lambda e: e.matmul(yp[:, j * 256:(j + 1) * 256], xv[:, n1, :], AB[:], start=True, stop=True),
                 reads=[xs, AB], writes=[yp])
        k.evac(Y, Y[:, 2 * i:2 * i + 2, :].rearrange("p a b -> p (a b)"), yp, yp[:])
    zps = [k.ps([64, 512]) for _ in range(4)]
    zr = [k.sb([64, 4, 128]) for _ in range(2)]
    zi = [k.sb([64, 4, 128]) for _ in range(2)]
    t1 = k.sb([64, 2, 128]); t2 = k.sb([64, 2, 128])
    ops_ = [k.ps([128, 256]) for _ in range(2)]
    FTM = k.sb([128, 64, 128])
    tcb = tws.a[:, 0:128].unsqueeze(1).broadcast_to([64, 2, 128])
    tsb = tws.a[:, 128:256].unsqueeze(1).broadcast_to([64, 2, 128])
    for blk in range(32):
        zrb, zib = zr[blk % 2], zi[blk % 2]
        for hb in range(2):
            zp = zps[(2 * blk + hb) % 4]
            for j in range(2):
                d = blk * 4 + hb * 2 + j
                k.op("pe", lambda e: e.matmul(zp[:, j * 256:(j + 1) * 256], Y[:, :, d], cs128s[:, 0:256],
                                              start=True, stop=False), reads=[Y, cs128s], writes=[zp])
                k.op("pe", lambda e: e.matmul(zp[:, j * 256:(j + 1) * 256], Y[:, :, 128 + d], cs128s[:, 256:512],
                                              start=False, stop=True), reads=[Y, cs128s], writes=[zp])
            zv = zp.a.rearrange("p (d c m) -> p d c m", d=2, c=2)
            sl = slice(hb * 2, hb * 2 + 2)
            k.op("dve", lambda e: e.tensor_tensor(out=t1[:], in0=zv[:, :, 0, :], in1=tcb, op=ALU.mult),
                 reads=[zp, tws], writes=[t1])
            k.op("dve", lambda e: e.tensor_tensor(out=t2[:], in0=zv[:, :, 1, :], in1=tsb, op=ALU.mult),
                 reads=[zp, tws], writes=[t2])
            k.op("pool", lambda e: e.tensor_tensor(out=zrb[:, sl, :], in0=t1[:], in1=t2[:], op=ALU.subtract),
                 reads=[t1, t2], writes=[zrb])
            k.op("dve", lambda e: e.tensor_tensor(out=t1[:], in0=zv[:, :, 0, :], in1=tsb, op=ALU.mult),
                 reads=[zp, tws], writes=[t1])
            k.op("dve", lambda e: e.tensor_tensor(out=t2[:], in0=zv[:, :, 1, :], in1=tcb, op=ALU.mult),
                 reads=[zp, tws], writes=[t2])
            k.op("pool", lambda e: e.tensor_tensor(out=zib[:, sl, :], in0=t1[:], in1=t2[:], op=ALU.add),
                 reads=[t1, t2], writes=[zib])
        op_ = ops_[blk % 2]
        for j in range(4):
            k.op("pe", lambda e: e.matmul(op_[:, j * 64:(j + 1) * 64], zrb[:, j, :], cs64s[:, 0:64], start=True, stop=False),
                 reads=[cs64s, zrb], writes=[op_])
            k.op("pe", lambda e: e.matmul(op_[:, j * 64:(j + 1) * 64], zib[:, j, :], cs64s[:, 64:128], start=False, stop=True),
                 reads=[cs64s, zib], writes=[op_])
        k.evac(FTM, FTM[:, :, blk * 4:(blk + 1) * 4], op_, op_.a.rearrange("p (d m) -> p m d", d=4))
    sv = g.SEND.a[:, 0:128].rearrange("(m1 m2) d -> m2 m1 d", m2=128)
    for q4 in range(4):
        k.dma("pool", sv[:, q4 * 16:(q4 + 1) * 16, :], FTM[:, q4 * 16:(q4 + 1) * 16, :], reads=[FTM], acc=[g.SEND])
    if ctx_out:
        xcs = k.sb([128, NCTX])
        k.dma("sp", xcs[:], g.FT_[:, 0:NCTX], reads=[g.FT_], writes=[xcs])
        dfs = load(g.dftc)
        Yc = k.sb([128, 2, 256])
        for t in range(2):
            yp = yps[t % 2]
            k.op("pe", lambda e: e.matmul(yp[:, 0:256], xcs[:, t * 128:(t + 1) * 128], AB[:], start=True, stop=True),
                 reads=[xcs, AB], writes=[yp])
            k.evac(Yc, Yc[:, t, :], yp, yp[:, 0:256])
        for mt in range(2):
            pv = yps[mt % 2]
            i = 0
            for t in range(2):
                for c in range(2):
                    k.op("pe", lambda e: e.matmul(pv[:, 0:128], dfs[:, t, c * 256 + mt * 128:c * 256 + (mt + 1) * 128],
                                                  Yc[:, t, c * 128:(c + 1) * 128], start=(i == 0), stop=(i == 3)),
                         reads=[dfs, Yc], writes=[pv])
                    i += 1
            oc = k.sb([128, 128])
            k.evac(oc, oc[:], pv, pv[:, 0:128])
            k.dma("pool", g.CMINE[mt * 128:(mt + 1) * 128, 0:128], oc[:], reads=[oc], acc=[g.CMINE])
    k.stage_end()


def st_p3(k, g, L, ctx_out):
    k.stage_begin()

    def load(t):
        s = k.sb(list(t.a.shape))
        k.dma("sp", s[:], t[:], reads=[t], writes=[s])
        return s
    modT = g.modT
    g2s = load(L.g2); wrs = load(L.wr); ident = load(g.idn); hs = load(g.hsel)
    wos = k.sb([128, 8, 8, 128])
    for j in range(8):
        k.dma("sp", wos[:, j], L.wo[j], reads=[L.wo], writes=[wos])
    ones = k.sb([128, 128])
    k.op("dve", lambda e: e.memset(ones[:], 1.0), writes=[ones])
    epsb = k.sb([128, 1])
    k.op("dve", lambda e: e.memset(epsb[:], EPS), writes=[epsb])
    a2 = k.sb([128, 8, 2])
    k.op("dve", lambda e: e.tensor_scalar(out=a2[:], in0=modT[:, 32:40, :], scalar1=1.0, scalar2=None, op0=ALU.add),
         reads=[modT], writes=[a2])
    k.op("dve", lambda e: e.tensor_tensor(out=a2[:], in0=a2[:], in1=g2s[:].unsqueeze(2).broadcast_to([128, 8, 2]),
                                          op=ALU.mult), reads=[a2, g2s], writes=[a2])
    chunks = [(i * 512, 512, 0) for i in range(TL // 512)]
    if ctx_out:
        chunks.append((TL, NCTX, 1))
    mb = k.sb([128, 8, 512])
    xb = [k.sb([128, 8, 512]) for _ in range(2)]
    x1 = k.sb([128, 8, 512])
    h2 = k.sb([128, 8, 512])
    sq = k.sb([128, 8, 512])
    TA = [k.sb([128, 2, 320]) for _ in range(2)]
    TB = [k.sb([128, 2, 320]) for _ in range(2)]
    MT = [k.sb([128, D]) for _ in range(2)]
    H2T = [k.sb([128, D]) for _ in range(2)]
    pps = [k.ps([128, 512]) for _ in range(2)]
    tps = [k.ps([128, 512]) for _ in range(2)]
    ssp = k.ps([128, 512])
    rs = k.sb([128, 512])
    lps = [k.ps([128, 16]) for _ in range(2)]
    mx = k.sb([128, 1]); sm = k.sb([128, 1]); ex = k.sb([128, 16])
    pb = [k.sb([128, 16]) for _ in range(2)]
    xov = g.xown.a.rearrange("(f p) t -> p f t", p=128)
    xcv = g.xc.a.rearrange("(f p) t -> p f t", p=128)
    x1v = g.X1T.a.rearrange("(f p) t -> p f t", p=128)
    cnt = {"s": 0, "p": 0, "t": 0}
    pieces = [(0, 0, 128, 0), (1, 0, 128, 128), (0, 128, 320, 640), (1, 128, 320, 832)]
    for ci, (t0, n, col) in enumerate(chunks):
        x_ = xb[ci % 2]
        k.dma("sp", x_[:, :, :n], (xov[:, :, t0:t0 + n] if col == 0 else xcv[:, :, :]), reads=[g.xown if col == 0 else g.xc],
              writes=[x_])
        for sub in range(n // 128):
            i = cnt["s"] % 2
            cnt["s"] += 1
            ta, tb, mt = TA[i], TB[i], MT[i]
            r0 = t0 + sub * 128
            if col == 0:
                for r in range(2):
                    ra, rb = r0, TL + r0
                    k.dma("sp", ta[:, r, :], g.MG[ra // 1024, r, ra % 1024:ra % 1024 + 128, :], reads=[g.MG], writes=[ta] if r == 0 else [], acc=[] if r == 0 else [ta])
                    k.dma("sp", tb[:, r, :], g.MG[rb // 1024, r, rb % 1024:rb % 1024 + 128, :], reads=[g.MG], writes=[tb] if r == 0 else [], acc=[] if r == 0 else [tb])
                k.dma("sp", mt[:, 256:640], g.DA[r0:r0 + 128, :], reads=[g.DA], writes=[mt])
                k.op("pool", lambda e: e.tensor_tensor(out=tb[:], in0=tb[:], in1=ta[:], op=ALU.subtract), reads=[ta, tb], writes=[tb])
                for (r, c0, c1, dc) in pieces:
                    k.op("dve", lambda e: e.scalar_tensor_tensor(out=mt[:, dc:dc + c1 - c0], in0=tb[:, r, c0:c1], scalar=hs[:],
                                                                 in1=ta[:, r, c0:c1], op0=ALU.mult, op1=ALU.add),
                         reads=[tb, hs, ta], writes=[mt])
            else:
                rr = sub * 128
                for (r, c0, c1, dc) in pieces:
                    k.dma("sp", mt[:, dc:dc + c1 - c0], g.CG[r, rr:rr + 128, c0:c1], reads=[g.CG], writes=[mt])
                k.dma("sp", mt[:, 256:640], g.DAC[rr:rr + 128, :], reads=[g.DAC], writes=[mt])
            for hf in range(2):
                tp = tps[cnt["t"] % 2]
                cnt["t"] += 1
                for j in range(4):
                    c = hf * 4 + j
                    k.op("pe", lambda e: e.transpose(tp[:, j * 128:(j + 1) * 128], mt[:, c * 128:(c + 1) * 128], ident[:]),
                         reads=[mt, ident], writes=[tp])
                k.evac(mb, mb[:, hf * 4:hf * 4 + 4, sub * 128:(sub + 1) * 128], tp, tp[:].rearrange("p (a b) -> p a b", a=4))
        for j in range(8):
            pp = pps[cnt["p"] % 2]
            cnt["p"] += 1
            for f in range(8):
                k.op("pe", lambda e: e.matmul(pp[:, :n], wos[:, j, f, :], mb[:, f, :n], start=(f == 0), stop=(f == 7)),
                     reads=[wos, mb], writes=[pp])
            k.op("dve", lambda e: e.scalar_tensor_tensor(out=x1[:, j, :n], in0=pp[:, :n], scalar=modT[:, 16 + j, col:col + 1],
                                                         in1=x_[:, j, :n], op0=ALU.mult, op1=ALU.add),
                 reads=[pp, modT, x_], writes=[x1])
        k.dma("pool", x1v[:, :, t0:t0 + n], x1[:, :, :n], reads=[x1], acc=[g.X1T])
        k.op("act", lambda e: e.activation(out=sq[:, :, :n], in_=x1[:, :, :n], func=AF.Square), reads=[x1], writes=[sq])
        for f in range(8):
            k.op("pe", lambda e: e.matmul(ssp[:, :n], ones[:], sq[:, f, :n], start=(f == 0), stop=(f == 7)),
                 reads=[ones, sq], writes=[ssp])
        k.op("act", lambda e: e.activation(out=rs[:, :n], in_=ssp[:, :n], func=AF.Sqrt, scale=1.0 / D, bias=epsb[:]),
             reads=[ssp, epsb], writes=[rs])
        k.op("dve", lambda e: e.reciprocal(out=rs[:, :n], in_=rs[:, :n]), reads=[rs], writes=[rs])
        for f in range(8):
            k.op("dve", lambda e: e.scalar_tensor_tensor(out=h2[:, f, :n], in0=x1[:, f, :n], scalar=a2[:, f, col:col + 1],
                                                         in1=rs[:, :n], op0=ALU.mult, op1=ALU.mult),
                 reads=[x1, a2, rs], writes=[h2])
            k.op("pool", lambda e: e.tensor_scalar(out=h2[:, f, :n], in0=h2[:, f, :n], scalar1=modT[:, 24 + f, col:col + 1],
                                                   scalar2=None, op0=ALU.add), reads=[h2, modT], writes=[h2])
        for st in range(n // 128):
            ht = H2T[st % 2]
            for hf in range(2):
                tp = tps[cnt["t"] % 2]
                cnt["t"] += 1
                for j in range(4):
                    f = hf * 4 + j
                    k.op("pe", lambda e: e.transpose(tp[:, j * 128:(j + 1) * 128], h2[:, f, st * 128:(st + 1) * 128], ident[:]),
                         reads=[h2, ident], writes=[tp])
                k.evac(ht, ht[:, hf * 512:(hf + 1) * 512], tp, tp[:])
            if col == 0:
                k.dma("pool", g.H2SEND[t0 + st * 128:t0 + (st + 1) * 128, :], ht[:], reads=[ht], acc=[g.H2SEND])
            else:
                k.dma("pool", g.H2C[st * 128:(st + 1) * 128, :], ht[:], reads=[ht], acc=[g.H2C])
            lp = lps[st % 2]
            p_ = pb[st % 2]
            for f in range(8):
                k.op("pe", lambda e: e.matmul(lp[:], h2[:, f, st * 128:(st + 1) * 128], wrs[:, f, :], start=(f == 0), stop=(f == 7)),
                     reads=[h2, wrs], writes=[lp])
            k.op("dve", lambda e: e.tensor_reduce(out=mx[:], in_=lp[:], op=ALU.max, axis=AX.X, negate=True),
                 reads=[lp], writes=[mx])
            k.op("act", lambda e: e.activation(out=ex[:], in_=lp[:], func=AF.Exp, bias=mx[:], accum_out=sm[:]),
                 reads=[lp, mx], writes=[ex, sm])
            k.op("dve", lambda e: e.reciprocal(out=sm[:], in_=sm[:]), reads=[sm], writes=[sm])
            k.op("dve", lambda e: e.tensor_scalar(out=p_[:], in0=ex[:], scalar1=sm[:], scalar2=None, op0=ALU.mult),
                 reads=[ex, sm], writes=[p_])
            if col == 0:
                k.dma("pool", g.PRSEND[t0 + st * 128:t0 + (st + 1) * 128, :], p_[:], reads=[p_], acc=[g.PRSEND])
            else:
                k.dma("pool", g.PRC[st * 128:(st + 1) * 128, :], p_[:], reads=[p_], acc=[g.PRC])
    k.stage_end()


def st_moe(k, g, L, ctx_out):
    k.stage_begin()
    NL, NC_ = S, (NCTX if ctx_out else 0)
    capL, capC = 2 * NL // NEXP, 2 * NC_ // NEXP
    NSLOT = capL + (128 if NC_ else 0)
    NST = NSLOT // 128
    xs, ys = g.xs, g.ys

    def load(t):
        s = k.sb(list(t.a.shape))
        k.dma("sp", s[:], t[:], reads=[t], writes=[s])
        return s
    lsts = load(g.lst); ident = load(g.idn); hs = load(g.hsel)
    ones = k.sb([128, 128])
    k.op("dve", lambda e: e.memset(ones[:], 1.0), writes=[ones])
    banks = [k.ps([128, 512]) for _ in range(8)]
    breg = g.breg[NSLOT]

    def route(prt, N, cap, base):
        ntl = N // 128
        pr16 = k.sb([128, ntl, 16])
        prs = k.sb([128, ntl, 8]); cmp_ = k.sb([128, ntl, 8]); mask = k.sb([128, ntl, 8]); gate = k.sb([128, ntl, 8])
        off = k.sb([128, ntl, 8]); idxf = k.sb([128, ntl, 8]); idx = k.sb([128, ntl, 8], I32)
        lo = k.sb([128, 8]); mid = k.sb([128, 8]); cpart = k.sb([128, 8]); sel = k.sb([128, 8])
        k.dma("sp", pr16[:], prt.a.rearrange("(t p) e -> p t e", p=128), reads=[prt], writes=[pr16])
        k.op("dve", lambda e: e.tensor_tensor(out=cmp_[:], in0=pr16[:, :, 8:16], in1=pr16[:, :, 0:8], op=ALU.subtract),
             reads=[pr16], writes=[cmp_])
        k.op("dve", lambda e: e.scalar_tensor_tensor(out=prs[:], in0=cmp_[:], scalar=hs[:], in1=pr16[:, :, 0:8],
                                                     op0=ALU.mult, op1=ALU.add), reads=[cmp_, hs, pr16], writes=[prs])
        k.op("dve", lambda e: e.memset(lo[:], 0.0), writes=[lo])
        tot = banks[6]
        for it in range(NBIS):
            w = 2.0 ** -(it + 1)
            k.op("dve", lambda e: e.tensor_scalar(out=mid[:], in0=lo[:], scalar1=w, scalar2=None, op0=ALU.add),
                 reads=[lo], writes=[mid])
            k.op("dve", lambda e: e.tensor_tensor(out=cmp_[:], in0=prs[:], in1=mid[:].unsqueeze(1).broadcast_to([128, ntl, 8]),
                                                  op=ALU.is_gt), reads=[prs, mid], writes=[cmp_])
            k.op("dve", lambda e: e.tensor_reduce(out=cpart[:], in_=cmp_[:].rearrange("p t e -> p e t"), op=ALU.add, axis=AX.X),
                 reads=[cmp_], writes=[cpart])
            k.op("pe", lambda e: e.matmul(tot[:, 0:8], ones[:], cpart[:], start=True, stop=True), reads=[ones, cpart], writes=[tot])
            k.op("dve", lambda e: e.tensor_scalar(out=sel[:], in0=tot[:, 0:8], scalar1=cap - 0.5, scalar2=w, op0=ALU.is_ge,
                                                  op1=ALU.mult), reads=[tot], writes=[sel])
            k.op("dve", lambda e: e.tensor_tensor(out=lo[:], in0=lo[:], in1=sel[:], op=ALU.add), reads=[lo, sel], writes=[lo])
        k.op("dve", lambda e: e.tensor_tensor(out=mask[:], in0=prs[:], in1=lo[:].unsqueeze(1).broadcast_to([128, ntl, 8]),
                                              op=ALU.is_gt), reads=[prs, lo], writes=[mask])
        k.op("dve", lambda e: e.tensor_tensor(out=gate[:], in0=prs[:], in1=mask[:], op=ALU.mult), reads=[prs, mask], writes=[gate])
        m2 = mask.a.rearrange("p t e -> p (t e)")
        pre, cnt = banks[6], banks[7]
        k.op("pe", lambda e: e.matmul(pre[:, 0:ntl * 8], lsts[:], m2, start=True, stop=True), reads=[lsts, mask], writes=[pre])
        k.op("pe", lambda e: e.matmul(cnt[:, 0:ntl * 8], ones[:], m2, start=True, stop=True), reads=[ones, mask], writes=[cnt])
        cv = cnt.a[:, 0:ntl * 8].rearrange("p (t e) -> p t e", e=8)
        pv = pre.a[:, 0:ntl * 8].rearrange("p (t e) -> p t e", e=8)
        k.op("dve", lambda e: e.memset(off[:, 0, :], float(base)), writes=[off])
        for i in range(ntl - 1):
            k.op("dve", lambda e: e.tensor_tensor(out=off[:, i + 1, :], in0=cv[:, i, :], in1=off[:, i, :], op=ALU.add),
                 reads=[cnt, off], writes=[off])
        k.op("dve", lambda e: e.tensor_tensor(out=off[:], in0=pv, in1=off[:], op=ALU.add), reads=[pre, off], writes=[off])
        k.op("dve", lambda e: e.tensor_scalar(out=idxf[:], in0=mask[:], scalar1=-8192.0, scalar2=8192.0, op0=ALU.mult, op1=ALU.add),
             reads=[mask], writes=[idxf])
        k.op("dve", lambda e: e.tensor_tensor(out=idxf[:], in0=idxf[:], in1=off[:], op=ALU.add), reads=[idxf, off], writes=[idxf])
        k.op("dve", lambda e: e.tensor_copy(out=idx[:], in_=idxf[:]), reads=[idxf], writes=[idx])
        return idx, gate, ntl

    sets = [(g.H2ALL, g.PRALL, NL, capL, 0, g.PART)]
    if NC_:
        sets.append((g.H2C, g.PRC, NC_, capC, capL, g.PARTC))
    stage = [k.sb([128, D]) for _ in range(2)]
    routed = []
    ns = 0
    for (ht, prt, N, cap, base, po) in sets:
        idx, gate, ntl = route(prt, N, cap, base)
        routed.append((idx, gate, ntl, po))
        for i in range(ntl):
            st_ = stage[ns % 2]
            ns += 1
            if ht is g.H2ALL:
                rr_, tt_ = (i * 128) // TL, (i * 128) % TL
                k.dma("sp", st_[:], ht[tt_ // 256, rr_, tt_ % 256:tt_ % 256 + 128, :], reads=[ht], writes=[st_])
            else:
                k.dma("sp", st_[:], ht[i * 128:(i + 1) * 128, :], reads=[ht], writes=[st_])
            for e_ in range(8):
                k.dma("pool", None, None, reads=[st_, idx], acc=[xs[e_]],
                      fn=lambda eng: eng.indirect_dma_start(
                          out=xs[e_][0:NSLOT, :], out_offset=bass.IndirectOffsetOnAxis(ap=idx[:, i, e_:e_ + 1], axis=0),
                          in_=st_[:, :], in_offset=None, bounds_check=breg, oob_is_err=False))

    xsT = k.sb([128, 8, NSLOT]); hid = k.sb([128, 16, NSLOT])
    w1b = [k.sb([128, 8, 128]) for _ in range(2)]; w3b = [k.sb([128, 8, 128]) for _ in range(2)]
    w2b = [k.sb([128, 16, 256]) for _ in range(1)]
    sil = [k.sb([128, 512]) for _ in range(2)]
    yb = [k.sb([128, 256]) for _ in range(2)]
    pieces = [(c0, min(512, NSLOT - c0)) for c0 in range(0, NSLOT, 512)]
    cnt = {"w": 0, "p": 0, "y": 0}
    for e_ in range(8):
        for st in range(NST):
            sg = stage[ns % 2]
            ns += 1
            k.dma("sp", sg[:], xs[e_][st * 128:(st + 1) * 128, :], reads=[xs[e_]], writes=[sg])
            for hf in range(2):
                tp = banks[hf]
                for j in range(4):
                    kc = hf * 4 + j
                    k.op("pe", lambda e: e.transpose(tp[:, j * 128:(j + 1) * 128], sg[:, kc * 128:(kc + 1) * 128], ident[:]),
                         reads=[sg, ident], writes=[tp])
                k.evac(xsT, xsT[:, hf * 4:hf * 4 + 4, st * 128:(st + 1) * 128], tp, tp[:].rearrange("p (a b) -> p a b", a=4))
        for fg in range(16):
            wb1, wb3 = w1b[cnt["w"] % 2], w3b[cnt["w"] % 2]
            cnt["w"] += 1
            k.dma("sp", wb1[:], L.w1c[e_, fg], reads=[L.w1c], writes=[wb1])
            k.dma("sp", wb3[:], L.w3c[e_, fg], reads=[L.w3c], writes=[wb3])
            for (c0, n) in pieces:
                p1, p3 = banks[2 + cnt["p"] % 2], banks[4 + cnt["p"] % 2]
                sl_ = sil[cnt["p"] % 2]
                cnt["p"] += 1
                for kc in range(8):
                    k.op("pe", lambda e: e.matmul(p1[:, :n], wb1[:, kc, :], xsT[:, kc, c0:c0 + n], start=(kc == 0), stop=(kc == 7)),
                         reads=[wb1, xsT], writes=[p1])
                for kc in range(8):
                    k.op("pe", lambda e: e.matmul(p3[:, :n], wb3[:, kc, :], xsT[:, kc, c0:c0 + n], start=(kc == 0), stop=(kc == 7)),
                         reads=[wb3, xsT], writes=[p3])
                k.op("act", lambda e: e.activation(out=sl_[:, :n], in_=p1[:, :n], func=AF.Silu), reads=[p1], writes=[sl_])
                k.op("dve", lambda e: e.tensor_tensor(out=hid[:, fg, c0:c0 + n], in0=p3[:, :n], in1=sl_[:, :n], op=ALU.mult),
                     reads=[p3, sl_], writes=[hid])
        for dq in range(4):
            wb2 = w2b[0]
            for q4 in range(4):
                k.dma("sp", wb2[:, q4 * 4:(q4 + 1) * 4, :], L.w2c[e_, dq, :, q4 * 4:(q4 + 1) * 4, :], reads=[L.w2c], writes=[wb2])
            for st in range(NST):
                po_ = banks[cnt["y"] % 2]
                y_ = yb[cnt["y"] % 2]
                cnt["y"] += 1
                for fg in range(16):
                    k.op("pe", lambda e: e.matmul(po_[:, 0:256], hid[:, fg, st * 128:(st + 1) * 128], wb2[:, fg, :],
                                                  start=(fg == 0), stop=(fg == 15)), reads=[hid, wb2], writes=[po_])
                k.evac(y_, y_[:], po_, po_[:, 0:256])
                k.dma("pool", ys[e_][st * 128:(st + 1) * 128, dq * 256:(dq + 1) * 256], y_[:], reads=[y_], acc=[ys[e_]])

    accb = [k.sb([128, D]) for _ in range(2)]
    for s_ in stage:
        k.op("dve", lambda e: e.memset(s_[:], 0.0), writes=[s_])
    na = 0
    for (idx, gate, ntl, po) in routed:
        for i in range(ntl):
            ac = accb[na % 2]
            na += 1
            for e_ in range(8):
                g_ = stage[ns % 2]
                ns += 1
                k.dma("pool", None, None, reads=[ys[e_], idx], writes=[g_],
                      fn=lambda eng: eng.indirect_dma_start(
                          out=g_[:, :], out_offset=None, in_=ys[e_][0:NSLOT, :],
                          in_offset=bass.IndirectOffsetOnAxis(ap=idx[:, i, e_:e_ + 1], axis=0),
                          bounds_check=breg, oob_is_err=False))
                if e_ == 0:
                    k.op("dve", lambda e: e.tensor_scalar(out=ac[:], in0=g_[:], scalar1=gate[:, i, 0:1], scalar2=None, op0=ALU.mult),
                         reads=[g_, gate], writes=[ac])
                else:
                    k.op("dve", lambda e: e.scalar_tensor_tensor(out=ac[:], in0=g_[:], scalar=gate[:, i, e_:e_ + 1], in1=ac[:],
                                                                 op0=ALU.mult, op1=ALU.add), reads=[g_, gate, ac], writes=[ac])
            k.dma("sp", po[i * 128:(i + 1) * 128, :], ac[:], reads=[ac], acc=[po])
    k.stage_end()


def st_p5(k, g, L, ctx_out, final):
    k.stage_begin()
    modT = g.modT
    ident = k.sb([128, 128])
    k.dma("sp", ident[:], g.idn[:], reads=[g.idn], writes=[ident])
    if final:
        fgs = k.sb([128, 8])
        k.dma("sp", fgs[:], g.fg[:], reads=[g.fg], writes=[fgs])
        ones = k.sb([128, 128])
        k.op("dve", lambda e: e.memset(ones[:], 1.0), writes=[ones])
        epsb = k.sb([128, 1])
        k.op("dve", lambda e: e.memset(epsb[:], EPS), writes=[epsb])
        sq = k.sb([128, 8, 512]); ssp = k.ps([128, 512]); rs = k.sb([128, 512])
    chunks = [(i * 512, 512, 0) for i in range(TL // 512)]
    if ctx_out:
        chunks.append((TL, NCTX, 1))
    xb = [k.sb([128, 8, 512]) for _ in range(2)]
    mo = k.sb([128, 8, 512])
    mtile = [k.sb([128, D]) for _ in range(2)]
    tps = [k.ps([128, 512]) for _ in range(2)]
    x1v = g.X1T.a.rearrange("(f p) t -> p f t", p=128)
    cnt = {"t": 0, "m": 0}
    for ci, (t0, n, col) in enumerate(chunks):
        x_ = xb[ci % 2]
        k.dma("sp", x_[:, :, :n], x1v[:, :, t0:t0 + n], reads=[g.X1T], writes=[x_])
        for sub in range(n // 128):
            mt = mtile[cnt["m"] % 2]
            cnt["m"] += 1
            if col == 0:
                k.dma("sp", mt[:], g.MOE[t0 + sub * 128:t0 + (sub + 1) * 128, :], reads=[g.MOE], writes=[mt])
            else:
                k.dma("sp", mt[:], g.MOEC[sub * 128:(sub + 1) * 128, :], reads=[g.MOEC], writes=[mt])
            for hf in range(2):
                tp = tps[cnt["t"] % 2]
                cnt["t"] += 1
                for j in range(4):
                    c = hf * 4 + j
                    k.op("pe", lambda e: e.transpose(tp[:, j * 128:(j + 1) * 128], mt[:, c * 128:(c + 1) * 128], ident[:]),
                         reads=[mt, ident], writes=[tp])
                k.evac(mo, mo[:, hf * 4:hf * 4 + 4, sub * 128:(sub + 1) * 128], tp, tp[:].rearrange("p (a b) -> p a b", a=4))
        for f in range(8):
            k.op("dve", lambda e: e.scalar_tensor_tensor(out=x_[:, f, :n], in0=mo[:, f, :n], scalar=modT[:, 40 + f, col:col + 1],
                                                         in1=x_[:, f, :n], op0=ALU.mult, op1=ALU.add),
                 reads=[mo, modT, x_], writes=[x_])
        if final:
            k.op("act", lambda e: e.activation(out=sq[:, :, :n], in_=x_[:, :, :n], func=AF.Square), reads=[x_], writes=[sq])
            for f in range(8):
                k.op("pe", lambda e: e.matmul(ssp[:, :n], ones[:], sq[:, f, :n], start=(f == 0), stop=(f == 7)),
                     reads=[ones, sq], writes=[ssp])
            k.op("act", lambda e: e.activation(out=rs[:, :n], in_=ssp[:, :n], func=AF.Sqrt, scale=1.0 / D, bias=epsb[:]),
                 reads=[ssp, epsb], writes=[rs])
            k.op("dve", lambda e: e.reciprocal(out=rs[:, :n], in_=rs[:, :n]), reads=[rs], writes=[rs])
            for f in range(8):
                k.op("dve", lambda e: e.scalar_tensor_tensor(out=x_[:, f, :n], in0=x_[:, f, :n], scalar=fgs[:, f:f + 1],
                                                             in1=rs[:, :n], op0=ALU.mult, op1=ALU.mult),
                     reads=[x_, fgs, rs], writes=[x_])
            ov = g.out.a.rearrange("(f p) t -> p f t", p=128)
            k.dma("pool", ov[:, :, t0:t0 + n], x_[:, :, :n], reads=[x_], writes=[g.out], is_output=True)
        else:
            if col == 0:
                ov = g.xown_next.a.rearrange("(f p) t -> p f t", p=128)
                k.dma("pool", ov[:, :, t0:t0 + n], x_[:, :, :n], reads=[x_], acc=[g.xown_next])
            else:
                ov = g.xc_next.a.rearrange("(f p) t -> p f t", p=128)
                k.dma("pool", ov[:, :, :], x_[:, :, :n], reads=[x_], acc=[g.xc_next])
    k.stage_end()


def build_fused(nlayer, stop=None):
    k = KB()
    g = Ctx()
    step = [0]

    def chk(exports):
        step[0] += 1
        if stop is not None and step[0] == stop:
            for nm, t in exports:
                o = k.outp('dbg_' + nm, list(t.a.shape))
                k.dma('sp', o[:], t[:], reads=[t], writes=[o], is_output=True)
            return True
        return False
    g.xall = k.inp("xall", [16, 2, 64, TL])
    g.xown = k.inp("xown", [D, TL]); g.xc = k.inp("xc", [D, NCTX])
    g.cv = k.inp("cv", [128, 8, 2])
    g.cosk = k.inp("cosk", [128, NKALL]); g.sink = k.inp("sink", [128, NKALL])
    g.cosq = k.inp("cosq", [128, TL]); g.sinq = k.inp("sinq", [128, TL])
    g.msk = k.inp("msk", [128, 6, 128]); g.idn = k.inp("idn", [128, 128]); g.lst = k.inp("lst", [128, 128])
    g.ccb = k.inp("ccb", [128, 256]); g.cs128 = k.inp("cs128", [128, 512]); g.tw = k.inp("tw", [64, 256])
    g.cs64 = k.inp("cs64", [64, 128]); g.dftc = k.inp("dftc", [128, 2, 512])
    g.hsel = k.inp("hsel", [128, 1]); g.fg = k.inp("fg", [128, 8])
    g.out = k.outp("outT", [D, TL])
    Ls = []
    for l in range(nlayer):
        L = Ctx()
        sfx = "_%d" % l
        L.adaw = k.inp("adaw" + sfx, [12, 128, 8, 512]); L.adab = k.inp("adab" + sfx, [128, 48]); L.g1 = k.inp("g1" + sfx, [128, 8])
        L.wAfm = k.inp("wAfm" + sfx, [NFM, 128, 8, 128]); L.wAtm = k.inp("wAtm" + sfx, [128, 8, TMW])
        L.wQ = k.inp("wQ" + sfx, [6, 128, 8, 128])
        L.dlam = k.inp("dlam" + sfx, [128]); L.dng = k.inp("dng" + sfx, [64])
        L.cw = k.inp("cw" + sfx, [2, 96, 8]); L.gb = k.inp("gb" + sfx, [8]); L.mng = k.inp("mng" + sfx, [192])
        L.fwblk = k.inp("fwblk" + sfx, [128, 128])
        L.wo = k.inp("wo" + sfx, [8, 128, 8, 128]); L.g2 = k.inp("g2" + sfx, [128, 8]); L.wr = k.inp("wr" + sfx, [128, 8, 16])
        Ls.append(L)
    g.FT_ = k.dram("FT_", [128, NKALL]); g.KT_ = k.dram("KT_", [384, NKALL]); g.KST_ = k.dram("KST_", [384, NKALL])
    g.MQP = k.dram("MQP", [2, 96, XPW]); g.MKP = k.dram("MKP", [2, 96, XPW])
    g.TM = k.dram("TM", [NKALL, TMW])
    g.QT_ = k.dram("QT_", [384, TL]); g.QST_ = k.dram("QST_", [384, TL]); g.QCT = k.dram("QCT", [384, NCTX])
    g.DA = k.dram("DA", [TL, 384]); g.DAC = k.dram("DAC", [NCTX, 384])
    g.SEND = k.dram("SEND", [S, 320]); g.MG = k.dram("MGc", [8, 2, 1024, 320])
    g.CMINE = k.dram("CMINE", [NCTX, 320]); CG2 = k.dram("CG2", [2 * NCTX, 320])
    g.CG = T(CG2.a.rearrange("(r t) c -> r t c", r=2)); g.CG2 = CG2
    g.X1T = k.dram("X1T", [D, TL + NCTX])
    g.H2SEND = k.dram("H2SEND", [TL, D]); g.H2ALL = k.dram("H2ALLc", [16, 2, 256, D]); g.H2C = k.dram("H2C", [NCTX, D])
    g.PRSEND = k.dram("PRSEND", [TL, NEXP]); g.PRALL = k.dram("PRALL", [S, NEXP]); g.PRC = k.dram("PRC", [NCTX, NEXP])
    g.xs = [k.dram("xs%d" % e, [1152, D]) for e in range(8)]
    g.ys = [k.dram("ys%d" % e, [1152, D]) for e in range(8)]
    g.PART = k.dram("PART", [S, D]); g.PARTC = k.dram("PARTC", [NCTX, D])
    g.MOE = k.dram("MOE", [TL, D]); g.MOEC = k.dram("MOEC", [NCTX, D])
    xown1 = k.dram("xown1", [D, TL]); xall1 = k.dram("xall1", [16, 2, 64, TL]); xc1 = k.dram("xc1", [D, NCTX])
    g.modT = k.sbp([128, 48, 2])
    g.breg = {}
    for ns in (1152, 1024):
        r = k.nc.gpsimd.alloc_register("bchk%d" % ns)
        k.nc.gpsimd.reg_mov(r, ns - 1)
        g.breg[ns] = r
    for l in range(nlayer):
        L = Ls[l]
        ctx_out = l < nlayer - 1
        final = not ctx_out
        lam_init = 0.8 - 0.6 * math.exp(-0.3 * l)
        st_proj(k, g, L, ctx_out)
        if chk([("TM", g.TM), ("KT", g.KT_), ("KST", g.KST_), ("QT", g.QT_), ("QST", g.QST_), ("QCT", g.QCT), ("MQP", g.MQP),
                ("MKP", g.MKP), ("FT", g.FT_)]):
            return k
        st_attn(k, g, L, lam_init, ctx_out)
        if chk([("DA", g.DA), ("DAC", g.DAC)]):
            return k
        st_mlstm(k, g, L)
        if chk([("SEND", g.SEND), ("CMINE", g.CMINE)]):
            return k
        st_fourier(k, g, L, ctx_out)
        if chk([("SEND", g.SEND), ("CMINE", g.CMINE)]):
            return k
        for i in range(8):
            k.coll("AllGather", ALU.bypass, g.SEND, g.MG, RG, in_ap=g.SEND.a[i * 1024:(i + 1) * 1024, :],
                   out_ap=g.MG.a[i].rearrange("r t c -> (r t) c"))
        if ctx_out:
            k.coll("AllGather", ALU.bypass, g.CMINE, g.CG2, RG)
            g.CG.w = g.CG2.w
        if chk([("MG", g.MG), ("CG2", g.CG2), ("DA", g.DA), ("DAC", g.DAC)]):
            return k
        st_p3(k, g, L, ctx_out)
        if chk([("X1T", g.X1T), ("H2SEND", g.H2SEND), ("PRSEND", g.PRSEND), ("H2C", g.H2C), ("PRC", g.PRC)]):
            return k
        for i in range(16):
            k.coll("AllGather", ALU.bypass, g.H2SEND, g.H2ALL, RG, in_ap=g.H2SEND.a[i * 256:(i + 1) * 256, :],
                   out_ap=g.H2ALL.a[i].rearrange("r t c -> (r t) c"))
        k.coll("AllGather", ALU.bypass, g.PRSEND, g.PRALL, RG)
        if chk([("H2ALL", g.H2ALL), ("PRALL", g.PRALL)]):
            return k
        sfx = "_%d" % l
        L.w1c = k.inp("w1c" + sfx, [8, 16, 128, 8, 128]); L.w3c = k.inp("w3c" + sfx, [8, 16, 128, 8, 128])
        L.w2c = k.inp("w2c" + sfx, [8, 4, 128, 16, 256])
        st_moe(k, g, L, ctx_out)
        if chk([("PART", g.PART), ("PARTC", g.PARTC)]):
            return k
        k.coll("ReduceScatter", ALU.add, g.PART, g.MOE, RG)
        if ctx_out:
            k.coll("AllReduce", ALU.add, g.PARTC, g.MOEC, RG)
        if chk([("MOE", g.MOE), ("MOEC", g.MOEC)]):
            return k
        g.xown_next, g.xc_next = xown1, xc1
        st_p5(k, g, L, ctx_out, final)
        if not final:
            for i in range(16):
                k.coll("AllGather", ALU.bypass, xown1, xall1, RG, in_ap=xown1.a[i * 64:(i + 1) * 64, :],
                       out_ap=xall1.a[i].rearrange("r w t -> (r w) t"))
            g.xall = xall1
            g.xown, g.xc = xown1, xc1
            if chk([("xown1", xown1), ("xall1", xall1), ("xc1", xc1)]):
                return k
    return k


OFF_F, OFF_DQ, OFF_MO, OFF_MQ, OFF_MK, OFF_DK, OFF_DV, OFF_MV, OFF_G = 0, 256, 640, 1024, 1408, 1792, 2176, 2560, 2944


def _swap_perm():
    p = np.zeros(32, np.int64)
    for a in range(2):
        for pp in range(2):
            for j in range(8):
                p[a * 16 + pp * 8 + j] = a * 16 + (1 - pp) * 8 + j
    return np.concatenate([gi * 32 + p for gi in range(12)])


def chunk_groups(wm, groups):
    out = np.zeros((len(groups), 128, 8, 128), np.float32)
    for i, cols in enumerate(groups):
        blk = wm[:, cols]
        out[i, :, :, :len(cols)] = blk.reshape(8, 128, len(cols)).transpose(1, 0, 2)
    return out


def chunk_w(wm, nj):
    K_, n = wm.shape
    out = np.zeros((K_, nj * 128), np.float32)
    out[:, :n] = wm
    return np.ascontiguousarray(out.reshape(K_ // 128, 128, nj, 128).transpose(2, 1, 0, 3))


def vecT(v, nchunk):
    return np.ascontiguousarray(v.reshape(nchunk, 128).T)


def rope_tables(nrows_lat):
    t_row = np.repeat(np.arange(nrows_lat), 64).astype(np.float32)
    t_col = np.tile(np.arange(64), nrows_lat).astype(np.float32)
    inv = (np.float32(10000.0) ** (-np.arange(8, dtype=np.float32) / np.float32(8))).astype(np.float32)
    ar = t_row[:, None] * inv
    ac = t_col[:, None] * inv
    ang = np.concatenate([ar, ar, ac, ac], -1)
    cos = np.cos(ang).astype(np.float32)
    sin = np.sin(ang).astype(np.float32)
    sgn = np.tile(np.concatenate([-np.ones(8), np.ones(8)]), 2).astype(np.float32)
    ssin = sin * sgn
    cosk = np.concatenate([np.ones((NCTX, 32), np.float32), cos], 0).T
    sink = np.concatenate([np.zeros((NCTX, 32), np.float32), ssin], 0).T
    return (np.ascontiguousarray(np.tile(cosk, (4, 1))), np.ascontiguousarray(np.tile(sink, (4, 1))))


def mlstm_masks():
    j = np.arange(128)[:, None]
    s = np.arange(128)[None, :]
    UF = (j > s); TRF = (j <= s)
    MF = np.where(s < j, -30000.0, 0.0)
    UB = (j < s); TRB = (j >= s)
    MB = np.where(j < s, -30000.0, 0.0)
    return np.ascontiguousarray(np.stack([UF, TRF, MF, UB, TRB, MB], 1).astype(np.float32))


def fourier_tables():
    def cs(n, m, N):
        ang = 2.0 * np.pi * np.outer(np.arange(n), np.arange(m)) / N
        return np.cos(ang), np.sin(ang)
    c64, s64 = cs(64, 64, 64)
    z = np.zeros((64, 64))
    ccb = np.concatenate([np.block([[c64, z], [z, c64]]), np.block([[s64, z], [z, s64]])], 1)
    c128, s128 = cs(128, 128, 128)
    cs128 = np.concatenate([c128, s128, -s128, c128], 1)
    tc, ts = cs(64, 128, S)
    tw = np.concatenate([tc, ts], 1) / np.sqrt(64.0 * S)
    cs64 = np.concatenate([c64, -s64], 1)
    cn, sn = cs(NCTX, NCTX, NCTX)
    dft = np.concatenate([cn, -sn], 1) / np.sqrt(64.0 * NCTX)
    dftc = dft.reshape(2, 128, 512).transpose(1, 0, 2)
    f = lambda a: np.ascontiguousarray(a.astype(np.float32))
    return {"ccb": f(ccb), "cs128": f(cs128), "tw": f(tw), "cs64": f(cs64), "dftc": f(dftc)}


def host_inputs(inp):
    nlayer = inp["w_in"].shape[0]
    cosk, sink = rope_tables(S // 64)
    tabs = fourier_tables()
    sw = _swap_perm()
    ar = np.arange
    shared = {"cosk": cosk, "sink": sink, "msk": mlstm_masks(), "idn": np.eye(128, dtype=np.float32),
              "lst": np.ascontiguousarray(np.triu(np.ones((128, 128), np.float32), 1)),
              "fg": vecT(inp["final_g"], 8)}
    shared.update(tabs)
    for l in range(nlayer):
        sfx = "_%d" % l
        shared["adaw" + sfx] = np.ascontiguousarray(inp["ada_w"][l].reshape(8, 128, 12, 512).transpose(2, 1, 0, 3))
        shared["adab" + sfx] = vecT(inp["ada_b"][l], 48)
        shared["g1" + sfx] = vecT(inp["norm1_g"][l], 8)
        shared["wQ" + sfx] = chunk_groups(inp["w_in"][l], [OFF_DQ + ar(j * 128, (j + 1) * 128) for j in range(3)]
                                          + [OFF_DQ + sw[j * 128:(j + 1) * 128] for j in range(3)])
        shared["dlam" + sfx] = np.ascontiguousarray(inp["d_lam"][l].reshape(128))
        shared["dng" + sfx] = inp["d_norm_g"][l]
        shared["wo" + sfx] = chunk_w(inp["w_out"][l], 8)
        shared["g2" + sfx] = vecT(inp["norm2_g"][l], 8)
        shared["wr" + sfx] = np.ascontiguousarray(inp["router_w"][l].reshape(8, 128, 16).transpose(1, 0, 2))
    maps = []
    for c in range(NCORE):
        b, h = c // 2, c % 2
        m = dict(shared)
        m["xall"] = np.ascontiguousarray(inp["x"][b].reshape(2, TL, 16, 64).transpose(2, 0, 3, 1))
        m["xown"] = np.ascontiguousarray(inp["x"][b, h * TL:(h + 1) * TL].T)
        m["xc"] = np.ascontiguousarray(inp["ctx"][b].T)
        cv = np.stack([inp["c"][b], inp["c_ctx"]], axis=-1)
        m["cv"] = np.ascontiguousarray(cv.reshape(8, 128, 2).transpose(1, 0, 2))
        q0 = NCTX + h * TL
        m["cosq"] = np.ascontiguousarray(cosk[:, q0:q0 + TL]); m["sinq"] = np.ascontiguousarray(sink[:, q0:q0 + TL])
        m["hsel"] = np.full((128, 1), float(h), np.float32)
        heads = [2 * h, 2 * h + 1]
        es = slice(8 * h, 8 * h + 8)
        for l in range(nlayer):
            sfx = "_%d" % l
            w = inp["w_in"][l]
            groups = [OFF_F + h * 128 + ar(128)]
            groups += [OFF_DK + ar(j * 128, (j + 1) * 128) for j in range(3)]
            groups += [OFF_DK + sw[j * 128:(j + 1) * 128] for j in range(3)]
            groups += [OFF_MQ + hh * 96 + ar(96) for hh in heads]
            groups += [OFF_MK + hh * 96 + ar(96) for hh in heads]
            m["wAfm" + sfx] = chunk_groups(w, groups)
            gcols = [[hh, 4 + hh, 8 + hh, 12 + hh] for hh in heads]
            tmc = np.concatenate([OFF_DV + ar(384)] + [OFF_MV + hh * 96 + ar(96) for hh in heads]
                                 + [OFF_MO + hh * 96 + ar(96) for hh in heads] + [OFF_G + np.array(gc) for gc in gcols])
            m["wAtm" + sfx] = np.ascontiguousarray(w[:, tmc].reshape(8, 128, TMW).transpose(1, 0, 2))
            cwl, cbl = inp["m_conv_w"][l], inp["m_conv_b"][l]
            cw = np.zeros((2, 96, 8), np.float32)
            for i, hh in enumerate(heads):
                cw[i, :, 0:3] = cwl[:, hh * 96:(hh + 1) * 96].T
                cw[i, :, 3] = cbl[hh * 96:(hh + 1) * 96]
                cw[i, :, 4:7] = cwl[:, 384 + hh * 96:384 + (hh + 1) * 96].T
                cw[i, :, 7] = cbl[384 + hh * 96:384 + (hh + 1) * 96]
            m["cw" + sfx] = cw
            m["gb" + sfx] = np.ascontiguousarray(np.stack([inp["m_gate_b"][l][gc] for gc in gcols]).reshape(8))
            m["mng" + sfx] = np.ascontiguousarray(inp["m_norm_g"][l][heads[0] * 96:(heads[1] + 1) * 96])
            fw = inp["four_w"][l]
            fwblk = np.zeros((128, 128), np.float32)
            fwblk[0:64, 0:64] = fw[2 * h]
            fwblk[64:128, 64:128] = fw[2 * h + 1]
            m["fwblk" + sfx] = fwblk
            w1 = inp["exp_w1"][l][es]; w3 = inp["exp_w3"][l][es]; w2 = inp["exp_w2"][l][es]
            m["w1c" + sfx] = np.ascontiguousarray(w1.reshape(8, 8, 128, 16, 128).transpose(0, 3, 2, 1, 4))
            m["w3c" + sfx] = np.ascontiguousarray(w3.reshape(8, 8, 128, 16, 128).transpose(0, 3, 2, 1, 4))
            m["w2c" + sfx] = np.ascontiguousarray(w2.reshape(8, 16, 128, 4, 256).transpose(0, 3, 2, 1, 4))
        maps.append(m)
    return maps


def kernel(**inp):
    inp = {k_: np.asarray(v_) for k_, v_ in inp.items()}
    nlayer = inp["w_in"].shape[0]
    kb = build_fused(nlayer)
    res = run(kb, host_inputs(inp))
    out = np.zeros((B, S, D), np.float32)
    for c in range(NCORE):
        b, h = c // 2, c % 2
        out[b, h * TL:(h + 1) * TL] = res[c]["outT"].T
    return out
```

```python
import math
import numpy as np
import concourse.bass as bass
import concourse.mybir as mybir
from concourse.bass_utils import run_bass_kernel_spmd
from contextlib import ExitStack

F32 = mybir.dt.float32
I32 = mybir.dt.int32
U32 = mybir.dt.uint32
AF = mybir.ActivationFunctionType
ALU = mybir.AluOpType
AX = mybir.AxisListType

D = 1024
B = 4
S = 8192
NCTX = 256
NCORE = 8
TL = S // 2
EPS = 1e-6
PW = 2960
NEXP = 16

SEM_ROT = 12000
N_DMA_SEM = 32


class T:
    __slots__ = ("a", "w", "r", "name")

    def __init__(self, a, name=""):
        self.a = a
        self.w = {}
        self.r = {}
        self.name = name

    def __getitem__(self, idx):
        return self.a[idx]

    def sub(self, idx, name=""):
        return T(self.a[idx], name)


def _merge(d, stamp):
    k = id(stamp[0])
    if k not in d or d[k][1] < stamp[1]:
        d[k] = stamp


class KB:
    def __init__(self):
        self.nc = bass.Bass("TRN2", target_bir_lowering=False)
        nc = self.nc
        self.es = ExitStack()
        self.eng = {"pe": nc.tensor, "act": nc.scalar, "dve": nc.vector,
                    "pool": nc.gpsimd, "sp": nc.sync}
        self.cur = {}
        self.owner = {}
        self.waited = {e: {} for e in self.eng}
        self.nsem = 0
        self.dma_pool = []
        self.dma_rr = 0
        self.nname = 0
        self.out_stamps = []
        self.n_wait = 0
        self.n_op = 0
        self.rr = 0
        self.coll_stamps = []
        self.stk = self.es

    def new_sem(self):
        self.nsem += 1
        return self.es.enter_context(self.nc.semaphore("s%d" % self.nsem))

    def _nm(self, p):
        self.nname += 1
        return "%s%d" % (p, self.nname)

    def sb(self, shape, dt=F32, name=None):
        name = name or self._nm("sb")
        g = self.nc.sbuf_tensor(name, list(shape), dt)
        return T(self.stk.enter_context(g).ap(), name)

    def sbp(self, shape, dt=F32, name=None):
        name = name or self._nm("sbp")
        return T(self.nc.alloc_sbuf_tensor(name, list(shape), dt).ap(), name)

    def ps(self, shape, dt=F32, name=None):
        name = name or self._nm("ps")
        g = self.nc.psum_tensor(name, list(shape), dt)
        return T(self.stk.enter_context(g).ap(), name)

    def barrier(self):
        st = []
        for e, c in self.cur.items():
            st.append((c[0], c[1]))
        for ent in self.dma_pool:
            if ent[1] > 0:
                st.append((ent[0], ent[1]))
        st.extend(self.coll_stamps)
        for e in ("pe", "act", "dve", "pool", "sp"):
            self._wait(e, st)

    def stage_begin(self):
        self.stk = ExitStack()

    def stage_end(self):
        self.barrier()
        self.stk.close()

    def coll(self, kind, op, in_t, out_t, groups, in_ap=None, out_ap=None):
        self._wait("pool", self._deps([in_t], [out_t]))
        if not hasattr(self, "coll_pool"):
            self.coll_pool = [[self.new_sem(), 0] for _ in range(12)]
            self.coll_n = 0
        ent = self.coll_pool[self.coll_n % len(self.coll_pool)]
        self.coll_n += 1
        ia = in_t.a if in_ap is None else in_ap
        oa = out_t.a if out_ap is None else out_ap
        ins = self.nc.gpsimd.collective_compute(kind, op, replica_groups=groups, ins=[ia.opt()], outs=[oa.opt()])
        ent[1] += 1
        ins.then_inc(ent[0], 1)
        stamp = (ent[0], ent[1])
        self.coll_stamps = [st_ for st_ in self.coll_stamps if st_[0] is not ent[0]] + [stamp]
        _merge(in_t.r, stamp)
        _merge(out_t.w, stamp)

    def dram(self, name, shape, dt=F32, kind="Internal"):
        return T(self.nc.dram_tensor(name, list(shape), dt, kind=kind).ap(), name)

    def inp(self, name, shape, dt=F32):
        if not hasattr(self, "in_names"):
            self.in_names = set()
        self.in_names.add(name)
        return self.dram(name, shape, dt, kind="ExternalInput")

    def outp(self, name, shape, dt=F32):
        return self.dram(name, shape, dt, kind="ExternalOutput")

    def _stamp_new(self, e):
        c = self.cur.get(e)
        if c is None or c[1] >= SEM_ROT:
            c = [self.new_sem(), 0]
            self.owner[id(c[0])] = e
            self.cur[e] = c
        c[1] += 1
        return (c[0], c[1])

    def _wait(self, e, stamps):
        eng = self.eng[e]
        wd = self.waited[e]
        need = {}
        for (sem, val) in stamps:
            k = id(sem)
            if e == "pe" and self.owner.get(k) == "pe":
                continue
            if wd.get(k, 0) >= val:
                continue
            if k not in need or need[k][1] < val:
                need[k] = (sem, val)
        for k, (sem, val) in need.items():
            eng.wait_ge(sem, val)
            wd[k] = val
            self.n_wait += 1

    def _deps(self, reads, writes):
        st = []
        for t in reads:
            st.extend(t.w.values())
        for t in writes:
            st.extend(t.w.values())
            st.extend(t.r.values())
        return st

    def _record(self, stamp, reads, writes):
        for t in reads:
            _merge(t.r, stamp)
        for t in writes:
            t.w = {id(stamp[0]): stamp}
            t.r = {}

    def op(self, e, ins_fn, reads=(), writes=()):
        self._wait(e, self._deps(reads, writes))
        ins = ins_fn(self.eng[e])
        stamp = self._stamp_new(e)
        ins.then_inc(stamp[0], 1)
        self._record(stamp, reads, writes)
        self.n_op += 1
        return ins

    def dma(self, e, out_ap, in_ap, reads=(), writes=(), is_output=False, fn=None, acc=(), **kw):
        if len(self.dma_pool) < N_DMA_SEM:
            ent = [self.new_sem(), 0]
            self.dma_pool.append(ent)
        else:
            ent = self.dma_pool[self.dma_rr % len(self.dma_pool)]
            self.dma_rr += 1
        st = self._deps(reads, writes)
        if ent[1] > 0:
            st.append((ent[0], ent[1]))
        self._wait(e, st)
        eng = self.eng[e]
        if fn is None:
            ins = eng.dma_start(out=out_ap, in_=in_ap, **kw)
        else:
            ins = fn(eng)
        ent[1] += 16
        stamp = (ent[0], ent[1])
        ins.then_inc(ent[0], 16)
        self._record(stamp, reads, writes)
        for t in acc:
            _merge(t.w, stamp)
        if is_output:
            self.out_stamps.append(stamp)
        self.n_op += 1
        return ins

    def finish(self):
        self._wait("sp", self.out_stamps)
        return self.nc

    def evac(self, out_t, out_ap, in_t, in_ap):
        self.rr += 1
        if self.rr % 2:
            self.op("act", lambda e: e.copy(out=out_ap, in_=in_ap), reads=[in_t], writes=[out_t])
        else:
            self.op("dve", lambda e: e.tensor_copy(out=out_ap, in_=in_ap), reads=[in_t], writes=[out_t])


def run(kb, in_maps):
    nc = kb.finish()
    names = getattr(kb, "in_names", None)
    if names is not None:
        in_maps = [{n: m[n] for n in names} for m in in_maps]
    res = run_bass_kernel_spmd(nc, in_maps, core_ids=list(range(NCORE)))
    return res.results


RG = [[0, 1], [2, 3], [4, 5], [6, 7]]
NKALL = NCTX + S
NT = NKALL // 128
XPW = NKALL + 4
NFM = 11
TMW = 776
NBIS = 30


def mcol(t):
    return t * 128 + (2 if t >= 2 else 0)


def pcol(c):
    return c + 1 if c < NCTX else c + 3


class Ctx:
    pass


def st_proj(k, g, L, ctx_out):
    k.stage_begin()
    ones = k.sb([128, 128])
    k.op("dve", lambda e: e.memset(ones[:], 1.0), writes=[ones])
    epsb = k.sb([128, 1])
    k.op("dve", lambda e: e.memset(epsb[:], EPS), writes=[epsb])
    zer = k.sb([96, 2])
    k.op("dve", lambda e: e.memset(zer[:], 0.0), writes=[zer])
    for tp in (g.MQP, g.MKP):
        for hd in range(2):
            for c0 in (0, NCTX + 1, XPW - 1):
                n = 2 if c0 == NCTX + 1 else 1
                k.dma("sp", tp[hd, :, c0:c0 + n], zer[:, 0:n], reads=[zer], acc=[tp], allow_slow_non_contiguous=True)
    cvs = k.sb([128, 8, 2])
    k.dma("sp", cvs[:], g.cv[:], reads=[g.cv], writes=[cvs])
    scv = k.sb([128, 8, 2])
    k.op("act", lambda e: e.activation(out=scv[:], in_=cvs[:], func=AF.Silu), reads=[cvs], writes=[scv])
    adabs = k.sb([128, 48])
    k.dma("sp", adabs[:], L.adab[:], reads=[L.adab], writes=[adabs])
    g1s = k.sb([128, 8])
    k.dma("sp", g1s[:], L.g1[:], reads=[L.g1], writes=[g1s])
    modT = g.modT
    abuf = [k.sb([128, 8, 512]) for _ in range(2)]
    mps = [k.ps([128, 2]) for _ in range(2)]
    for gi in range(12):
        ab = abuf[gi % 2]
        k.dma("sp", ab[:], L.adaw[gi], reads=[L.adaw], writes=[ab])
        for jj in range(4):
            j = 4 * gi + jj
            mp = mps[j % 2]
            for kc in range(8):
                k.op("pe", lambda e: e.matmul(mp[:], ab[:, kc, jj * 128:(jj + 1) * 128], scv[:, kc, :],
                                              start=(kc == 0), stop=(kc == 7)), reads=[ab, scv], writes=[mp])
            k.op("dve", lambda e: e.tensor_scalar(out=modT[:, j, :], in0=mp[:], scalar1=adabs[:, j:j + 1], scalar2=None,
                                                  op0=ALU.add), reads=[mp, adabs], writes=[modT])
    a1 = k.sb([128, 8, 2])
    k.op("dve", lambda e: e.tensor_scalar(out=a1[:], in0=modT[:, 8:16, :], scalar1=1.0, scalar2=None, op0=ALU.add),
         reads=[modT], writes=[a1])
    k.op("dve", lambda e: e.tensor_tensor(out=a1[:], in0=a1[:], in1=g1s[:].unsqueeze(2).broadcast_to([128, 8, 2]),
                                          op=ALU.mult), reads=[a1, g1s], writes=[a1])
    wtm = k.sb([128, 8, TMW])
    k.dma("sp", wtm[:], L.wAtm[:], reads=[L.wAtm], writes=[wtm])
    xbuf = [k.sb([128, 8, 512]) for _ in range(2)]
    sq = k.sb([128, 8, 512])
    hT = [k.sb([128, 8, 512]) for _ in range(2)]
    ssp = k.ps([128, 512])
    rs = k.sb([128, 512])
    wbuf = [k.sb([128, 8, 128]) for _ in range(4)]
    pps = [k.ps([128, 512]) for _ in range(2)]
    obuf = [k.sb([128, 512]) for _ in range(2)]
    tps = [k.ps([128, 512]) for _ in range(2)]
    tob = [k.sb([128, TMW]) for _ in range(2)]
    cnt = {"c": 0, "w": 0, "t": 0}

    def norm_chunk(src_t, src_ap, n, col):
        ci = cnt["c"]
        cnt["c"] += 1
        xb, h = xbuf[ci % 2], hT[ci % 2]
        if isinstance(src_ap, tuple):
            xv_, t0_ = src_ap
            k.dma("sp", xb[0:64, :, :n], xv_[0, :, :, t0_:t0_ + n], reads=[src_t], writes=[xb])
            k.dma("sp", xb[64:128, :, :n], xv_[1, :, :, t0_:t0_ + n], reads=[src_t], acc=[xb])
        else:
            k.dma("sp", xb[:, :, :n], src_ap, reads=[src_t], writes=[xb])
        k.op("act", lambda e: e.activation(out=sq[:, :, :n], in_=xb[:, :, :n], func=AF.Square), reads=[xb], writes=[sq])
        for f in range(8):
            k.op("pe", lambda e: e.matmul(ssp[:, :n], ones[:], sq[:, f, :n], start=(f == 0), stop=(f == 7)),
                 reads=[ones, sq], writes=[ssp])
        k.op("act", lambda e: e.activation(out=rs[:, :n], in_=ssp[:, :n], func=AF.Sqrt, scale=1.0 / D, bias=epsb[:]),
             reads=[ssp, epsb], writes=[rs])
        k.op("dve", lambda e: e.reciprocal(out=rs[:, :n], in_=rs[:, :n]), reads=[rs], writes=[rs])
        for f in range(8):
            k.op("dve", lambda e: e.scalar_tensor_tensor(out=h[:, f, :n], in0=xb[:, f, :n], scalar=a1[:, f, col:col + 1],
                                                         in1=rs[:, :n], op0=ALU.mult, op1=ALU.mult),
                 reads=[xb, a1, rs], writes=[h])
            k.op("act", lambda e: e.activation(out=h[:, f, :n], in_=h[:, f, :n], func=AF.Identity,
                                               bias=modT[:, f, col:col + 1]), reads=[h, modT], writes=[h])
        return h

    def fm_group(h, n, wsrc, j, M, dst_t, dst_ap):
        i = cnt["w"] % 2
        wb = wbuf[cnt["w"] % 4]
        cnt["w"] += 1
        pp, ob = pps[i], obuf[i]
        k.dma("sp", wb[:], wsrc[j], reads=[wsrc], writes=[wb])
        for f in range(8):
            k.op("pe", lambda e: e.matmul(pp[:M, :n], wb[:, f, :M], h[:, f, :n], start=(f == 0), stop=(f == 7)),
                 reads=[wb, h], writes=[pp])
        k.evac(ob, ob[:M, :n], pp, pp[:M, :n])
        k.dma("pool", dst_ap, ob[:M, :n], reads=[ob], acc=[dst_t])

    xav = [g.xall.a[:, r, :, :].rearrange("(f ph) w t -> ph w f t", ph=2) for r in range(2)]
    xcv = g.xc.a.rearrange("(f p) t -> p f t", p=128)
    xov = g.xown.a.rearrange("(f p) t -> p f t", p=128)
    chunks = [(g.xc, xcv[:, :, :], NCTX, 1, 0)]
    for r in range(2):
        for i in range(TL // 512):
            chunks.append((g.xall, (xav[r], i * 512), 512, 0, NCTX + r * TL + i * 512))
    for (src_t, src_ap, n, col, c0) in chunks:
        h = norm_chunk(src_t, src_ap, n, col)
        fm_group(h, n, L.wAfm, 0, 128, g.FT_, g.FT_[:, c0:c0 + n])
        for j in range(3):
            fm_group(h, n, L.wAfm, 1 + j, 128, g.KT_, g.KT_[j * 128:(j + 1) * 128, c0:c0 + n])
            fm_group(h, n, L.wAfm, 4 + j, 128, g.KST_, g.KST_[j * 128:(j + 1) * 128, c0:c0 + n])
        p0 = pcol(c0)
        for hd in range(2):
            fm_group(h, n, L.wAfm, 7 + hd, 96, g.MQP, g.MQP[hd, :, p0:p0 + n])
            fm_group(h, n, L.wAfm, 9 + hd, 96, g.MKP, g.MKP[hd, :, p0:p0 + n])
        for sub in range(n // 128):
            i = cnt["t"] % 2
            cnt["t"] += 1
            tp, to = tps[i], tob[i]
            for f in range(8):
                k.op("pe", lambda e: e.matmul(tp[:, 0:512], h[:, f, sub * 128:(sub + 1) * 128], wtm[:, f, 0:512],
                                              start=(f == 0), stop=(f == 7)), reads=[h, wtm], writes=[tp])
            k.evac(to, to[:, 0:512], tp, tp[:, 0:512])
            for f in range(8):
                k.op("pe", lambda e: e.matmul(tp[:, 0:TMW - 512], h[:, f, sub * 128:(sub + 1) * 128], wtm[:, f, 512:TMW],
                                              start=(f == 0), stop=(f == 7)), reads=[h, wtm], writes=[tp])
            k.evac(to, to[:, 512:TMW], tp, tp[:, 0:TMW - 512])
            k.dma("pool", g.TM[c0 + sub * 128:c0 + (sub + 1) * 128, :], to[:], reads=[to], acc=[g.TM])
    qchunks = [(g.xown, xov[:, :, i * 512:(i + 1) * 512], 512, 0, i * 512, False) for i in range(TL // 512)]
    if ctx_out:
        qchunks.append((g.xc, xcv[:, :, :], NCTX, 1, 0, True))
    for (src_t, src_ap, n, col, c0, isc) in qchunks:
        h = norm_chunk(src_t, src_ap, n, col)
        for j in range(3):
            if isc:
                fm_group(h, n, L.wQ, j, 128, g.QCT, g.QCT[j * 128:(j + 1) * 128, 0:n])
            else:
                fm_group(h, n, L.wQ, j, 128, g.QT_, g.QT_[j * 128:(j + 1) * 128, c0:c0 + n])
                fm_group(h, n, L.wQ, 3 + j, 128, g.QST_, g.QST_[j * 128:(j + 1) * 128, c0:c0 + n])
    k.stage_end()


def st_attn(k, g, L, lam_init, ctx_out):
    k.stage_begin()
    NQ, NK = TL, NKALL
    NKT = NK // 128
    scale = 32 ** -0.5
    dl = k.sb([128, 128])
    k.dma("sp", dl[:], L.dlam.a.partition_broadcast(128), reads=[L.dlam], writes=[dl])
    pr = k.sb([128, 2, 32]); s2 = k.sb([128, 2]); lam = k.sb([128, 1]); nlam = k.sb([128, 1])
    dlv = dl.a.rearrange("p (a b c) -> p a b c", a=2, b=2)
    k.op("dve", lambda e: e.tensor_tensor(out=pr[:], in0=dlv[:, :, 0, :], in1=dlv[:, :, 1, :], op=ALU.mult),
         reads=[dl], writes=[pr])
    k.op("dve", lambda e: e.tensor_reduce(out=s2[:], in_=pr[:], op=ALU.add, axis=AX.X), reads=[pr], writes=[s2])
    k.op("act", lambda e: e.activation(out=s2[:], in_=s2[:], func=AF.Exp), reads=[s2], writes=[s2])
    k.op("dve", lambda e: e.tensor_tensor(out=lam[:], in0=s2[:, 0:1], in1=s2[:, 1:2], op=ALU.subtract),
         reads=[s2], writes=[lam])
    k.op("dve", lambda e: e.tensor_scalar(out=nlam[:], in0=lam[:], scalar1=-1.0, scalar2=-lam_init,
                                          op0=ALU.mult, op1=ALU.add), reads=[lam], writes=[nlam])
    gbc = k.sb([128, 64])
    k.dma("sp", gbc[:], L.dng.a.partition_broadcast(128), reads=[L.dng], writes=[gbc])
    k.op("dve", lambda e: e.tensor_scalar(out=gbc[:], in0=gbc[:], scalar1=1.0 - lam_init, scalar2=None, op0=ALU.mult),
         reads=[gbc], writes=[gbc])
    epsb = k.sb([128, 1])
    k.op("dve", lambda e: e.memset(epsb[:], EPS), writes=[epsb])
    kA = k.sb([96, NK]); kB = k.sb([96, NK])
    vaug = k.sb([128, NKT, 65])
    ta = [k.sb([64, 512]) for _ in range(2)]
    tb = [k.sb([64, 512]) for _ in range(2)]
    tcs = [k.sb([64, 512]) for _ in range(2)]
    tsn = [k.sb([64, 512]) for _ in range(2)]
    qAs = [k.sb([96, 512]) for _ in range(2)]
    qBs = [k.sb([96, 512]) for _ in range(2)]
    Sps = [[k.ps([128, 512]) for _ in range(3)] for _ in range(2)]
    Psb = [[k.sb([128, 512]) for _ in range(3)] for _ in range(2)]
    accp = [k.ps([128, 512]) for _ in range(2)]
    ot = [k.sb([128, 4, 64]) for _ in range(2)]
    o1 = k.sb([128, 4, 64]); o2 = k.sb([128, 4, 64]); osq = k.sb([128, 4, 64])
    r0 = k.sb([128, 4]); r1 = k.sb([128, 4]); ss = k.sb([128, 4])
    vv = g.TM.a[:, 0:384].rearrange("(t p) c -> p t c", p=128)
    dav = g.DA.a.rearrange("(s p) c -> p s c", p=128)
    state = {"i": 0, "q": 0, "o": 0}

    def rope(dst_t, dst_ap, src, ssrc, ct, st, r0_, c0, n):
        i = state["i"] % 2
        state["i"] += 1
        a, b_, c_, s_ = ta[i], tb[i], tcs[i], tsn[i]
        k.dma("sp", a[:, :n], src[r0_:r0_ + 64, c0:c0 + n], reads=[src], writes=[a])
        k.dma("sp", b_[:, :n], ssrc[r0_:r0_ + 64, c0:c0 + n], reads=[ssrc], writes=[b_])
        k.dma("sp", c_[:, :n], ct[0:64, c0:c0 + n], reads=[ct], writes=[c_])
        k.dma("sp", s_[:, :n], st[0:64, c0:c0 + n], reads=[st], writes=[s_])
        k.op("pool", lambda e: e.tensor_tensor(out=a[:, :n], in0=a[:, :n], in1=c_[:, :n], op=ALU.mult),
             reads=[a, c_], writes=[a])
        k.op("pool", lambda e: e.tensor_tensor(out=b_[:, :n], in0=b_[:, :n], in1=s_[:, :n], op=ALU.mult),
             reads=[b_, s_], writes=[b_])
        k.op("pool", lambda e: e.tensor_tensor(out=dst_ap, in0=a[:, :n], in1=b_[:, :n], op=ALU.add),
             reads=[a, b_], writes=[dst_t])

    def replicate(tA, tB, n):
        k.dma("sp", tA[64:96, :n], tA[0:32, :n], reads=[tA], acc=[tA])
        k.dma("sp", tB[0:32, :n], tA[32:64, :n], reads=[tA], writes=[tB])
        k.dma("sp", tB[32:64, :n], tA[0:32, :n], reads=[tA], acc=[tB])
        k.dma("sp", tB[64:96, :n], tA[32:64, :n], reads=[tA], acc=[tB])

    def attend(qA, qB, nq, nkt, out_dram_t, out_ap):
        nsub = nq // 128
        pairs = [(kt, m) for kt in range(nkt) for m in range(2)]
        triples = [pairs[i:i + 3] for i in range(0, len(pairs), 3)]

        def scores(ti):
            st_ = ti % 2
            tk, tq = (kA, qA) if st_ == 0 else (kB, qB)
            for j, (kt, m) in enumerate(triples[ti]):
                sp_, pb = Sps[st_][j], Psb[st_][j]
                k.op("pe", lambda e: e.matmul(sp_[:, :nq], tk[32 * j:32 * j + 32, kt * 128:(kt + 1) * 128],
                                              tq[32 * j:32 * j + 32, :nq], start=True, stop=True),
                     reads=[tk, tq], writes=[sp_])
            for j, (kt, m) in enumerate(triples[ti]):
                sp_, pb = Sps[st_][j], Psb[st_][j]
                k.op("act", lambda e: e.activation(out=pb[:, :nq], in_=sp_[:, :nq], func=AF.Exp, scale=scale),
                     reads=[sp_], writes=[pb])

        def avs(ti):
            st_ = ti % 2
            for j, (kt, m) in enumerate(triples[ti]):
                pb = Psb[st_][j]
                for sub in range(nsub):
                    k.op("pe", lambda e: e.matmul(accp[m][:, sub * 65:(sub + 1) * 65], pb[:, sub * 128:(sub + 1) * 128],
                                                  vaug[:, kt, :], start=(kt == 0 and sub == 0),
                                                  stop=(kt == nkt - 1), skip_group_check=True),
                         reads=[pb, vaug], writes=[accp[m]])
        scores(0)
        for ti in range(len(triples)):
            if ti + 1 < len(triples):
                scores(ti + 1)
            avs(ti)
        a0 = accp[0].a[:, 0:nsub * 65].rearrange("p (s c) -> p s c", c=65)
        a1 = accp[1].a[:, 0:nsub * 65].rearrange("p (s c) -> p s c", c=65)
        o = ot[state["o"] % 2]
        state["o"] += 1
        k.op("dve", lambda e: e.reciprocal(out=r0[:, :nsub], in_=a0[:, :, 64]), reads=[accp[0]], writes=[r0])
        k.op("dve", lambda e: e.reciprocal(out=r1[:, :nsub], in_=a1[:, :, 64]), reads=[accp[1]], writes=[r1])
        k.op("dve", lambda e: e.tensor_scalar(out=r1[:, :nsub], in0=r1[:, :nsub], scalar1=nlam[:], scalar2=None,
                                              op0=ALU.mult), reads=[r1, nlam], writes=[r1])
        k.op("dve", lambda e: e.tensor_tensor(out=o1[:, :nsub, :], in0=a0[:, :, 0:64],
                                              in1=r0[:, :nsub].unsqueeze(2).broadcast_to([128, nsub, 64]), op=ALU.mult),
             reads=[accp[0], r0], writes=[o1])
        k.op("dve", lambda e: e.tensor_tensor(out=o2[:, :nsub, :], in0=a1[:, :, 0:64],
                                              in1=r1[:, :nsub].unsqueeze(2).broadcast_to([128, nsub, 64]), op=ALU.mult),
             reads=[accp[1], r1], writes=[o2])
        k.op("dve", lambda e: e.tensor_tensor(out=o1[:, :nsub, :], in0=o1[:, :nsub, :], in1=o2[:, :nsub, :], op=ALU.add),
             reads=[o1, o2], writes=[o1])
        k.op("pool", lambda e: e.tensor_tensor(out=osq[:, :nsub, :], in0=o1[:, :nsub, :], in1=o1[:, :nsub, :], op=ALU.mult),
             reads=[o1], writes=[osq])
        k.op("dve", lambda e: e.tensor_reduce(out=ss[:, :nsub], in_=osq[:, :nsub, :], op=ALU.add, axis=AX.X),
             reads=[osq], writes=[ss])
        k.op("act", lambda e: e.activation(out=ss[:, :nsub], in_=ss[:, :nsub], func=AF.Sqrt, scale=1.0 / 64, bias=epsb[:]),
             reads=[ss, epsb], writes=[ss])
        k.op("dve", lambda e: e.reciprocal(out=ss[:, :nsub], in_=ss[:, :nsub]), reads=[ss], writes=[ss])
        k.op("dve", lambda e: e.tensor_tensor(out=o1[:, :nsub, :], in0=o1[:, :nsub, :],
                                              in1=ss[:, :nsub].unsqueeze(2).broadcast_to([128, nsub, 64]), op=ALU.mult),
             reads=[o1, ss], writes=[o1])
        k.op("dve", lambda e: e.tensor_tensor(out=o[:, :nsub, :], in0=o1[:, :nsub, :],
                                              in1=gbc[:].unsqueeze(1).broadcast_to([128, nsub, 64]), op=ALU.mult),
             reads=[o1, gbc], writes=[o])
        k.dma("pool", out_ap, o[:, :nsub, :], reads=[o], acc=[out_dram_t])

    for head in range(6):
        for c0 in range(0, NK, 512):
            n = min(512, NK - c0)
            rope(kA, kA[0:64, c0:c0 + n], g.KT_, g.KST_, g.cosk, g.sink, head * 64, c0, n)
        replicate(kA, kB, NK)
        k.op("dve", lambda e: e.memset(vaug[:], 1.0), writes=[vaug])
        for t0 in range(0, NKT, 11):
            t1 = min(NKT, t0 + 11)
            k.dma("sp", vaug[:, t0:t1, 0:64], vv[:, t0:t1, head * 64:(head + 1) * 64], reads=[g.TM], writes=[vaug])
        for qb in range(NQ // 512):
            qA, qB = qAs[state["q"] % 2], qBs[state["q"] % 2]
            state["q"] += 1
            rope(qA, qA[0:64, :512], g.QT_, g.QST_, g.cosq, g.sinq, head * 64, qb * 512, 512)
            replicate(qA, qB, 512)
            attend(qA, qB, 512, NKT, g.DA, dav[:, qb * 4:(qb + 1) * 4, head * 64:(head + 1) * 64])
        if ctx_out:
            qA, qB = qAs[state["q"] % 2], qBs[state["q"] % 2]
            state["q"] += 1
            k.dma("sp", qA[0:64, :NCTX], g.QCT[head * 64:(head + 1) * 64, :], reads=[g.QCT], writes=[qA])
            replicate(qA, qB, NCTX)
            attend(qA, qB, NCTX, NCTX // 128, g.DAC,
                   g.DAC.a.rearrange("(s p) c -> p s c", p=128)[:, :, head * 64:(head + 1) * 64])
    k.stage_end()


def st_mlstm(k, g, L):
    k.stage_begin()

    def load(t):
        s = k.sb(list(t.a.shape))
        k.dma("sp", s[:], t[:], reads=[t], writes=[s])
        return s
    msks = load(g.msk); ident = load(g.idn)
    ones = k.sb([128, 96])
    k.op("dve", lambda e: e.memset(ones[:], 1.0), writes=[ones])
    epsb = k.sb([128, 1])
    k.op("dve", lambda e: e.memset(epsb[:], EPS), writes=[epsb])
    gbs = k.sb([128, 8])
    k.dma("sp", gbs[:], L.gb.a.partition_broadcast(128), reads=[L.gb], writes=[gbs])
    mngs = k.sb([128, 192])
    k.dma("sp", mngs[:], L.mng.a.partition_broadcast(128), reads=[L.mng], writes=[mngs])
    W = NT * 128 + 2
    qc = k.sb([96, W]); kc = k.sb([96, W])
    xin = [k.sb([96, 2050]) for _ in range(2)]
    tmp = [k.sb([96, 2048]) for _ in range(2)]
    cws = k.sb([96, 8])
    vaug = k.sb([128, NT, 97])
    G = k.sb([128, NT, 4]); EB = k.sb([128, NT, 2]); DEC = k.sb([96, NT, 2])
    Hsb = k.sb([128, NT, 96])
    Cst = [k.sb([96, 97]) for _ in range(2)]
    LU = [k.sb([128, 128]) for _ in range(2)]
    DT = [k.sb([128, 128]) for _ in range(2)]
    PT = [k.sb([128, 128]) for _ in range(2)]
    isb = [k.sb([128, 97]) for _ in range(2)]
    nd = [k.sb([128, 97]) for _ in range(2)]
    kw = [k.sb([128, 96]) for _ in range(2)]
    den = [k.sb([128, 1]) for _ in range(2)]
    osb = [k.sb([128, 96]) for _ in range(2)]
    yt = [k.sb([128, 96]) for _ in range(2)]
    hsq = k.sb([128, 96])
    ssn = [k.sb([128, 1]) for _ in range(2)]
    Bps = [k.ps([128, 512]) for _ in range(2)]
    Sps = [k.ps([128, 128]) for _ in range(2)]
    bigps = Bps
    ips = k.ps([128, 97]); nps = k.ps([128, 97]); kps = k.ps([128, 96]); dps = k.ps([96, 97])
    mvv = g.TM.a[:, 384:576].rearrange("(t p) (h c) -> p t h c", p=128, h=2)
    mov = g.TM.a[:, 576:768].rearrange("(t p) (h c) -> p t h c", p=128, h=2)
    gtv = g.TM.a[:, 768:776].rearrange("(t p) (h c) -> p t h c", p=128, h=2)
    cnt = {"i": 0}

    for hd in range(2):
        k.dma("sp", cws[:], L.cw[hd], reads=[L.cw], writes=[cws])
        for (src, dst, o0, scl) in ((g.MQP, qc, 0, None), (g.MKP, kc, 4, 96 ** -0.5)):
            for c0 in range(0, W, 2048):
                n = min(2048, W - c0)
                i = cnt["i"] % 2
                cnt["i"] += 1
                xi, tm = xin[i], tmp[i]
                k.dma("sp", xi[:, :n + 2], src[hd, :, c0:c0 + n + 2], reads=[src], writes=[xi])
                k.op("dve", lambda e: e.tensor_scalar(out=tm[:, :n], in0=xi[:, 0:n], scalar1=cws[:, o0:o0 + 1],
                                                      scalar2=cws[:, o0 + 3:o0 + 4], op0=ALU.mult, op1=ALU.add),
                     reads=[xi, cws], writes=[tm])
                k.op("dve", lambda e: e.scalar_tensor_tensor(out=tm[:, :n], in0=xi[:, 1:n + 1], scalar=cws[:, o0 + 1:o0 + 2],
                                                             in1=tm[:, :n], op0=ALU.mult, op1=ALU.add),
                     reads=[xi, cws, tm], writes=[tm])
                k.op("dve", lambda e: e.scalar_tensor_tensor(out=tm[:, :n], in0=xi[:, 2:n + 2], scalar=cws[:, o0 + 2:o0 + 3],
                                                             in1=tm[:, :n], op0=ALU.mult, op1=ALU.add),
                     reads=[xi, cws, tm], writes=[tm])
                k.op("act", lambda e: e.activation(out=dst[:, c0:c0 + n], in_=tm[:, :n], func=AF.Silu),
                     reads=[tm], writes=[dst])
                if scl is not None:
                    k.op("pool", lambda e: e.tensor_scalar(out=dst[:, c0:c0 + n], in0=dst[:, c0:c0 + n], scalar1=scl,
                                                           scalar2=None, op0=ALU.mult), reads=[dst], writes=[dst])
        k.op("dve", lambda e: e.memset(vaug[:], 1.0), writes=[vaug])
        for t0 in range(0, NT, 11):
            k.dma("sp", vaug[:, t0:t0 + 11, 0:96], mvv[:, t0:t0 + 11, hd, :], reads=[g.TM], writes=[vaug])
        k.dma("sp", G[:], gtv[:, :, hd, :], reads=[g.TM], writes=[G])
        k.op("dve", lambda e: e.tensor_tensor(out=G[:], in0=G[:], in1=gbs[:, hd * 4:(hd + 1) * 4].unsqueeze(1).broadcast_to([128, NT, 4]),
                                              op=ALU.add), reads=[G, gbs], writes=[G])
        Gd = G.a.rearrange("p t (d y) -> p t d y", d=2)
        k.op("act", lambda e: e.activation(out=Gd[:, :, :, 1], in_=Gd[:, :, :, 1], func=AF.Exp, scale=-1.0), reads=[G], writes=[G])
        k.op("act", lambda e: e.activation(out=Gd[:, :, :, 1], in_=Gd[:, :, :, 1], func=AF.Ln, bias=1.0), reads=[G], writes=[G])
        k.op("dve", lambda e: e.tensor_scalar(out=Gd[:, :, :, 1], in0=Gd[:, :, :, 1], scalar1=-1.0, scalar2=None, op0=ALU.mult),
             reads=[G], writes=[G])
        G2 = G.a.rearrange("p t c -> p (t c)")
        for dr in range(2):
            k.op("pe", lambda e: e.matmul(bigps[dr][:, 0:NT * 4], msks[:, 1 + 3 * dr, :], G2, start=True, stop=True),
                 reads=[msks, G], writes=[bigps[dr]])
            bv = bigps[dr].a[:, 0:NT * 4].rearrange("p (t c) -> p t c", c=4)
            k.op("act", lambda e: e.activation(out=EB[:, :, dr], in_=bv[:, :, 2 * dr + 1], func=AF.Exp),
                 reads=[bigps[dr]], writes=[EB])
        k.op("pe", lambda e: e.matmul(bigps[0][0:96, 0:NT * 4], ones[:], G2, start=True, stop=True),
             reads=[ones, G, EB], writes=[bigps[0]])
        bv = bigps[0].a[0:96, 0:NT * 4].rearrange("p (t d y) -> p t d y", d=2, y=2)
        k.op("act", lambda e: e.activation(out=DEC[:], in_=bv[:, :, :, 1], func=AF.Exp), reads=[bigps[0]], writes=[DEC])

        for dr in range(2):
            order = list(range(NT)) if dr == 0 else [1, 0] + list(range(NT - 1, 1, -1))
            U, TR, MK = msks.a[:, 3 * dr, :], msks.a[:, 3 * dr + 1, :], msks.a[:, 3 * dr + 2, :]
            last = 127 if dr == 0 else 0
            cs = Cst[0]
            k.op("dve", lambda e: e.memset(cs[:], 0.0), writes=[cs])
            for si, t in enumerate(order):
                b2 = si % 2
                c0 = mcol(t)
                lf = G.a[:, t, 2 * dr + 1:2 * dr + 2]
                li = G.a[:, t, 2 * dr:2 * dr + 1]
                lu, dt_, pt, bp, sp_ = LU[b2], DT[b2], PT[b2], Bps[b2], Sps[b2]
                k.op("pool", lambda e: e.tensor_scalar(out=lu[:], in0=U, scalar1=lf, scalar2=None, op0=ALU.mult),
                     reads=[msks, G], writes=[lu])
                k.op("pe", lambda e: e.matmul(bp[:, 0:128], lu[:], TR, start=True, stop=False), reads=[lu, msks], writes=[bp])
                k.op("pe", lambda e: e.matmul(bp[:, 0:128], ident[:], MK, start=False, stop=True), reads=[ident, msks], writes=[bp])
                k.op("act", lambda e: e.activation(out=dt_[:], in_=bp[:, 0:128], func=AF.Exp, bias=li), reads=[bp, G], writes=[dt_])
                k.op("pe", lambda e: e.matmul(sp_[:], kc[:, c0:c0 + 128], qc[:, c0:c0 + 128], start=True, stop=True),
                     reads=[kc, qc], writes=[sp_])
                k.op("dve", lambda e: e.tensor_tensor(out=pt[:], in0=sp_[:], in1=dt_[:], op=ALU.mult),
                     reads=[sp_, dt_], writes=[pt])
                k.op("pe", lambda e: e.matmul(ips[:], pt[:], vaug[:, t, :], start=True, stop=True), reads=[pt, vaug], writes=[ips])
                k.op("pe", lambda e: e.matmul(nps[:], qc[:, c0:c0 + 128], cs[:], start=True, stop=True),
                     reads=[qc, cs], writes=[nps])
                ib, ndb, dn = isb[b2], nd[b2], den[b2]
                k.op("dve", lambda e: e.tensor_copy(out=ib[:], in_=ips[:]), reads=[ips], writes=[ib])
                k.op("dve", lambda e: e.scalar_tensor_tensor(out=ndb[:], in0=nps[:], scalar=EB[:, t, dr:dr + 1], in1=ib[:],
                                                             op0=ALU.mult, op1=ALU.add), reads=[nps, EB, ib], writes=[ndb])
                k.op("dve", lambda e: e.tensor_scalar(out=dn[:], in0=ndb[:, 96:97], scalar1=1.0, scalar2=None, op0=ALU.max),
                     reads=[ndb], writes=[dn])
                k.op("dve", lambda e: e.scalar_tensor_tensor(out=dn[:], in0=ndb[:, 96:97], scalar=-1.0, in1=dn[:],
                                                             op0=ALU.mult, op1=ALU.max), reads=[ndb, dn], writes=[dn])
                k.op("dve", lambda e: e.reciprocal(out=dn[:], in_=dn[:]), reads=[dn], writes=[dn])
                if dr == 0:
                    k.op("dve", lambda e: e.tensor_scalar(out=Hsb[:, t, :], in0=ndb[:, 0:96], scalar1=dn[:], scalar2=None,
                                                          op0=ALU.mult), reads=[ndb, dn], writes=[Hsb])
                else:
                    y, ob_, s1 = yt[b2], osb[b2], ssn[b2]
                    k.dma("sp", ob_[:], mov[:, t, hd, :], reads=[g.TM], writes=[ob_])
                    k.op("dve", lambda e: e.scalar_tensor_tensor(out=y[:], in0=ndb[:, 0:96], scalar=dn[:], in1=Hsb[:, t, :],
                                                                 op0=ALU.mult, op1=ALU.add), reads=[ndb, dn, Hsb], writes=[y])
                    k.op("dve", lambda e: e.tensor_tensor(out=hsq[:], in0=y[:], in1=y[:], op=ALU.mult), reads=[y], writes=[hsq])
                    k.op("dve", lambda e: e.tensor_reduce(out=s1[:], in_=hsq[:], op=ALU.add, axis=AX.X), reads=[hsq], writes=[s1])
                    k.op("act", lambda e: e.activation(out=s1[:], in_=s1[:], func=AF.Ln, scale=1.0 / 96, bias=epsb[:]),
                         reads=[s1, epsb], writes=[s1])
                    k.op("act", lambda e: e.activation(out=s1[:], in_=s1[:], func=AF.Exp, scale=-0.5), reads=[s1], writes=[s1])
                    k.op("dve", lambda e: e.scalar_tensor_tensor(out=y[:], in0=y[:], scalar=s1[:], in1=mngs[:, hd * 96:(hd + 1) * 96],
                                                                 op0=ALU.mult, op1=ALU.mult), reads=[y, s1, mngs], writes=[y])
                    k.op("act", lambda e: e.activation(out=ob_[:], in_=ob_[:], func=AF.Exp, scale=-1.0), reads=[ob_], writes=[ob_])
                    k.op("pool", lambda e: e.tensor_scalar(out=ob_[:], in0=ob_[:], scalar1=1.0, scalar2=None, op0=ALU.add),
                         reads=[ob_], writes=[ob_])
                    k.op("dve", lambda e: e.reciprocal(out=ob_[:], in_=ob_[:]), reads=[ob_], writes=[ob_])
                    k.op("dve", lambda e: e.tensor_tensor(out=y[:], in0=y[:], in1=ob_[:], op=ALU.mult), reads=[y, ob_], writes=[y])
                    cc = 128 + hd * 96
                    if t < 2:
                        k.dma("pool", g.CMINE[t * 128:(t + 1) * 128, cc:cc + 96], y[:], reads=[y], acc=[g.CMINE])
                    else:
                        k.dma("pool", g.SEND[(t - 2) * 128:(t - 1) * 128, cc:cc + 96], y[:], reads=[y], acc=[g.SEND])
                kwb = kw[b2]
                k.op("pe", lambda e: e.transpose(kps[:], kc[:, c0:c0 + 128], ident[0:96, 0:96]), reads=[kc, ident], writes=[kps])
                k.op("dve", lambda e: e.tensor_scalar(out=kwb[:], in0=kps[:], scalar1=dt_[:, last:last + 1], scalar2=None,
                                                      op0=ALU.mult), reads=[kps, dt_], writes=[kwb])
                k.op("pe", lambda e: e.matmul(dps[:], kwb[:], vaug[:, t, :], start=True, stop=True), reads=[kwb, vaug], writes=[dps])
                cn = Cst[(si + 1) % 2]
                k.op("dve", lambda e: e.scalar_tensor_tensor(out=cn[:], in0=cs[:], scalar=DEC[:, t, dr:dr + 1], in1=dps[:],
                                                             op0=ALU.mult, op1=ALU.add), reads=[cs, DEC, dps], writes=[cn])
                cs = cn
    k.stage_end()


def st_fourier(k, g, L, ctx_out):
    k.stage_begin()

    def load(t):
        s = k.sb(list(t.a.shape))
        k.dma("sp", s[:], t[:], reads=[t], writes=[s])
        return s
    fws = load(L.fwblk); ccbs = load(g.ccb); cs128s = load(g.cs128); tws = load(g.tw); cs64s = load(g.cs64)
    AB = k.sb([128, 256])
    yps = [k.ps([128, 512]) for _ in range(2)]
    pab = yps[0]
    for i in range(2):
        k.op("pe", lambda e: e.matmul(pab[:, i * 128:(i + 1) * 128], ccbs[:, i * 128:(i + 1) * 128], fws[:],
                                      start=True, stop=True), reads=[ccbs, fws], writes=[pab])
    k.op("dve", lambda e: e.tensor_copy(out=AB[:], in_=pab[:, 0:256]), reads=[pab], writes=[AB])
    xs = k.sb([128, S])
    for i in range(4):
        k.dma("sp", xs[:, i * 2048:(i + 1) * 2048], g.FT_[:, NCTX + i * 2048:NCTX + (i + 1) * 2048], reads=[g.FT_], writes=[xs])
    Y = k.sb([128, 64, 256])
    xv = xs.a.rearrange("p (n2 n1) -> p n1 n2", n1=64)
    for i in range(32):
        yp = yps[i % 2]
        for j in range(2):
            n1 = 2 * i + j
            k.op("pe", lambda e: e.matmul(yp[:, j * 256:(j + 1) * 256], xv[:, n1, :], AB[:], start=True, stop=True),
                 reads=[xs, AB], writes=[yp])
        k.evac(Y, Y[:, 2 * i:2 * i + 2, :].rearrange("p a b -> p (a b)"), yp, yp[:])
    zps = [k.ps([64, 512]) for _ in range(4)]
    zr = [k.sb([64, 4, 128]) for _ in range(2)]
    zi = [k.sb([64, 4, 128]) for _ in range(2)]
    t1 = k.sb([64, 2, 128]); t2 = k.sb([64, 2, 128])
    ops_ = [k.ps([128, 256]) for _ in range(2)]
    FTM = k.sb([128, 64, 128])
    tcb = tws.a[:, 0:128].unsqueeze(1).broadcast_to([64, 2, 128])
    tsb = tws.a[:, 128:256].unsqueeze(1).broadcast_to([64, 2, 128])
    for blk in range(32):
        zrb, zib = zr[blk % 2], zi[blk % 2]
        for hb in range(2):
            zp = zps[(2 * blk + hb) % 4]
            for j in range(2):
                d = blk * 4 + hb * 2 + j
                k.op("pe", lambda e: e.matmul(zp[:, j * 256:(j + 1) * 256], Y[:, :, d], cs128s[:, 0:256],
                                              start=True, stop=False), reads=[Y, cs128s], writes=[zp])
                k.op("pe", lambda e: e.matmul(zp[:, j * 256:(j + 1) * 256], Y[:, :, 128 + d], cs128s[:, 256:512],
                                              start=False, stop=True), reads=[Y, cs128s], writes=[zp])
            zv = zp.a.rearrange("p (d c m) -> p d c m", d=2, c=2)
            sl = slice(hb * 2, hb * 2 + 2)
            k.op("dve", lambda e: e.tensor_tensor(out=t1[:], in0=zv[:, :, 0, :], in1=tcb, op=ALU.mult),
                 reads=[zp, tws], writes=[t1])
            k.op("dve", lambda e: e.tensor_tensor(out=t2[:], in0=zv[:, :, 1, :], in1=tsb, op=ALU.mult),
                 reads=[zp, tws], writes=[t2])
            k.op("pool", lambda e: e.tensor_tensor(out=zrb[:, sl, :], in0=t1[:], in1=t2[:], op=ALU.subtract),
                 reads=[t1, t2], writes=[zrb])
            k.op("dve", lambda e: e.tensor_tensor(out=t1[:], in0=zv[:, :, 0, :], in1=tsb, op=ALU.mult),
                 reads=[zp, tws], writes=[t1])
            k.op("dve", lambda e: e.tensor_tensor(out=t2[:], in0=zv[:, :, 1, :], in1=tcb, op=ALU.mult),
                 reads=[zp, tws], writes=[t2])
            k.op("pool", lambda e: e.tensor_tensor(out=zib[:, sl, :], in0=t1[:], in1=t2[:], op=ALU.add),
                 reads=[t1, t2], writes=[zib])
        op_ = ops_[blk % 2]
        for j in range(4):
            k.op("pe", lambda e: e.matmul(op_[:, j * 64:(j + 1) * 64], zrb[:, j, :], cs64s[:, 0:64], start=True, stop=False),
                 reads=[cs64s, zrb], writes=[op_])
            k.op("pe", lambda e: e.matmul(op_[:, j * 64:(j + 1) * 64], zib[:, j, :], cs64s[:, 64:128], start=False, stop=True),
                 reads=[cs64s, zib], writes=[op_])
        k.evac(FTM, FTM[:, :, blk * 4:(blk + 1) * 4], op_, op_.a.rearrange("p (d m) -> p m d", d=4))
    sv = g.SEND.a[:, 0:128].rearrange("(m1 m2) d -> m2 m1 d", m2=128)
    for q4 in range(4):
        k.dma("pool", sv[:, q4 * 16:(q4 + 1) * 16, :], FTM[:, q4 * 16:(q4 + 1) * 16, :], reads=[FTM], acc=[g.SEND])
    if ctx_out:
        xcs = k.sb([128, NCTX])
        k.dma("sp", xcs[:], g.FT_[:, 0:NCTX], reads=[g.FT_], writes=[xcs])
        dfs = load(g.dftc)
        Yc = k.sb([128, 2, 256])
        for t in range(2):
            yp = yps[t % 2]
            k.op("pe", lambda e: e.matmul(yp[:, 0:256], xcs[:, t * 128:(t + 1) * 128], AB[:], start=True, stop=True),
                 reads=[xcs, AB], writes=[yp])
            k.evac(Yc, Yc[:, t, :], yp, yp[:, 0:256])
        for mt in range(2):
            pv = yps[mt % 2]
            i = 0
            for t in range(2):
                for c in range(2):
                    k.op("pe", lambda e: e.matmul(pv[:, 0:128], dfs[:, t, c * 256 + mt * 128:c * 256 + (mt + 1) * 128],
                                                  Yc[:, t, c * 128:(c + 1) * 128], start=(i == 0), stop=(i == 3)),
                         reads=[dfs, Yc], writes=[pv])
                    i += 1
            oc = k.sb([128, 128])
            k.evac(oc, oc[:], pv, pv[:, 0:128])
            k.dma("pool", g.CMINE[mt * 128:(mt + 1) * 128, 0:128], oc[:], reads=[oc], acc=[g.CMINE])
    k.stage_end()


def st_p3(k, g, L, ctx_out):
    k.stage_begin()

    def load(t):
        s = k.sb(list(t.a.shape))
        k.dma("sp", s[:], t[:], reads=[t], writes=[s])
        return s
    modT = g.modT
    g2s = load(L.g2); wrs = load(L.wr); ident = load(g.idn); hs = load(g.hsel)
    wos = k.sb([128, 8, 8, 128])
    for j in range(8):
        k.dma("sp", wos[:, j], L.wo[j], reads=[L.wo], writes=[wos])
    ones = k.sb([128, 128])
    k.op("dve", lambda e: e.memset(ones[:], 1.0), writes=[ones])
    epsb = k.sb([128, 1])
    k.op("dve", lambda e: e.memset(epsb[:], EPS), writes=[epsb])
    a2 = k.sb([128, 8, 2])
    k.op("dve", lambda e: e.tensor_scalar(out=a2[:], in0=modT[:, 32:40, :], scalar1=1.0, scalar2=None, op0=ALU.add),
         reads=[modT], writes=[a2])
    k.op("dve", lambda e: e.tensor_tensor(out=a2[:], in0=a2[:], in1=g2s[:].unsqueeze(2).broadcast_to([128, 8, 2]),
                                          op=ALU.mult), reads=[a2, g2s], writes=[a2])
    chunks = [(i * 512, 512, 0) for i in range(TL // 512)]
    if ctx_out:
        chunks.append((TL, NCTX, 1))
    mb = k.sb([128, 8, 512])
    xb = [k.sb([128, 8, 512]) for _ in range(2)]
    x1 = k.sb([128, 8, 512])
    h2 = k.sb([128, 8, 512])
    sq = k.sb([128, 8, 512])
    TA = [k.sb([128, 2, 320]) for _ in range(2)]
    TB = [k.sb([128, 2, 320]) for _ in range(2)]
    MT = [k.sb([128, D]) for _ in range(2)]
    H2T = [k.sb([128, D]) for _ in range(2)]
    pps = [k.ps([128, 512]) for _ in range(2)]
    tps = [k.ps([128, 512]) for _ in range(2)]
    ssp = k.ps([128, 512])
    rs = k.sb([128, 512])
    lps = [k.ps([128, 16]) for _ in range(2)]
    mx = k.sb([128, 1]); sm = k.sb([128, 1]); ex = k.sb([128, 16])
    pb = [k.sb([128, 16]) for _ in range(2)]
    xov = g.xown.a.rearrange("(f p) t -> p f t", p=128)
    xcv = g.xc.a.rearrange("(f p) t -> p f t", p=128)
    x1v = g.X1T.a.rearrange("(f p) t -> p f t", p=128)
    cnt = {"s": 0, "p": 0, "t": 0}
    pieces = [(0, 0, 128, 0), (1, 0, 128, 128), (0, 128, 320, 640), (1, 128, 320, 832)]
    for ci, (t0, n, col) in enumerate(chunks):
        x_ = xb[ci % 2]
        k.dma("sp", x_[:, :, :n], (xov[:, :, t0:t0 + n] if col == 0 else xcv[:, :, :]), reads=[g.xown if col == 0 else g.xc],
              writes=[x_])
        for sub in range(n // 128):
            i = cnt["s"] % 2
            cnt["s"] += 1
            ta, tb, mt = TA[i], TB[i], MT[i]
            r0 = t0 + sub * 128
            if col == 0:
                for r in range(2):
                    ra, rb = r0, TL + r0
                    k.dma("sp", ta[:, r, :], g.MG[ra // 1024, r, ra % 1024:ra % 1024 + 128, :], reads=[g.MG], writes=[ta] if r == 0 else [], acc=[] if r == 0 else [ta])
                    k.dma("sp", tb[:, r, :], g.MG[rb // 1024, r, rb % 1024:rb % 1024 + 128, :], reads=[g.MG], writes=[tb] if r == 0 else [], acc=[] if r == 0 else [tb])
                k.dma("sp", mt[:, 256:640], g.DA[r0:r0 + 128, :], reads=[g.DA], writes=[mt])
                k.op("pool", lambda e: e.tensor_tensor(out=tb[:], in0=tb[:], in1=ta[:], op=ALU.subtract), reads=[ta, tb], writes=[tb])
                for (r, c0, c1, dc) in pieces:
                    k.op("dve", lambda e: e.scalar_tensor_tensor(out=mt[:, dc:dc + c1 - c0], in0=tb[:, r, c0:c1], scalar=hs[:],
                                                                 in1=ta[:, r, c0:c1], op0=ALU.mult, op1=ALU.add),
                         reads=[tb, hs, ta], writes=[mt])
            else:
                rr = sub * 128
                for (r, c0, c1, dc) in pieces:
                    k.dma("sp", mt[:, dc:dc + c1 - c0], g.CG[r, rr:rr + 128, c0:c1], reads=[g.CG], writes=[mt])
                k.dma("sp", mt[:, 256:640], g.DAC[rr:rr + 128, :], reads=[g.DAC], writes=[mt])
            for hf in range(2):
                tp = tps[cnt["t"] % 2]
                cnt["t"] += 1
                for j in range(4):
                    c = hf * 4 + j
                    k.op("pe", lambda e: e.transpose(tp[:, j * 128:(j + 1) * 128], mt[:, c * 128:(c + 1) * 128], ident[:]),
                         reads=[mt, ident], writes=[tp])
                k.evac(mb, mb[:, hf * 4:hf * 4 + 4, sub * 128:(sub + 1) * 128], tp, tp[:].rearrange("p (a b) -> p a b", a=4))
        for j in range(8):
            pp = pps[cnt["p"] % 2]
            cnt["p"] += 1
            for f in range(8):
                k.op("pe", lambda e: e.matmul(pp[:, :n], wos[:, j, f, :], mb[:, f, :n], start=(f == 0), stop=(f == 7)),
                     reads=[wos, mb], writes=[pp])
            k.op("dve", lambda e: e.scalar_tensor_tensor(out=x1[:, j, :n], in0=pp[:, :n], scalar=modT[:, 16 + j, col:col + 1],
                                                         in1=x_[:, j, :n], op0=ALU.mult, op1=ALU.add),
                 reads=[pp, modT, x_], writes=[x1])
        k.dma("pool", x1v[:, :, t0:t0 + n], x1[:, :, :n], reads=[x1], acc=[g.X1T])
        k.op("act", lambda e: e.activation(out=sq[:, :, :n], in_=x1[:, :, :n], func=AF.Square), reads=[x1], writes=[sq])
        for f in range(8):
            k.op("pe", lambda e: e.matmul(ssp[:, :n], ones[:], sq[:, f, :n], start=(f == 0), stop=(f == 7)),
                 reads=[ones, sq], writes=[ssp])
        k.op("act", lambda e: e.activation(out=rs[:, :n], in_=ssp[:, :n], func=AF.Sqrt, scale=1.0 / D, bias=epsb[:]),
             reads=[ssp, epsb], writes=[rs])
        k.op("dve", lambda e: e.reciprocal(out=rs[:, :n], in_=rs[:, :n]), reads=[rs], writes=[rs])
        for f in range(8):
            k.op("dve", lambda e: e.scalar_tensor_tensor(out=h2[:, f, :n], in0=x1[:, f, :n], scalar=a2[:, f, col:col + 1],
                                                         in1=rs[:, :n], op0=ALU.mult, op1=ALU.mult),
                 reads=[x1, a2, rs], writes=[h2])
            k.op("act", lambda e: e.activation(out=h2[:, f, :n], in_=h2[:, f, :n], func=AF.Identity,
                                               bias=modT[:, 24 + f, col:col + 1]), reads=[h2, modT], writes=[h2])
        for st in range(n // 128):
            ht = H2T[st % 2]
            for hf in range(2):
                tp = tps[cnt["t"] % 2]
                cnt["t"] += 1
                for j in range(4):
                    f = hf * 4 + j
                    k.op("pe", lambda e: e.transpose(tp[:, j * 128:(j + 1) * 128], h2[:, f, st * 128:(st + 1) * 128], ident[:]),
                         reads=[h2, ident], writes=[tp])
                k.evac(ht, ht[:, hf * 512:(hf + 1) * 512], tp, tp[:])
            if col == 0:
                k.dma("pool", g.H2SEND[t0 + st * 128:t0 + (st + 1) * 128, :], ht[:], reads=[ht], acc=[g.H2SEND])
            else:
                k.dma("pool", g.H2C[st * 128:(st + 1) * 128, :], ht[:], reads=[ht], acc=[g.H2C])
            lp = lps[st % 2]
            p_ = pb[st % 2]
            for f in range(8):
                k.op("pe", lambda e: e.matmul(lp[:], h2[:, f, st * 128:(st + 1) * 128], wrs[:, f, :], start=(f == 0), stop=(f == 7)),
                     reads=[h2, wrs], writes=[lp])
            k.op("dve", lambda e: e.tensor_reduce(out=mx[:], in_=lp[:], op=ALU.max, axis=AX.X, negate=True),
                 reads=[lp], writes=[mx])
            k.op("act", lambda e: e.activation(out=ex[:], in_=lp[:], func=AF.Exp, bias=mx[:], accum_out=sm[:]),
                 reads=[lp, mx], writes=[ex, sm])
            k.op("dve", lambda e: e.reciprocal(out=sm[:], in_=sm[:]), reads=[sm], writes=[sm])
            k.op("dve", lambda e: e.tensor_scalar(out=p_[:], in0=ex[:], scalar1=sm[:], scalar2=None, op0=ALU.mult),
                 reads=[ex, sm], writes=[p_])
            if col == 0:
                k.dma("pool", g.PRSEND[t0 + st * 128:t0 + (st + 1) * 128, :], p_[:], reads=[p_], acc=[g.PRSEND])
            else:
                k.dma("pool", g.PRC[st * 128:(st + 1) * 128, :], p_[:], reads=[p_], acc=[g.PRC])
    k.stage_end()


def st_moe(k, g, L, ctx_out):
    k.stage_begin()
    NL, NC_ = S, (NCTX if ctx_out else 0)
    capL, capC = 2 * NL // NEXP, 2 * NC_ // NEXP
    NSLOT = capL + (128 if NC_ else 0)
    NST = NSLOT // 128
    xs, ys = g.xs, g.ys

    def load(t):
        s = k.sb(list(t.a.shape))
        k.dma("sp", s[:], t[:], reads=[t], writes=[s])
        return s
    lsts = load(g.lst); ident = load(g.idn); hs = load(g.hsel)
    ones = k.sb([128, 128])
    k.op("dve", lambda e: e.memset(ones[:], 1.0), writes=[ones])
    banks = [k.ps([128, 512]) for _ in range(8)]
    breg = g.breg[NSLOT]

    def route(prt, N, cap, base):
        ntl = N // 128
        pr16 = k.sb([128, ntl, 16])
        prs = k.sb([128, ntl, 8]); cmp_ = k.sb([128, ntl, 8]); mask = k.sb([128, ntl, 8]); gate = k.sb([128, ntl, 8])
        off = k.sb([128, ntl, 8]); idxf = k.sb([128, ntl, 8]); idx = k.sb([128, ntl, 8], I32)
        lo = k.sb([128, 8]); mid = k.sb([128, 8]); cpart = k.sb([128, 8]); sel = k.sb([128, 8])
        k.dma("sp", pr16[:], prt.a.rearrange("(t p) e -> p t e", p=128), reads=[prt], writes=[pr16])
        k.op("dve", lambda e: e.tensor_tensor(out=cmp_[:], in0=pr16[:, :, 8:16], in1=pr16[:, :, 0:8], op=ALU.subtract),
             reads=[pr16], writes=[cmp_])
        k.op("dve", lambda e: e.scalar_tensor_tensor(out=prs[:], in0=cmp_[:], scalar=hs[:], in1=pr16[:, :, 0:8],
                                                     op0=ALU.mult, op1=ALU.add), reads=[cmp_, hs, pr16], writes=[prs])
        k.op("dve", lambda e: e.memset(lo[:], 0.0), writes=[lo])
        tot = banks[6]
        for it in range(NBIS):
            w = 2.0 ** -(it + 1)
            k.op("dve", lambda e: e.tensor_scalar(out=mid[:], in0=lo[:], scalar1=w, scalar2=None, op0=ALU.add),
                 reads=[lo], writes=[mid])
            k.op("dve", lambda e: e.tensor_tensor(out=cmp_[:], in0=prs[:], in1=mid[:].unsqueeze(1).broadcast_to([128, ntl, 8]),
                                                  op=ALU.is_gt), reads=[prs, mid], writes=[cmp_])
            k.op("dve", lambda e: e.tensor_reduce(out=cpart[:], in_=cmp_[:].rearrange("p t e -> p e t"), op=ALU.add, axis=AX.X),
                 reads=[cmp_], writes=[cpart])
            k.op("pe", lambda e: e.matmul(tot[:, 0:8], ones[:], cpart[:], start=True, stop=True), reads=[ones, cpart], writes=[tot])
            k.op("dve", lambda e: e.tensor_scalar(out=sel[:], in0=tot[:, 0:8], scalar1=cap - 0.5, scalar2=w, op0=ALU.is_ge,
                                                  op1=ALU.mult), reads=[tot], writes=[sel])
            k.op("dve", lambda e: e.tensor_tensor(out=lo[:], in0=lo[:], in1=sel[:], op=ALU.add), reads=[lo, sel], writes=[lo])
        k.op("dve", lambda e: e.tensor_tensor(out=mask[:], in0=prs[:], in1=lo[:].unsqueeze(1).broadcast_to([128, ntl, 8]),
                                              op=ALU.is_gt), reads=[prs, lo], writes=[mask])
        k.op("dve", lambda e: e.tensor_tensor(out=gate[:], in0=prs[:], in1=mask[:], op=ALU.mult), reads=[prs, mask], writes=[gate])
        m2 = mask.a.rearrange("p t e -> p (t e)")
        pre, cnt = banks[6], banks[7]
        k.op("pe", lambda e: e.matmul(pre[:, 0:ntl * 8], lsts[:], m2, start=True, stop=True), reads=[lsts, mask], writes=[pre])
        k.op("pe", lambda e: e.matmul(cnt[:, 0:ntl * 8], ones[:], m2, start=True, stop=True), reads=[ones, mask], writes=[cnt])
        cv = cnt.a[:, 0:ntl * 8].rearrange("p (t e) -> p t e", e=8)
        pv = pre.a[:, 0:ntl * 8].rearrange("p (t e) -> p t e", e=8)
        k.op("dve", lambda e: e.memset(off[:, 0, :], float(base)), writes=[off])
        for i in range(ntl - 1):
            k.op("dve", lambda e: e.tensor_tensor(out=off[:, i + 1, :], in0=cv[:, i, :], in1=off[:, i, :], op=ALU.add),
                 reads=[cnt, off], writes=[off])
        k.op("dve", lambda e: e.tensor_tensor(out=off[:], in0=pv, in1=off[:], op=ALU.add), reads=[pre, off], writes=[off])
        k.op("dve", lambda e: e.tensor_scalar(out=idxf[:], in0=mask[:], scalar1=-8192.0, scalar2=8192.0, op0=ALU.mult, op1=ALU.add),
             reads=[mask], writes=[idxf])
        k.op("dve", lambda e: e.tensor_tensor(out=idxf[:], in0=idxf[:], in1=off[:], op=ALU.add), reads=[idxf, off], writes=[idxf])
        k.op("dve", lambda e: e.tensor_copy(out=idx[:], in_=idxf[:]), reads=[idxf], writes=[idx])
        return idx, gate, ntl

    sets = [(g.H2ALL, g.PRALL, NL, capL, 0, g.PART)]
    if NC_:
        sets.append((g.H2C, g.PRC, NC_, capC, capL, g.PARTC))
    stage = [k.sb([128, D]) for _ in range(2)]
    routed = []
    ns = 0
    for (ht, prt, N, cap, base, po) in sets:
        idx, gate, ntl = route(prt, N, cap, base)
        routed.append((idx, gate, ntl, po))
        for i in range(ntl):
            st_ = stage[ns % 2]
            ns += 1
            if ht is g.H2ALL:
                rr_, tt_ = (i * 128) // TL, (i * 128) % TL
                k.dma("sp", st_[:], ht[tt_ // 256, rr_, tt_ % 256:tt_ % 256 + 128, :], reads=[ht], writes=[st_])
            else:
                k.dma("sp", st_[:], ht[i * 128:(i + 1) * 128, :], reads=[ht], writes=[st_])
            for e_ in range(8):
                k.dma("pool", None, None, reads=[st_, idx], acc=[xs[e_]],
                      fn=lambda eng: eng.indirect_dma_start(
                          out=xs[e_][0:NSLOT, :], out_offset=bass.IndirectOffsetOnAxis(ap=idx[:, i, e_:e_ + 1], axis=0),
                          in_=st_[:, :], in_offset=None, bounds_check=breg, oob_is_err=False))

    xsT = k.sb([128, 8, NSLOT]); hid = k.sb([128, 16, NSLOT])
    w1b = [k.sb([128, 8, 128]) for _ in range(2)]; w3b = [k.sb([128, 8, 128]) for _ in range(2)]
    w2b = [k.sb([128, 16, 256]) for _ in range(1)]
    sil = [k.sb([128, 512]) for _ in range(2)]
    yb = [k.sb([128, 256]) for _ in range(2)]
    pieces = [(c0, min(512, NSLOT - c0)) for c0 in range(0, NSLOT, 512)]
    cnt = {"w": 0, "p": 0, "y": 0}
    for e_ in range(8):
        for st in range(NST):
            sg = stage[ns % 2]
            ns += 1
            k.dma("sp", sg[:], xs[e_][st * 128:(st + 1) * 128, :], reads=[xs[e_]], writes=[sg])
            for hf in range(2):
                tp = banks[hf]
                for j in range(4):
                    kc = hf * 4 + j
                    k.op("pe", lambda e: e.transpose(tp[:, j * 128:(j + 1) * 128], sg[:, kc * 128:(kc + 1) * 128], ident[:]),
                         reads=[sg, ident], writes=[tp])
                k.evac(xsT, xsT[:, hf * 4:hf * 4 + 4, st * 128:(st + 1) * 128], tp, tp[:].rearrange("p (a b) -> p a b", a=4))
        for fg in range(16):
            wb1, wb3 = w1b[cnt["w"] % 2], w3b[cnt["w"] % 2]
            cnt["w"] += 1
            k.dma("sp", wb1[:], L.w1c[e_, fg], reads=[L.w1c], writes=[wb1])
            k.dma("sp", wb3[:], L.w3c[e_, fg], reads=[L.w3c], writes=[wb3])
            for (c0, n) in pieces:
                p1, p3 = banks[2 + cnt["p"] % 2], banks[4 + cnt["p"] % 2]
                sl_ = sil[cnt["p"] % 2]
                cnt["p"] += 1
                for kc in range(8):
                    k.op("pe", lambda e: e.matmul(p1[:, :n], wb1[:, kc, :], xsT[:, kc, c0:c0 + n], start=(kc == 0), stop=(kc == 7)),
                         reads=[wb1, xsT], writes=[p1])
                for kc in range(8):
                    k.op("pe", lambda e: e.matmul(p3[:, :n], wb3[:, kc, :], xsT[:, kc, c0:c0 + n], start=(kc == 0), stop=(kc == 7)),
                         reads=[wb3, xsT], writes=[p3])
                k.op("act", lambda e: e.activation(out=sl_[:, :n], in_=p1[:, :n], func=AF.Silu), reads=[p1], writes=[sl_])
                k.op("dve", lambda e: e.tensor_tensor(out=hid[:, fg, c0:c0 + n], in0=p3[:, :n], in1=sl_[:, :n], op=ALU.mult),
                     reads=[p3, sl_], writes=[hid])
        for dq in range(4):
            wb2 = w2b[0]
            for q4 in range(4):
                k.dma("sp", wb2[:, q4 * 4:(q4 + 1) * 4, :], L.w2c[e_, dq, :, q4 * 4:(q4 + 1) * 4, :], reads=[L.w2c], writes=[wb2])
            for st in range(NST):
                po_ = banks[cnt["y"] % 2]
                y_ = yb[cnt["y"] % 2]
                cnt["y"] += 1
                for fg in range(16):
                    k.op("pe", lambda e: e.matmul(po_[:, 0:256], hid[:, fg, st * 128:(st + 1) * 128], wb2[:, fg, :],
                                                  start=(fg == 0), stop=(fg == 15)), reads=[hid, wb2], writes=[po_])
                k.evac(y_, y_[:], po_, po_[:, 0:256])
                k.dma("pool", ys[e_][st * 128:(st + 1) * 128, dq * 256:(dq + 1) * 256], y_[:], reads=[y_], acc=[ys[e_]])

    accb = [k.sb([128, D]) for _ in range(2)]
    stage = stage + [k.sb([128, D]) for _ in range(2)]
    for s_ in stage:
        k.op("dve", lambda e: e.memset(s_[:], 0.0), writes=[s_])
    na = 0
    for (idx, gate, ntl, po) in routed:
        for i in range(ntl):
            ac = accb[na % 2]
            na += 1
            for e_ in range(8):
                g_ = stage[ns % 4]
                ns += 1
                k.dma("pool", None, None, reads=[ys[e_], idx], writes=[g_],
                      fn=lambda eng: eng.indirect_dma_start(
                          out=g_[:, :], out_offset=None, in_=ys[e_][0:NSLOT, :],
                          in_offset=bass.IndirectOffsetOnAxis(ap=idx[:, i, e_:e_ + 1], axis=0),
                          bounds_check=breg, oob_is_err=False))
                if e_ == 0:
                    k.op("dve", lambda e: e.tensor_scalar(out=ac[:], in0=g_[:], scalar1=gate[:, i, 0:1], scalar2=None, op0=ALU.mult),
                         reads=[g_, gate], writes=[ac])
                else:
                    k.op("dve", lambda e: e.scalar_tensor_tensor(out=ac[:], in0=g_[:], scalar=gate[:, i, e_:e_ + 1], in1=ac[:],
                                                                 op0=ALU.mult, op1=ALU.add), reads=[g_, gate, ac], writes=[ac])
            k.dma("sp", po[i * 128:(i + 1) * 128, :], ac[:], reads=[ac], acc=[po])
    k.stage_end()


def st_p5(k, g, L, ctx_out, final):
    k.stage_begin()
    modT = g.modT
    ident = k.sb([128, 128])
    k.dma("sp", ident[:], g.idn[:], reads=[g.idn], writes=[ident])
    if final:
        fgs = k.sb([128, 8])
        k.dma("sp", fgs[:], g.fg[:], reads=[g.fg], writes=[fgs])
        ones = k.sb([128, 128])
        k.op("dve", lambda e: e.memset(ones[:], 1.0), writes=[ones])
        epsb = k.sb([128, 1])
        k.op("dve", lambda e: e.memset(epsb[:], EPS), writes=[epsb])
        sq = k.sb([128, 8, 512]); ssp = k.ps([128, 512]); rs = k.sb([128, 512])
    chunks = [(i * 512, 512, 0) for i in range(TL // 512)]
    if ctx_out:
        chunks.append((TL, NCTX, 1))
    xb = [k.sb([128, 8, 512]) for _ in range(2)]
    mo = k.sb([128, 8, 512])
    mtile = [k.sb([128, D]) for _ in range(2)]
    tps = [k.ps([128, 512]) for _ in range(2)]
    x1v = g.X1T.a.rearrange("(f p) t -> p f t", p=128)
    cnt = {"t": 0, "m": 0}
    for ci, (t0, n, col) in enumerate(chunks):
        x_ = xb[ci % 2]
        k.dma("sp", x_[:, :, :n], x1v[:, :, t0:t0 + n], reads=[g.X1T], writes=[x_])
        for sub in range(n // 128):
            mt = mtile[cnt["m"] % 2]
            cnt["m"] += 1
            if col == 0:
                k.dma("sp", mt[:], g.MOE[t0 + sub * 128:t0 + (sub + 1) * 128, :], reads=[g.MOE], writes=[mt])
            else:
                k.dma("sp", mt[:], g.MOEC[sub * 128:(sub + 1) * 128, :], reads=[g.MOEC], writes=[mt])
            for hf in range(2):
                tp = tps[cnt["t"] % 2]
                cnt["t"] += 1
                for j in range(4):
                    c = hf * 4 + j
                    k.op("pe", lambda e: e.transpose(tp[:, j * 128:(j + 1) * 128], mt[:, c * 128:(c + 1) * 128], ident[:]),
                         reads=[mt, ident], writes=[tp])
                k.evac(mo, mo[:, hf * 4:hf * 4 + 4, sub * 128:(sub + 1) * 128], tp, tp[:].rearrange("p (a b) -> p a b", a=4))
        for f in range(8):
            k.op("dve", lambda e: e.scalar_tensor_tensor(out=x_[:, f, :n], in0=mo[:, f, :n], scalar=modT[:, 40 + f, col:col + 1],
                                                         in1=x_[:, f, :n], op0=ALU.mult, op1=ALU.add),
                 reads=[mo, modT, x_], writes=[x_])
        if final:
            k.op("act", lambda e: e.activation(out=sq[:, :, :n], in_=x_[:, :, :n], func=AF.Square), reads=[x_], writes=[sq])
            for f in range(8):
                k.op("pe", lambda e: e.matmul(ssp[:, :n], ones[:], sq[:, f, :n], start=(f == 0), stop=(f == 7)),
                     reads=[ones, sq], writes=[ssp])
            k.op("act", lambda e: e.activation(out=rs[:, :n], in_=ssp[:, :n], func=AF.Sqrt, scale=1.0 / D, bias=epsb[:]),
                 reads=[ssp, epsb], writes=[rs])
            k.op("dve", lambda e: e.reciprocal(out=rs[:, :n], in_=rs[:, :n]), reads=[rs], writes=[rs])
            for f in range(8):
                k.op("dve", lambda e: e.scalar_tensor_tensor(out=x_[:, f, :n], in0=x_[:, f, :n], scalar=fgs[:, f:f + 1],
                                                             in1=rs[:, :n], op0=ALU.mult, op1=ALU.mult),
                     reads=[x_, fgs, rs], writes=[x_])
            ov = g.out.a.rearrange("(f p) t -> p f t", p=128)
            k.dma("pool", ov[:, :, t0:t0 + n], x_[:, :, :n], reads=[x_], writes=[g.out], is_output=True)
        else:
            if col == 0:
                ov = g.xown_next.a.rearrange("(f p) t -> p f t", p=128)
                k.dma("pool", ov[:, :, t0:t0 + n], x_[:, :, :n], reads=[x_], acc=[g.xown_next])
            else:
                ov = g.xc_next.a.rearrange("(f p) t -> p f t", p=128)
                k.dma("pool", ov[:, :, :], x_[:, :, :n], reads=[x_], acc=[g.xc_next])
    k.stage_end()


def build_fused(nlayer, stop=None):
    k = KB()
    g = Ctx()
    step = [0]

    def chk(exports):
        step[0] += 1
        if stop is not None and step[0] == stop:
            for nm, t in exports:
                o = k.outp('dbg_' + nm, list(t.a.shape))
                k.dma('sp', o[:], t[:], reads=[t], writes=[o], is_output=True)
            return True
        return False
    g.xall = k.inp("xall", [16, 2, 64, TL])
    g.xown = k.inp("xown", [D, TL]); g.xc = k.inp("xc", [D, NCTX])
    g.cv = k.inp("cv", [128, 8, 2])
    g.cosk = k.inp("cosk", [128, NKALL]); g.sink = k.inp("sink", [128, NKALL])
    g.cosq = k.inp("cosq", [128, TL]); g.sinq = k.inp("sinq", [128, TL])
    g.msk = k.inp("msk", [128, 6, 128]); g.idn = k.inp("idn", [128, 128]); g.lst = k.inp("lst", [128, 128])
    g.ccb = k.inp("ccb", [128, 256]); g.cs128 = k.inp("cs128", [128, 512]); g.tw = k.inp("tw", [64, 256])
    g.cs64 = k.inp("cs64", [64, 128]); g.dftc = k.inp("dftc", [128, 2, 512])
    g.hsel = k.inp("hsel", [128, 1]); g.fg = k.inp("fg", [128, 8])
    g.out = k.outp("outT", [D, TL])
    Ls = []
    for l in range(nlayer):
        L = Ctx()
        sfx = "_%d" % l
        L.adaw = k.inp("adaw" + sfx, [12, 128, 8, 512]); L.adab = k.inp("adab" + sfx, [128, 48]); L.g1 = k.inp("g1" + sfx, [128, 8])
        L.wAfm = k.inp("wAfm" + sfx, [NFM, 128, 8, 128]); L.wAtm = k.inp("wAtm" + sfx, [128, 8, TMW])
        L.wQ = k.inp("wQ" + sfx, [6, 128, 8, 128])
        L.dlam = k.inp("dlam" + sfx, [128]); L.dng = k.inp("dng" + sfx, [64])
        L.cw = k.inp("cw" + sfx, [2, 96, 8]); L.gb = k.inp("gb" + sfx, [8]); L.mng = k.inp("mng" + sfx, [192])
        L.fwblk = k.inp("fwblk" + sfx, [128, 128])
        L.wo = k.inp("wo" + sfx, [8, 128, 8, 128]); L.g2 = k.inp("g2" + sfx, [128, 8]); L.wr = k.inp("wr" + sfx, [128, 8, 16])
        Ls.append(L)
    g.FT_ = k.dram("FT_", [128, NKALL]); g.KT_ = k.dram("KT_", [384, NKALL]); g.KST_ = k.dram("KST_", [384, NKALL])
    g.MQP = k.dram("MQP", [2, 96, XPW]); g.MKP = k.dram("MKP", [2, 96, XPW])
    g.TM = k.dram("TM", [NKALL, TMW])
    g.QT_ = k.dram("QT_", [384, TL]); g.QST_ = k.dram("QST_", [384, TL]); g.QCT = k.dram("QCT", [384, NCTX])
    g.DA = k.dram("DA", [TL, 384]); g.DAC = k.dram("DAC", [NCTX, 384])
    g.SEND = k.dram("SEND", [S, 320]); g.MG = k.dram("MGc", [8, 2, 1024, 320])
    g.CMINE = k.dram("CMINE", [NCTX, 320]); CG2 = k.dram("CG2", [2 * NCTX, 320])
    g.CG = T(CG2.a.rearrange("(r t) c -> r t c", r=2)); g.CG2 = CG2
    g.X1T = k.dram("X1T", [D, TL + NCTX])
    g.H2SEND = k.dram("H2SEND", [TL, D]); g.H2ALL = k.dram("H2ALLc", [16, 2, 256, D]); g.H2C = k.dram("H2C", [NCTX, D])
    g.PRSEND = k.dram("PRSEND", [TL, NEXP]); g.PRALL = k.dram("PRALL", [S, NEXP]); g.PRC = k.dram("PRC", [NCTX, NEXP])
    g.xs = [k.dram("xs%d" % e, [1152, D]) for e in range(8)]
    g.ys = [k.dram("ys%d" % e, [1152, D]) for e in range(8)]
    g.PART = k.dram("PART", [S, D]); g.PARTC = k.dram("PARTC", [NCTX, D])
    g.MOE = k.dram("MOE", [TL, D]); g.MOEC = k.dram("MOEC", [NCTX, D])
    xown1 = k.dram("xown1", [D, TL]); xall1 = k.dram("xall1", [16, 2, 64, TL]); xc1 = k.dram("xc1", [D, NCTX])
    g.modT = k.sbp([128, 48, 2])
    g.breg = {}
    for ns in (1152, 1024):
        r = k.nc.gpsimd.alloc_register("bchk%d" % ns)
        k.nc.gpsimd.reg_mov(r, ns - 1)
        g.breg[ns] = r
    for l in range(nlayer):
        L = Ls[l]
        ctx_out = l < nlayer - 1
        final = not ctx_out
        lam_init = 0.8 - 0.6 * math.exp(-0.3 * l)
        st_proj(k, g, L, ctx_out)
        if chk([("TM", g.TM), ("KT", g.KT_), ("KST", g.KST_), ("QT", g.QT_), ("QST", g.QST_), ("QCT", g.QCT), ("MQP", g.MQP),
                ("MKP", g.MKP), ("FT", g.FT_)]):
            return k
        st_attn(k, g, L, lam_init, ctx_out)
        if chk([("DA", g.DA), ("DAC", g.DAC)]):
            return k
        st_mlstm(k, g, L)
        if chk([("SEND", g.SEND), ("CMINE", g.CMINE)]):
            return k
        st_fourier(k, g, L, ctx_out)
        if chk([("SEND", g.SEND), ("CMINE", g.CMINE)]):
            return k
        for i in range(8):
            k.coll("AllGather", ALU.bypass, g.SEND, g.MG, RG, in_ap=g.SEND.a[i * 1024:(i + 1) * 1024, :],
                   out_ap=g.MG.a[i].rearrange("r t c -> (r t) c"))
        if ctx_out:
            k.coll("AllGather", ALU.bypass, g.CMINE, g.CG2, RG)
            g.CG.w = g.CG2.w
        if chk([("MG", g.MG), ("CG2", g.CG2), ("DA", g.DA), ("DAC", g.DAC)]):
            return k
        st_p3(k, g, L, ctx_out)
        if chk([("X1T", g.X1T), ("H2SEND", g.H2SEND), ("PRSEND", g.PRSEND), ("H2C", g.H2C), ("PRC", g.PRC)]):
            return k
        for i in range(16):
            k.coll("AllGather", ALU.bypass, g.H2SEND, g.H2ALL, RG, in_ap=g.H2SEND.a[i * 256:(i + 1) * 256, :],
                   out_ap=g.H2ALL.a[i].rearrange("r t c -> (r t) c"))
        k.coll("AllGather", ALU.bypass, g.PRSEND, g.PRALL, RG)
        if chk([("H2ALL", g.H2ALL), ("PRALL", g.PRALL)]):
            return k
        sfx = "_%d" % l
        L.w1c = k.inp("w1c" + sfx, [8, 16, 128, 8, 128]); L.w3c = k.inp("w3c" + sfx, [8, 16, 128, 8, 128])
        L.w2c = k.inp("w2c" + sfx, [8, 4, 128, 16, 256])
        st_moe(k, g, L, ctx_out)
        if chk([("PART", g.PART), ("PARTC", g.PARTC)]):
            return k
        k.coll("ReduceScatter", ALU.add, g.PART, g.MOE, RG)
        if ctx_out:
            k.coll("AllReduce", ALU.add, g.PARTC, g.MOEC, RG)
        if chk([("MOE", g.MOE), ("MOEC", g.MOEC)]):
            return k
        g.xown_next, g.xc_next = xown1, xc1
        st_p5(k, g, L, ctx_out, final)
        if not final:
            for i in range(16):
                k.coll("AllGather", ALU.bypass, xown1, xall1, RG, in_ap=xown1.a[i * 64:(i + 1) * 64, :],
                       out_ap=xall1.a[i].rearrange("r w t -> (r w) t"))
            g.xall = xall1
            g.xown, g.xc = xown1, xc1
            if chk([("xown1", xown1), ("xall1", xall1), ("xc1", xc1)]):
                return k
    return k


OFF_F, OFF_DQ, OFF_MO, OFF_MQ, OFF_MK, OFF_DK, OFF_DV, OFF_MV, OFF_G = 0, 256, 640, 1024, 1408, 1792, 2176, 2560, 2944


def _swap_perm():
    p = np.zeros(32, np.int64)
    for a in range(2):
        for pp in range(2):
            for j in range(8):
                p[a * 16 + pp * 8 + j] = a * 16 + (1 - pp) * 8 + j
    return np.concatenate([gi * 32 + p for gi in range(12)])


def chunk_groups(wm, groups):
    out = np.zeros((len(groups), 128, 8, 128), np.float32)
    for i, cols in enumerate(groups):
        blk = wm[:, cols]
        out[i, :, :, :len(cols)] = blk.reshape(8, 128, len(cols)).transpose(1, 0, 2)
    return out


def chunk_w(wm, nj):
    K_, n = wm.shape
    out = np.zeros((K_, nj * 128), np.float32)
    out[:, :n] = wm
    return np.ascontiguousarray(out.reshape(K_ // 128, 128, nj, 128).transpose(2, 1, 0, 3))


def vecT(v, nchunk):
    return np.ascontiguousarray(v.reshape(nchunk, 128).T)


def rope_tables(nrows_lat):
    t_row = np.repeat(np.arange(nrows_lat), 64).astype(np.float32)
    t_col = np.tile(np.arange(64), nrows_lat).astype(np.float32)
    inv = (np.float32(10000.0) ** (-np.arange(8, dtype=np.float32) / np.float32(8))).astype(np.float32)
    ar = t_row[:, None] * inv
    ac = t_col[:, None] * inv
    ang = np.concatenate([ar, ar, ac, ac], -1)
    cos = np.cos(ang).astype(np.float32)
    sin = np.sin(ang).astype(np.float32)
    sgn = np.tile(np.concatenate([-np.ones(8), np.ones(8)]), 2).astype(np.float32)
    ssin = sin * sgn
    cosk = np.concatenate([np.ones((NCTX, 32), np.float32), cos], 0).T
    sink = np.concatenate([np.zeros((NCTX, 32), np.float32), ssin], 0).T
    return (np.ascontiguousarray(np.tile(cosk, (4, 1))), np.ascontiguousarray(np.tile(sink, (4, 1))))


def mlstm_masks():
    j = np.arange(128)[:, None]
    s = np.arange(128)[None, :]
    UF = (j > s); TRF = (j <= s)
    MF = np.where(s < j, -30000.0, 0.0)
    UB = (j < s); TRB = (j >= s)
    MB = np.where(j < s, -30000.0, 0.0)
    return np.ascontiguousarray(np.stack([UF, TRF, MF, UB, TRB, MB], 1).astype(np.float32))


def fourier_tables():
    def cs(n, m, N):
        ang = 2.0 * np.pi * np.outer(np.arange(n), np.arange(m)) / N
        return np.cos(ang), np.sin(ang)
    c64, s64 = cs(64, 64, 64)
    z = np.zeros((64, 64))
    ccb = np.concatenate([np.block([[c64, z], [z, c64]]), np.block([[s64, z], [z, s64]])], 1)
    c128, s128 = cs(128, 128, 128)
    cs128 = np.concatenate([c128, s128, -s128, c128], 1)
    tc, ts = cs(64, 128, S)
    tw = np.concatenate([tc, ts], 1) / np.sqrt(64.0 * S)
    cs64 = np.concatenate([c64, -s64], 1)
    cn, sn = cs(NCTX, NCTX, NCTX)
    dft = np.concatenate([cn, -sn], 1) / np.sqrt(64.0 * NCTX)
    dftc = dft.reshape(2, 128, 512).transpose(1, 0, 2)
    f = lambda a: np.ascontiguousarray(a.astype(np.float32))
    return {"ccb": f(ccb), "cs128": f(cs128), "tw": f(tw), "cs64": f(cs64), "dftc": f(dftc)}


def host_inputs(inp):
    nlayer = inp["w_in"].shape[0]
    cosk, sink = rope_tables(S // 64)
    tabs = fourier_tables()
    sw = _swap_perm()
    ar = np.arange
    shared = {"cosk": cosk, "sink": sink, "msk": mlstm_masks(), "idn": np.eye(128, dtype=np.float32),
              "lst": np.ascontiguousarray(np.triu(np.ones((128, 128), np.float32), 1)),
              "fg": vecT(inp["final_g"], 8)}
    shared.update(tabs)
    for l in range(nlayer):
        sfx = "_%d" % l
        shared["adaw" + sfx] = np.ascontiguousarray(inp["ada_w"][l].reshape(8, 128, 12, 512).transpose(2, 1, 0, 3))
        shared["adab" + sfx] = vecT(inp["ada_b"][l], 48)
        shared["g1" + sfx] = vecT(inp["norm1_g"][l], 8)
        shared["wQ" + sfx] = chunk_groups(inp["w_in"][l], [OFF_DQ + ar(j * 128, (j + 1) * 128) for j in range(3)]
                                          + [OFF_DQ + sw[j * 128:(j + 1) * 128] for j in range(3)])
        shared["dlam" + sfx] = np.ascontiguousarray(inp["d_lam"][l].reshape(128))
        shared["dng" + sfx] = inp["d_norm_g"][l]
        shared["wo" + sfx] = chunk_w(inp["w_out"][l], 8)
        shared["g2" + sfx] = vecT(inp["norm2_g"][l], 8)
        shared["wr" + sfx] = np.ascontiguousarray(inp["router_w"][l].reshape(8, 128, 16).transpose(1, 0, 2))
    maps = []
    for c in range(NCORE):
        b, h = c // 2, c % 2
        m = dict(shared)
        m["xall"] = np.ascontiguousarray(inp["x"][b].reshape(2, TL, 16, 64).transpose(2, 0, 3, 1))
        m["xown"] = np.ascontiguousarray(inp["x"][b, h * TL:(h + 1) * TL].T)
        m["xc"] = np.ascontiguousarray(inp["ctx"][b].T)
        cv = np.stack([inp["c"][b], inp["c_ctx"]], axis=-1)
        m["cv"] = np.ascontiguousarray(cv.reshape(8, 128, 2).transpose(1, 0, 2))
        q0 = NCTX + h * TL
        m["cosq"] = np.ascontiguousarray(cosk[:, q0:q0 + TL]); m["sinq"] = np.ascontiguousarray(sink[:, q0:q0 + TL])
        m["hsel"] = np.full((128, 1), float(h), np.float32)
        heads = [2 * h, 2 * h + 1]
        es = slice(8 * h, 8 * h + 8)
        for l in range(nlayer):
            sfx = "_%d" % l
            w = inp["w_in"][l]
            groups = [OFF_F + h * 128 + ar(128)]
            groups += [OFF_DK + ar(j * 128, (j + 1) * 128) for j in range(3)]
            groups += [OFF_DK + sw[j * 128:(j + 1) * 128] for j in range(3)]
            groups += [OFF_MQ + hh * 96 + ar(96) for hh in heads]
            groups += [OFF_MK + hh * 96 + ar(96) for hh in heads]
            m["wAfm" + sfx] = chunk_groups(w, groups)
            gcols = [[hh, 4 + hh, 8 + hh, 12 + hh] for hh in heads]
            tmc = np.concatenate([OFF_DV + ar(384)] + [OFF_MV + hh * 96 + ar(96) for hh in heads]
                                 + [OFF_MO + hh * 96 + ar(96) for hh in heads] + [OFF_G + np.array(gc) for gc in gcols])
            m["wAtm" + sfx] = np.ascontiguousarray(w[:, tmc].reshape(8, 128, TMW).transpose(1, 0, 2))
            cwl, cbl = inp["m_conv_w"][l], inp["m_conv_b"][l]
            cw = np.zeros((2, 96, 8), np.float32)
            for i, hh in enumerate(heads):
                cw[i, :, 0:3] = cwl[:, hh * 96:(hh + 1) * 96].T
                cw[i, :, 3] = cbl[hh * 96:(hh + 1) * 96]
                cw[i, :, 4:7] = cwl[:, 384 + hh * 96:384 + (hh + 1) * 96].T
                cw[i, :, 7] = cbl[384 + hh * 96:384 + (hh + 1) * 96]
            m["cw" + sfx] = cw
            m["gb" + sfx] = np.ascontiguousarray(np.stack([inp["m_gate_b"][l][gc] for gc in gcols]).reshape(8))
            m["mng" + sfx] = np.ascontiguousarray(inp["m_norm_g"][l][heads[0] * 96:(heads[1] + 1) * 96])
            fw = inp["four_w"][l]
            fwblk = np.zeros((128, 128), np.float32)
            fwblk[0:64, 0:64] = fw[2 * h]
            fwblk[64:128, 64:128] = fw[2 * h + 1]
            m["fwblk" + sfx] = fwblk
            w1 = inp["exp_w1"][l][es]; w3 = inp["exp_w3"][l][es]; w2 = inp["exp_w2"][l][es]
            m["w1c" + sfx] = np.ascontiguousarray(w1.reshape(8, 8, 128, 16, 128).transpose(0, 3, 2, 1, 4))
            m["w3c" + sfx] = np.ascontiguousarray(w3.reshape(8, 8, 128, 16, 128).transpose(0, 3, 2, 1, 4))
            m["w2c" + sfx] = np.ascontiguousarray(w2.reshape(8, 16, 128, 4, 256).transpose(0, 3, 2, 1, 4))
        maps.append(m)
    return maps


def kernel(**inp):
    inp = {k_: np.asarray(v_) for k_, v_ in inp.items()}
    nlayer = inp["w_in"].shape[0]
    kb = build_fused(nlayer)
    res = run(kb, host_inputs(inp))
    out = np.zeros((B, S, D), np.float32)
    for c in range(NCORE):
        b, h = c // 2, c % 2
        out[b, h * TL:(h + 1) * TL] = res[c]["outT"].T
    return out
```

```python
import math
import numpy as np
import concourse.bass as bass
import concourse.mybir as mybir
from concourse.bass_utils import run_bass_kernel_spmd
from contextlib import ExitStack

F32 = mybir.dt.float32
I32 = mybir.dt.int32
U32 = mybir.dt.uint32
AF = mybir.ActivationFunctionType
ALU = mybir.AluOpType
AX = mybir.AxisListType

D = 1024
B = 4
S = 8192
NCTX = 256
NCORE = 8
TL = S // 2
EPS = 1e-6
PW = 2960
NEXP = 16

SEM_ROT = 12000
N_DMA_SEM = 32


class T:
    __slots__ = ("a", "w", "r", "name")

    def __init__(self, a, name=""):
        self.a = a
        self.w = {}
        self.r = {}
        self.name = name

    def __getitem__(self, idx):
        return self.a[idx]

    def sub(self, idx, name=""):
        return T(self.a[idx], name)


def _merge(d, stamp):
    k = id(stamp[0])
    if k not in d or d[k][1] < stamp[1]:
        d[k] = stamp


class KB:
    def __init__(self):
        self.nc = bass.Bass("TRN2", target_bir_lowering=False)
        nc = self.nc
        self.es = ExitStack()
        self.eng = {"pe": nc.tensor, "act": nc.scalar, "dve": nc.vector,
                    "pool": nc.gpsimd, "sp": nc.sync}
        self.cur = {}
        self.owner = {}
        self.waited = {e: {} for e in self.eng}
        self.nsem = 0
        self.dma_pool = []
        self.dma_rr = 0
        self.nname = 0
        self.out_stamps = []
        self.n_wait = 0
        self.n_op = 0
        self.rr = 0
        self.coll_stamps = []
        self.stk = self.es

    def new_sem(self):
        self.nsem += 1
        return self.es.enter_context(self.nc.semaphore("s%d" % self.nsem))

    def _nm(self, p):
        self.nname += 1
        return "%s%d" % (p, self.nname)

    def sb(self, shape, dt=F32, name=None):
        name = name or self._nm("sb")
        g = self.nc.sbuf_tensor(name, list(shape), dt)
        return T(self.stk.enter_context(g).ap(), name)

    def sbp(self, shape, dt=F32, name=None):
        name = name or self._nm("sbp")
        return T(self.nc.alloc_sbuf_tensor(name, list(shape), dt).ap(), name)

    def ps(self, shape, dt=F32, name=None):
        name = name or self._nm("ps")
        g = self.nc.psum_tensor(name, list(shape), dt)
        return T(self.stk.enter_context(g).ap(), name)

    def barrier(self):
        st = []
        for e, c in self.cur.items():
            st.append((c[0], c[1]))
        for ent in self.dma_pool:
            if ent[1] > 0:
                st.append((ent[0], ent[1]))
        st.extend(self.coll_stamps)
        for e in ("pe", "act", "dve", "pool", "sp"):
            self._wait(e, st)

    def stage_begin(self):
        self.stk = ExitStack()

    def stage_end(self):
        self.barrier()
        self.stk.close()

    def coll(self, kind, op, in_t, out_t, groups, in_ap=None, out_ap=None):
        self._wait("pool", self._deps([in_t], [out_t]))
        if not hasattr(self, "coll_pool"):
            self.coll_pool = [[self.new_sem(), 0] for _ in range(12)]
            self.coll_n = 0
        ent = self.coll_pool[self.coll_n % len(self.coll_pool)]
        self.coll_n += 1
        ia = in_t.a if in_ap is None else in_ap
        oa = out_t.a if out_ap is None else out_ap
        ins = self.nc.gpsimd.collective_compute(kind, op, replica_groups=groups, ins=[ia.opt()], outs=[oa.opt()])
        ent[1] += 1
        ins.then_inc(ent[0], 1)
        stamp = (ent[0], ent[1])
        self.coll_stamps = [st_ for st_ in self.coll_stamps if st_[0] is not ent[0]] + [stamp]
        _merge(in_t.r, stamp)
        _merge(out_t.w, stamp)

    def dram(self, name, shape, dt=F32, kind="Internal"):
        return T(self.nc.dram_tensor(name, list(shape), dt, kind=kind).ap(), name)

    def inp(self, name, shape, dt=F32):
        if not hasattr(self, "in_names"):
            self.in_names = set()
        self.in_names.add(name)
        return self.dram(name, shape, dt, kind="ExternalInput")

    def outp(self, name, shape, dt=F32):
        return self.dram(name, shape, dt, kind="ExternalOutput")

    def _stamp_new(self, e):
        c = self.cur.get(e)
        if c is None or c[1] >= SEM_ROT:
            c = [self.new_sem(), 0]
            self.owner[id(c[0])] = e
            self.cur[e] = c
        c[1] += 1
        return (c[0], c[1])

    def _wait(self, e, stamps):
        eng = self.eng[e]
        wd = self.waited[e]
        need = {}
        for (sem, val) in stamps:
            k = id(sem)
            if e == "pe" and self.owner.get(k) == "pe":
                continue
            if wd.get(k, 0) >= val:
                continue
            if k not in need or need[k][1] < val:
                need[k] = (sem, val)
        for k, (sem, val) in need.items():
            eng.wait_ge(sem, val)
            wd[k] = val
            self.n_wait += 1

    def _deps(self, reads, writes):
        st = []
        for t in reads:
            st.extend(t.w.values())
        for t in writes:
            st.extend(t.w.values())
            st.extend(t.r.values())
        return st

    def _record(self, stamp, reads, writes):
        for t in reads:
            _merge(t.r, stamp)
        for t in writes:
            t.w = {id(stamp[0]): stamp}
            t.r = {}

    def op(self, e, ins_fn, reads=(), writes=()):
        self._wait(e, self._deps(reads, writes))
        ins = ins_fn(self.eng[e])
        stamp = self._stamp_new(e)
        ins.then_inc(stamp[0], 1)
        self._record(stamp, reads, writes)
        self.n_op += 1
        return ins

    def dma(self, e, out_ap, in_ap, reads=(), writes=(), is_output=False, fn=None, acc=(), **kw):
        if len(self.dma_pool) < N_DMA_SEM:
            ent = [self.new_sem(), 0]
            self.dma_pool.append(ent)
        else:
            ent = self.dma_pool[self.dma_rr % len(self.dma_pool)]
            self.dma_rr += 1
        st = self._deps(reads, writes)
        if ent[1] > 0:
            st.append((ent[0], ent[1]))
        self._wait(e, st)
        eng = self.eng[e]
        if fn is None:
            ins = eng.dma_start(out=out_ap, in_=in_ap, **kw)
        else:
            ins = fn(eng)
        ent[1] += 16
        stamp = (ent[0], ent[1])
        ins.then_inc(ent[0], 16)
        self._record(stamp, reads, writes)
        for t in acc:
            _merge(t.w, stamp)
        if is_output:
            self.out_stamps.append(stamp)
        self.n_op += 1
        return ins

    def finish(self):
        self._wait("sp", self.out_stamps)
        return self.nc

    def evac(self, out_t, out_ap, in_t, in_ap):
        self.rr += 1
        if self.rr % 2:
            self.op("act", lambda e: e.copy(out=out_ap, in_=in_ap), reads=[in_t], writes=[out_t])
        else:
            self.op("dve", lambda e: e.tensor_copy(out=out_ap, in_=in_ap), reads=[in_t], writes=[out_t])


def run(kb, in_maps):
    nc = kb.finish()
    names = getattr(kb, "in_names", None)
    if names is not None:
        in_maps = [{n: m[n] for n in names} for m in in_maps]
    res = run_bass_kernel_spmd(nc, in_maps, core_ids=list(range(NCORE)))
    return res.results


RG = [[0, 1], [2, 3], [4, 5], [6, 7]]
NKALL = NCTX + S
NT = NKALL // 128
XPW = NKALL + 4
NFM = 11
TMW = 776
NBIS = 30


def mcol(t):
    return t * 128 + (2 if t >= 2 else 0)


def pcol(c):
    return c + 1 if c < NCTX else c + 3


class Ctx:
    pass


def st_proj(k, g, L, ctx_out):
    k.stage_begin()
    ones = k.sb([128, 128])
    k.op("dve", lambda e: e.memset(ones[:], 1.0), writes=[ones])
    epsb = k.sb([128, 1])
    k.op("dve", lambda e: e.memset(epsb[:], EPS), writes=[epsb])
    zer = k.sb([96, 2])
    k.op("dve", lambda e: e.memset(zer[:], 0.0), writes=[zer])
    for tp in (g.MQP, g.MKP):
        for hd in range(2):
            for c0 in (0, NCTX + 1, XPW - 1):
                n = 2 if c0 == NCTX + 1 else 1
                k.dma("sp", tp[hd, :, c0:c0 + n], zer[:, 0:n], reads=[zer], acc=[tp], allow_slow_non_contiguous=True)
    cvs = k.sb([128, 8, 2])
    k.dma("sp", cvs[:], g.cv[:], reads=[g.cv], writes=[cvs])
    scv = k.sb([128, 8, 2])
    k.op("act", lambda e: e.activation(out=scv[:], in_=cvs[:], func=AF.Silu), reads=[cvs], writes=[scv])
    adabs = k.sb([128, 48])
    k.dma("sp", adabs[:], L.adab[:], reads=[L.adab], writes=[adabs])
    g1s = k.sb([128, 8])
    k.dma("sp", g1s[:], L.g1[:], reads=[L.g1], writes=[g1s])
    modT = g.modT
    abuf = [k.sb([128, 8, 512]) for _ in range(2)]
    mps = [k.ps([128, 2]) for _ in range(2)]
    for gi in range(12):
        ab = abuf[gi % 2]
        k.dma("sp", ab[:], L.adaw[gi], reads=[L.adaw], writes=[ab])
        for jj in range(4):
            j = 4 * gi + jj
            mp = mps[j % 2]
            for kc in range(8):
                k.op("pe", lambda e: e.matmul(mp[:], ab[:, kc, jj * 128:(jj + 1) * 128], scv[:, kc, :],
                                              start=(kc == 0), stop=(kc == 7)), reads=[ab, scv], writes=[mp])
            k.op("dve", lambda e: e.tensor_scalar(out=modT[:, j, :], in0=mp[:], scalar1=adabs[:, j:j + 1], scalar2=None,
                                                  op0=ALU.add), reads=[mp, adabs], writes=[modT])
    a1 = k.sb([128, 8, 2])
    k.op("dve", lambda e: e.tensor_scalar(out=a1[:], in0=modT[:, 8:16, :], scalar1=1.0, scalar2=None, op0=ALU.add),
         reads=[modT], writes=[a1])
    k.op("dve", lambda e: e.tensor_tensor(out=a1[:], in0=a1[:], in1=g1s[:].unsqueeze(2).broadcast_to([128, 8, 2]),
                                          op=ALU.mult), reads=[a1, g1s], writes=[a1])
    wtm = k.sb([128, 8, TMW])
    k.dma("sp", wtm[:], L.wAtm[:], reads=[L.wAtm], writes=[wtm])
    xbuf = [k.sb([128, 8, 512]) for _ in range(2)]
    sq = k.sb([128, 8, 512])
    hT = [k.sb([128, 8, 512]) for _ in range(2)]
    ssp = k.ps([128, 512])
    rs = k.sb([128, 512])
    wbuf = [k.sb([128, 8, 128]) for _ in range(4)]
    pps = [k.ps([128, 512]) for _ in range(2)]
    obuf = [k.sb([128, 512]) for _ in range(2)]
    tps = [k.ps([128, 512]) for _ in range(2)]
    tob = [k.sb([128, TMW]) for _ in range(2)]
    cnt = {"c": 0, "w": 0, "t": 0}

    def norm_chunk(src_t, src_ap, n, col):
        ci = cnt["c"]
        cnt["c"] += 1
        xb, h = xbuf[ci % 2], hT[ci % 2]
        if isinstance(src_ap, tuple):
            xv_, t0_ = src_ap
            k.dma("sp", xb[0:64, :, :n], xv_[0, :, :, t0_:t0_ + n], reads=[src_t], writes=[xb])
            k.dma("sp", xb[64:128, :, :n], xv_[1, :, :, t0_:t0_ + n], reads=[src_t], acc=[xb])
        else:
            k.dma("sp", xb[:, :, :n], src_ap, reads=[src_t], writes=[xb])
        k.op("act", lambda e: e.activation(out=sq[:, :, :n], in_=xb[:, :, :n], func=AF.Square), reads=[xb], writes=[sq])
        for f in range(8):
            k.op("pe", lambda e: e.matmul(ssp[:, :n], ones[:], sq[:, f, :n], start=(f == 0), stop=(f == 7)),
                 reads=[ones, sq], writes=[ssp])
        k.op("act", lambda e: e.activation(out=rs[:, :n], in_=ssp[:, :n], func=AF.Sqrt, scale=1.0 / D, bias=epsb[:]),
             reads=[ssp, epsb], writes=[rs])
        k.op("dve", lambda e: e.reciprocal(out=rs[:, :n], in_=rs[:, :n]), reads=[rs], writes=[rs])
        for f in range(8):
            k.op("dve", lambda e: e.scalar_tensor_tensor(out=h[:, f, :n], in0=xb[:, f, :n], scalar=a1[:, f, col:col + 1],
                                                         in1=rs[:, :n], op0=ALU.mult, op1=ALU.mult),
                 reads=[xb, a1, rs], writes=[h])
            k.op("act", lambda e: e.activation(out=h[:, f, :n], in_=h[:, f, :n], func=AF.Identity,
                                               bias=modT[:, f, col:col + 1]), reads=[h, modT], writes=[h])
        return h

    def fm_group(h, n, wsrc, j, M, dst_t, dst_ap):
        i = cnt["w"] % 2
        wb = wbuf[cnt["w"] % 4]
        cnt["w"] += 1
        pp, ob = pps[i], obuf[i]
        k.dma("sp", wb[:], wsrc[j], reads=[wsrc], writes=[wb])
        for f in range(8):
            k.op("pe", lambda e: e.matmul(pp[:M, :n], wb[:, f, :M], h[:, f, :n], start=(f == 0), stop=(f == 7)),
                 reads=[wb, h], writes=[pp])
        k.evac(ob, ob[:M, :n], pp, pp[:M, :n])
        k.dma("pool", dst_ap, ob[:M, :n], reads=[ob], acc=[dst_t])

    xav = [g.xall.a[:, r, :, :].rearrange("(f ph) w t -> ph w f t", ph=2) for r in range(2)]
    xcv = g.xc.a.rearrange("(f p) t -> p f t", p=128)
    xov = g.xown.a.rearrange("(f p) t -> p f t", p=128)
    chunks = [(g.xc, xcv[:, :, :], NCTX, 1, 0)]
    for r in range(2):
        for i in range(TL // 512):
            chunks.append((g.xall, (xav[r], i * 512), 512, 0, NCTX + r * TL + i * 512))
    for (src_t, src_ap, n, col, c0) in chunks:
        h = norm_chunk(src_t, src_ap, n, col)
        fm_group(h, n, L.wAfm, 0, 128, g.FT_, g.FT_[:, c0:c0 + n])
        for j in range(3):
            fm_group(h, n, L.wAfm, 1 + j, 128, g.KT_, g.KT_[j * 128:(j + 1) * 128, c0:c0 + n])
            fm_group(h, n, L.wAfm, 4 + j, 128, g.KST_, g.KST_[j * 128:(j + 1) * 128, c0:c0 + n])
        p0 = pcol(c0)
        for hd in range(2):
            fm_group(h, n, L.wAfm, 7 + hd, 96, g.MQP, g.MQP[hd, :, p0:p0 + n])
            fm_group(h, n, L.wAfm, 9 + hd, 96, g.MKP, g.MKP[hd, :, p0:p0 + n])
        for sub in range(n // 128):
            i = cnt["t"] % 2
            cnt["t"] += 1
            tp, to = tps[i], tob[i]
            for f in range(8):
                k.op("pe", lambda e: e.matmul(tp[:, 0:512], h[:, f, sub * 128:(sub + 1) * 128], wtm[:, f, 0:512],
                                              start=(f == 0), stop=(f == 7)), reads=[h, wtm], writes=[tp])
            k.evac(to, to[:, 0:512], tp, tp[:, 0:512])
            for f in range(8):
                k.op("pe", lambda e: e.matmul(tp[:, 0:TMW - 512], h[:, f, sub * 128:(sub + 1) * 128], wtm[:, f, 512:TMW],
                                              start=(f == 0), stop=(f == 7)), reads=[h, wtm], writes=[tp])
            k.evac(to, to[:, 512:TMW], tp, tp[:, 0:TMW - 512])
            k.dma("pool", g.TM[c0 + sub * 128:c0 + (sub + 1) * 128, :], to[:], reads=[to], acc=[g.TM])
    qchunks = [(g.xown, xov[:, :, i * 512:(i + 1) * 512], 512, 0, i * 512, False) for i in range(TL // 512)]
    if ctx_out:
        qchunks.append((g.xc, xcv[:, :, :], NCTX, 1, 0, True))
    for (src_t, src_ap, n, col, c0, isc) in qchunks:
        h = norm_chunk(src_t, src_ap, n, col)
        for j in range(3):
            if isc:
                fm_group(h, n, L.wQ, j, 128, g.QCT, g.QCT[j * 128:(j + 1) * 128, 0:n])
            else:
                fm_group(h, n, L.wQ, j, 128, g.QT_, g.QT_[j * 128:(j + 1) * 128, c0:c0 + n])
                fm_group(h, n, L.wQ, 3 + j, 128, g.QST_, g.QST_[j * 128:(j + 1) * 128, c0:c0 + n])
    k.stage_end()


def st_attn(k, g, L, lam_init, ctx_out):
    k.stage_begin()
    NQ, NK = TL, NKALL
    NKT = NK // 128
    scale = 32 ** -0.5
    dl = k.sb([128, 128])
    k.dma("sp", dl[:], L.dlam.a.partition_broadcast(128), reads=[L.dlam], writes=[dl])
    pr = k.sb([128, 2, 32]); s2 = k.sb([128, 2]); lam = k.sb([128, 1]); nlam = k.sb([128, 1])
    dlv = dl.a.rearrange("p (a b c) -> p a b c", a=2, b=2)
    k.op("dve", lambda e: e.tensor_tensor(out=pr[:], in0=dlv[:, :, 0, :], in1=dlv[:, :, 1, :], op=ALU.mult),
         reads=[dl], writes=[pr])
    k.op("dve", lambda e: e.tensor_reduce(out=s2[:], in_=pr[:], op=ALU.add, axis=AX.X), reads=[pr], writes=[s2])
    k.op("act", lambda e: e.activation(out=s2[:], in_=s2[:], func=AF.Exp), reads=[s2], writes=[s2])
    k.op("dve", lambda e: e.tensor_tensor(out=lam[:], in0=s2[:, 0:1], in1=s2[:, 1:2], op=ALU.subtract),
         reads=[s2], writes=[lam])
    k.op("dve", lambda e: e.tensor_scalar(out=nlam[:], in0=lam[:], scalar1=-1.0, scalar2=-lam_init,
                                          op0=ALU.mult, op1=ALU.add), reads=[lam], writes=[nlam])
    gbc = k.sb([128, 64])
    k.dma("sp", gbc[:], L.dng.a.partition_broadcast(128), reads=[L.dng], writes=[gbc])
    k.op("dve", lambda e: e.tensor_scalar(out=gbc[:], in0=gbc[:], scalar1=1.0 - lam_init, scalar2=None, op0=ALU.mult),
         reads=[gbc], writes=[gbc])
    epsb = k.sb([128, 1])
    k.op("dve", lambda e: e.memset(epsb[:], EPS), writes=[epsb])
    kA = k.sb([96, NK]); kB = k.sb([96, NK])
    vaug = k.sb([128, NKT, 65])
    ta = [k.sb([64, 512]) for _ in range(2)]
    tb = [k.sb([64, 512]) for _ in range(2)]
    tcs = [k.sb([64, 512]) for _ in range(2)]
    tsn = [k.sb([64, 512]) for _ in range(2)]
    qAs = [k.sb([96, 512]) for _ in range(2)]
    qBs = [k.sb([96, 512]) for _ in range(2)]
    Sps = [[k.ps([128, 512]) for _ in range(3)] for _ in range(2)]
    Psb = [[k.sb([128, 512]) for _ in range(3)] for _ in range(2)]
    accp = [k.ps([128, 512]) for _ in range(2)]
    ot = [k.sb([128, 4, 64]) for _ in range(2)]
    o1 = k.sb([128, 4, 64]); o2 = k.sb([128, 4, 64]); osq = k.sb([128, 4, 64])
    r0 = k.sb([128, 4]); r1 = k.sb([128, 4]); ss = k.sb([128, 4])
    vv = g.TM.a[:, 0:384].rearrange("(t p) c -> p t c", p=128)
    dav = g.DA.a.rearrange("(s p) c -> p s c", p=128)
    state = {"i": 0, "q": 0, "o": 0}

    def rope(dst_t, dst_ap, src, ssrc, ct, st, r0_, c0, n):
        i = state["i"] % 2
        state["i"] += 1
        a, b_, c_, s_ = ta[i], tb[i], tcs[i], tsn[i]
        k.dma("sp", a[:, :n], src[r0_:r0_ + 64, c0:c0 + n], reads=[src], writes=[a])
        k.dma("sp", b_[:, :n], ssrc[r0_:r0_ + 64, c0:c0 + n], reads=[ssrc], writes=[b_])
        k.dma("sp", c_[:, :n], ct[0:64, c0:c0 + n], reads=[ct], writes=[c_])
        k.dma("sp", s_[:, :n], st[0:64, c0:c0 + n], reads=[st], writes=[s_])
        k.op("pool", lambda e: e.tensor_tensor(out=a[:, :n], in0=a[:, :n], in1=c_[:, :n], op=ALU.mult),
             reads=[a, c_], writes=[a])
        k.op("pool", lambda e: e.tensor_tensor(out=b_[:, :n], in0=b_[:, :n], in1=s_[:, :n], op=ALU.mult),
             reads=[b_, s_], writes=[b_])
        k.op("pool", lambda e: e.tensor_tensor(out=dst_ap, in0=a[:, :n], in1=b_[:, :n], op=ALU.add),
             reads=[a, b_], writes=[dst_t])

    def replicate(tA, tB, n):
        k.dma("sp", tA[64:96, :n], tA[0:32, :n], reads=[tA], acc=[tA])
        k.dma("sp", tB[0:32, :n], tA[32:64, :n], reads=[tA], writes=[tB])
        k.dma("sp", tB[32:64, :n], tA[0:32, :n], reads=[tA], acc=[tB])
        k.dma("sp", tB[64:96, :n], tA[32:64, :n], reads=[tA], acc=[tB])

    def attend(qA, qB, nq, nkt, out_dram_t, out_ap):
        nsub = nq // 128
        pairs = [(kt, m) for kt in range(nkt) for m in range(2)]
        triples = [pairs[i:i + 3] for i in range(0, len(pairs), 3)]

        def scores(ti):
            st_ = ti % 2
            tk, tq = (kA, qA) if st_ == 0 else (kB, qB)
            for j, (kt, m) in enumerate(triples[ti]):
                sp_, pb = Sps[st_][j], Psb[st_][j]
                k.op("pe", lambda e: e.matmul(sp_[:, :nq], tk[32 * j:32 * j + 32, kt * 128:(kt + 1) * 128],
                                              tq[32 * j:32 * j + 32, :nq], start=True, stop=True),
                     reads=[tk, tq], writes=[sp_])
            for j, (kt, m) in enumerate(triples[ti]):
                sp_, pb = Sps[st_][j], Psb[st_][j]
                k.op("act", lambda e: e.activation(out=pb[:, :nq], in_=sp_[:, :nq], func=AF.Exp, scale=scale),
                     reads=[sp_], writes=[pb])

        def avs(ti):
            st_ = ti % 2
            for j, (kt, m) in enumerate(triples[ti]):
                pb = Psb[st_][j]
                for sub in range(nsub):
                    k.op("pe", lambda e: e.matmul(accp[m][:, sub * 65:(sub + 1) * 65], pb[:, sub * 128:(sub + 1) * 128],
                                                  vaug[:, kt, :], start=(kt == 0 and sub == 0),
                                                  stop=(kt == nkt - 1), skip_group_check=True),
                         reads=[pb, vaug], writes=[accp[m]])
        scores(0)
        for ti in range(len(triples)):
            if ti + 1 < len(triples):
                scores(ti + 1)
            avs(ti)
        a0 = accp[0].a[:, 0:nsub * 65].rearrange("p (s c) -> p s c", c=65)
        a1 = accp[1].a[:, 0:nsub * 65].rearrange("p (s c) -> p s c", c=65)
        o = ot[state["o"] % 2]
        state["o"] += 1
        k.op("dve", lambda e: e.reciprocal(out=r0[:, :nsub], in_=a0[:, :, 64]), reads=[accp[0]], writes=[r0])
        k.op("dve", lambda e: e.reciprocal(out=r1[:, :nsub], in_=a1[:, :, 64]), reads=[accp[1]], writes=[r1])
        k.op("dve", lambda e: e.tensor_scalar(out=r1[:, :nsub], in0=r1[:, :nsub], scalar1=nlam[:], scalar2=None,
                                              op0=ALU.mult), reads=[r1, nlam], writes=[r1])
        k.op("dve", lambda e: e.tensor_tensor(out=o1[:, :nsub, :], in0=a0[:, :, 0:64],
                                              in1=r0[:, :nsub].unsqueeze(2).broadcast_to([128, nsub, 64]), op=ALU.mult),
             reads=[accp[0], r0], writes=[o1])
        k.op("dve", lambda e: e.tensor_tensor(out=o2[:, :nsub, :], in0=a1[:, :, 0:64],
                                              in1=r1[:, :nsub].unsqueeze(2).broadcast_to([128, nsub, 64]), op=ALU.mult),
             reads=[accp[1], r1], writes=[o2])
        k.op("dve", lambda e: e.tensor_tensor(out=o1[:, :nsub, :], in0=o1[:, :nsub, :], in1=o2[:, :nsub, :], op=ALU.add),
             reads=[o1, o2], writes=[o1])
        k.op("pool", lambda e: e.tensor_tensor(out=osq[:, :nsub, :], in0=o1[:, :nsub, :], in1=o1[:, :nsub, :], op=ALU.mult),
             reads=[o1], writes=[osq])
        k.op("dve", lambda e: e.tensor_reduce(out=ss[:, :nsub], in_=osq[:, :nsub, :], op=ALU.add, axis=AX.X),
             reads=[osq], writes=[ss])
        k.op("act", lambda e: e.activation(out=ss[:, :nsub], in_=ss[:, :nsub], func=AF.Sqrt, scale=1.0 / 64, bias=epsb[:]),
             reads=[ss, epsb], writes=[ss])
        k.op("dve", lambda e: e.reciprocal(out=ss[:, :nsub], in_=ss[:, :nsub]), reads=[ss], writes=[ss])
        k.op("dve", lambda e: e.tensor_tensor(out=o1[:, :nsub, :], in0=o1[:, :nsub, :],
                                              in1=ss[:, :nsub].unsqueeze(2).broadcast_to([128, nsub, 64]), op=ALU.mult),
             reads=[o1, ss], writes=[o1])
        k.op("dve", lambda e: e.tensor_tensor(out=o[:, :nsub, :], in0=o1[:, :nsub, :],
                                              in1=gbc[:].unsqueeze(1).broadcast_to([128, nsub, 64]), op=ALU.mult),
             reads=[o1, gbc], writes=[o])
        k.dma("pool", out_ap, o[:, :nsub, :], reads=[o], acc=[out_dram_t])

    for head in range(6):
        for c0 in range(0, NK, 512):
            n = min(512, NK - c0)
            rope(kA, kA[0:64, c0:c0 + n], g.KT_, g.KST_, g.cosk, g.sink, head * 64, c0, n)
        replicate(kA, kB, NK)
        k.op("dve", lambda e: e.memset(vaug[:], 1.0), writes=[vaug])
        for t0 in range(0, NKT, 11):
            t1 = min(NKT, t0 + 11)
            k.dma("sp", vaug[:, t0:t1, 0:64], vv[:, t0:t1, head * 64:(head + 1) * 64], reads=[g.TM], writes=[vaug])
        for qb in range(NQ // 512):
            qA, qB = qAs[state["q"] % 2], qBs[state["q"] % 2]
            state["q"] += 1
            rope(qA, qA[0:64, :512], g.QT_, g.QST_, g.cosq, g.sinq, head * 64, qb * 512, 512)
            replicate(qA, qB, 512)
            attend(qA, qB, 512, NKT, g.DA, dav[:, qb * 4:(qb + 1) * 4, head * 64:(head + 1) * 64])
        if ctx_out:
            qA, qB = qAs[state["q"] % 2], qBs[state["q"] % 2]
            state["q"] += 1
            k.dma("sp", qA[0:64, :NCTX], g.QCT[head * 64:(head + 1) * 64, :], reads=[g.QCT], writes=[qA])
            replicate(qA, qB, NCTX)
            attend(qA, qB, NCTX, NCTX // 128, g.DAC,
                   g.DAC.a.rearrange("(s p) c -> p s c", p=128)[:, :, head * 64:(head + 1) * 64])
    k.stage_end()


def st_mlstm(k, g, L):
    k.stage_begin()

    def load(t):
        s = k.sb(list(t.a.shape))
        k.dma("sp", s[:], t[:], reads=[t], writes=[s])
        return s
    msks = load(g.msk); ident = load(g.idn)
    ones = k.sb([128, 96])
    k.op("dve", lambda e: e.memset(ones[:], 1.0), writes=[ones])
    epsb = k.sb([128, 1])
    k.op("dve", lambda e: e.memset(epsb[:], EPS), writes=[epsb])
    gbs = k.sb([128, 8])
    k.dma("sp", gbs[:], L.gb.a.partition_broadcast(128), reads=[L.gb], writes=[gbs])
    mngs = k.sb([128, 192])
    k.dma("sp", mngs[:], L.mng.a.partition_broadcast(128), reads=[L.mng], writes=[mngs])
    W = NT * 128 + 2
    qc = k.sb([96, W]); kc = k.sb([96, W])
    xin = [k.sb([96, 2050]) for _ in range(2)]
    tmp = [k.sb([96, 2048]) for _ in range(2)]
    cws = k.sb([96, 8])
    vaug = k.sb([128, NT, 97])
    G = k.sb([128, NT, 4]); EB = k.sb([128, NT, 2]); DEC = k.sb([96, NT, 2])
    Hsb = k.sb([128, NT, 96])
    Cst = [k.sb([96, 97]) for _ in range(2)]
    LU = [k.sb([128, 128]) for _ in range(2)]
    DT = [k.sb([128, 128]) for _ in range(2)]
    PT = [k.sb([128, 128]) for _ in range(2)]
    isb = [k.sb([128, 97]) for _ in range(2)]
    nd = [k.sb([128, 97]) for _ in range(2)]
    kw = [k.sb([128, 96]) for _ in range(2)]
    den = [k.sb([128, 1]) for _ in range(2)]
    osb = [k.sb([128, 96]) for _ in range(2)]
    yt = [k.sb([128, 96]) for _ in range(2)]
    hsq = k.sb([128, 96])
    ssn = [k.sb([128, 1]) for _ in range(2)]
    Bps = [k.ps([128, 512]) for _ in range(2)]
    Sps = [k.ps([128, 128]) for _ in range(2)]
    bigps = Bps
    ips = k.ps([128, 97]); nps = k.ps([128, 97]); kps = k.ps([128, 96]); dps = k.ps([96, 97])
    mvv = g.TM.a[:, 384:576].rearrange("(t p) (h c) -> p t h c", p=128, h=2)
    mov = g.TM.a[:, 576:768].rearrange("(t p) (h c) -> p t h c", p=128, h=2)
    gtv = g.TM.a[:, 768:776].rearrange("(t p) (h c) -> p t h c", p=128, h=2)
    cnt = {"i": 0}

    for hd in range(2):
        k.dma("sp", cws[:], L.cw[hd], reads=[L.cw], writes=[cws])
        for (src, dst, o0, scl) in ((g.MQP, qc, 0, None), (g.MKP, kc, 4, 96 ** -0.5)):
            for c0 in range(0, W, 2048):
                n = min(2048, W - c0)
                i = cnt["i"] % 2
                cnt["i"] += 1
                xi, tm = xin[i], tmp[i]
                k.dma("sp", xi[:, :n + 2], src[hd, :, c0:c0 + n + 2], reads=[src], writes=[xi])
                k.op("dve", lambda e: e.tensor_scalar(out=tm[:, :n], in0=xi[:, 0:n], scalar1=cws[:, o0:o0 + 1],
                                                      scalar2=cws[:, o0 + 3:o0 + 4], op0=ALU.mult, op1=ALU.add),
                     reads=[xi, cws], writes=[tm])
                k.op("dve", lambda e: e.scalar_tensor_tensor(out=tm[:, :n], in0=xi[:, 1:n + 1], scalar=cws[:, o0 + 1:o0 + 2],
                                                             in1=tm[:, :n], op0=ALU.mult, op1=ALU.add),
                     reads=[xi, cws, tm], writes=[tm])
                k.op("dve", lambda e: e.scalar_tensor_tensor(out=tm[:, :n], in0=xi[:, 2:n + 2], scalar=cws[:, o0 + 2:o0 + 3],
                                                             in1=tm[:, :n], op0=ALU.mult, op1=ALU.add),
                     reads=[xi, cws, tm], writes=[tm])
                k.op("act", lambda e: e.activation(out=dst[:, c0:c0 + n], in_=tm[:, :n], func=AF.Silu),
                     reads=[tm], writes=[dst])
                if scl is not None:
                    k.op("pool", lambda e: e.tensor_scalar(out=dst[:, c0:c0 + n], in0=dst[:, c0:c0 + n], scalar1=scl,
                                                           scalar2=None, op0=ALU.mult), reads=[dst], writes=[dst])
        k.op("dve", lambda e: e.memset(vaug[:], 1.0), writes=[vaug])
        for t0 in range(0, NT, 11):
            k.dma("sp", vaug[:, t0:t0 + 11, 0:96], mvv[:, t0:t0 + 11, hd, :], reads=[g.TM], writes=[vaug])
        k.dma("sp", G[:], gtv[:, :, hd, :], reads=[g.TM], writes=[G])
        k.op("dve", lambda e: e.tensor_tensor(out=G[:], in0=G[:], in1=gbs[:, hd * 4:(hd + 1) * 4].unsqueeze(1).broadcast_to([128, NT, 4]),
                                              op=ALU.add), reads=[G, gbs], writes=[G])
        Gd = G.a.rearrange("p t (d y) -> p t d y", d=2)
        k.op("act", lambda e: e.activation(out=Gd[:, :, :, 1], in_=Gd[:, :, :, 1], func=AF.Exp, scale=-1.0), reads=[G], writes=[G])
        k.op("act", lambda e: e.activation(out=Gd[:, :, :, 1], in_=Gd[:, :, :, 1], func=AF.Ln, bias=1.0), reads=[G], writes=[G])
        k.op("dve", lambda e: e.tensor_scalar(out=Gd[:, :, :, 1], in0=Gd[:, :, :, 1], scalar1=-1.0, scalar2=None, op0=ALU.mult),
             reads=[G], writes=[G])
        G2 = G.a.rearrange("p t c -> p (t c)")
        for dr in range(2):
            k.op("pe", lambda e: e.matmul(bigps[dr][:, 0:NT * 4], msks[:, 1 + 3 * dr, :], G2, start=True, stop=True),
                 reads=[msks, G], writes=[bigps[dr]])
            bv = bigps[dr].a[:, 0:NT * 4].rearrange("p (t c) -> p t c", c=4)
            k.op("act", lambda e: e.activation(out=EB[:, :, dr], in_=bv[:, :, 2 * dr + 1], func=AF.Exp),
                 reads=[bigps[dr]], writes=[EB])
        k.op("pe", lambda e: e.matmul(bigps[0][0:96, 0:NT * 4], ones[:], G2, start=True, stop=True),
             reads=[ones, G, EB], writes=[bigps[0]])
        bv = bigps[0].a[0:96, 0:NT * 4].rearrange("p (t d y) -> p t d y", d=2, y=2)
        k.op("act", lambda e: e.activation(out=DEC[:], in_=bv[:, :, :, 1], func=AF.Exp), reads=[bigps[0]], writes=[DEC])

        for dr in range(2):
            order = list(range(NT)) if dr == 0 else [1, 0] + list(range(NT - 1, 1, -1))
            U, TR, MK = msks.a[:, 3 * dr, :], msks.a[:, 3 * dr + 1, :], msks.a[:, 3 * dr + 2, :]
            last = 127 if dr == 0 else 0
            cs = Cst[0]
            k.op("dve", lambda e: e.memset(cs[:], 0.0), writes=[cs])
            for si, t in enumerate(order):
                b2 = si % 2
                c0 = mcol(t)
                lf = G.a[:, t, 2 * dr + 1:2 * dr + 2]
                li = G.a[:, t, 2 * dr:2 * dr + 1]
                lu, dt_, pt, bp, sp_ = LU[b2], DT[b2], PT[b2], Bps[b2], Sps[b2]
                k.op("pool", lambda e: e.tensor_scalar(out=lu[:], in0=U, scalar1=lf, scalar2=None, op0=ALU.mult),
                     reads=[msks, G], writes=[lu])
                k.op("pe", lambda e: e.matmul(bp[:, 0:128], lu[:], TR, start=True, stop=False), reads=[lu, msks], writes=[bp])
                k.op("pe", lambda e: e.matmul(bp[:, 0:128], ident[:], MK, start=False, stop=True), reads=[ident, msks], writes=[bp])
                k.op("act", lambda e: e.activation(out=dt_[:], in_=bp[:, 0:128], func=AF.Exp, bias=li), reads=[bp, G], writes=[dt_])
                k.op("pe", lambda e: e.matmul(sp_[:], kc[:, c0:c0 + 128], qc[:, c0:c0 + 128], start=True, stop=True),
                     reads=[kc, qc], writes=[sp_])
                k.op("dve", lambda e: e.tensor_tensor(out=pt[:], in0=sp_[:], in1=dt_[:], op=ALU.mult),
                     reads=[sp_, dt_], writes=[pt])
                k.op("pe", lambda e: e.matmul(ips[:], pt[:], vaug[:, t, :], start=True, stop=True), reads=[pt, vaug], writes=[ips])
                k.op("pe", lambda e: e.matmul(nps[:], qc[:, c0:c0 + 128], cs[:], start=True, stop=True),
                     reads=[qc, cs], writes=[nps])
                ib, ndb, dn = isb[b2], nd[b2], den[b2]
                k.op("dve", lambda e: e.tensor_copy(out=ib[:], in_=ips[:]), reads=[ips], writes=[ib])
                k.op("dve", lambda e: e.scalar_tensor_tensor(out=ndb[:], in0=nps[:], scalar=EB[:, t, dr:dr + 1], in1=ib[:],
                                                             op0=ALU.mult, op1=ALU.add), reads=[nps, EB, ib], writes=[ndb])
                k.op("dve", lambda e: e.tensor_scalar(out=dn[:], in0=ndb[:, 96:97], scalar1=1.0, scalar2=None, op0=ALU.max),
                     reads=[ndb], writes=[dn])
                k.op("dve", lambda e: e.scalar_tensor_tensor(out=dn[:], in0=ndb[:, 96:97], scalar=-1.0, in1=dn[:],
                                                             op0=ALU.mult, op1=ALU.max), reads=[ndb, dn], writes=[dn])
                k.op("dve", lambda e: e.reciprocal(out=dn[:], in_=dn[:]), reads=[dn], writes=[dn])
                if dr == 0:
                    k.op("dve", lambda e: e.tensor_scalar(out=Hsb[:, t, :], in0=ndb[:, 0:96], scalar1=dn[:], scalar2=None,
                                                          op0=ALU.mult), reads=[ndb, dn], writes=[Hsb])
                else:
                    y, ob_, s1 = yt[b2], osb[b2], ssn[b2]
                    k.dma("sp", ob_[:], mov[:, t, hd, :], reads=[g.TM], writes=[ob_])
                    k.op("dve", lambda e: e.scalar_tensor_tensor(out=y[:], in0=ndb[:, 0:96], scalar=dn[:], in1=Hsb[:, t, :],
                                                                 op0=ALU.mult, op1=ALU.add), reads=[ndb, dn, Hsb], writes=[y])
                    k.op("dve", lambda e: e.tensor_tensor(out=hsq[:], in0=y[:], in1=y[:], op=ALU.mult), reads=[y], writes=[hsq])
                    k.op("dve", lambda e: e.tensor_reduce(out=s1[:], in_=hsq[:], op=ALU.add, axis=AX.X), reads=[hsq], writes=[s1])
                    k.op("act", lambda e: e.activation(out=s1[:], in_=s1[:], func=AF.Ln, scale=1.0 / 96, bias=epsb[:]),
                         reads=[s1, epsb], writes=[s1])
                    k.op("act", lambda e: e.activation(out=s1[:], in_=s1[:], func=AF.Exp, scale=-0.5), reads=[s1], writes=[s1])
                    k.op("dve", lambda e: e.scalar_tensor_tensor(out=y[:], in0=y[:], scalar=s1[:], in1=mngs[:, hd * 96:(hd + 1) * 96],
                                                                 op0=ALU.mult, op1=ALU.mult), reads=[y, s1, mngs], writes=[y])
                    k.op("act", lambda e: e.activation(out=ob_[:], in_=ob_[:], func=AF.Exp, scale=-1.0), reads=[ob_], writes=[ob_])
                    k.op("pool", lambda e: e.tensor_scalar(out=ob_[:], in0=ob_[:], scalar1=1.0, scalar2=None, op0=ALU.add),
                         reads=[ob_], writes=[ob_])
                    k.op("dve", lambda e: e.reciprocal(out=ob_[:], in_=ob_[:]), reads=[ob_], writes=[ob_])
                    k.op("dve", lambda e: e.tensor_tensor(out=y[:], in0=y[:], in1=ob_[:], op=ALU.mult), reads=[y, ob_], writes=[y])
                    cc = 128 + hd * 96
                    if t < 2:
                        k.dma("pool", g.CMINE[t * 128:(t + 1) * 128, cc:cc + 96], y[:], reads=[y], acc=[g.CMINE])
                    else:
                        k.dma("pool", g.SEND[(t - 2) * 128:(t - 1) * 128, cc:cc + 96], y[:], reads=[y], acc=[g.SEND])
                kwb = kw[b2]
                k.op("pe", lambda e: e.transpose(kps[:], kc[:, c0:c0 + 128], ident[0:96, 0:96]), reads=[kc, ident], writes=[kps])
                k.op("dve", lambda e: e.tensor_scalar(out=kwb[:], in0=kps[:], scalar1=dt_[:, last:last + 1], scalar2=None,
                                                      op0=ALU.mult), reads=[kps, dt_], writes=[kwb])
                k.op("pe", lambda e: e.matmul(dps[:], kwb[:], vaug[:, t, :], start=True, stop=True), reads=[kwb, vaug], writes=[dps])
                cn = Cst[(si + 1) % 2]
                k.op("dve", lambda e: e.scalar_tensor_tensor(out=cn[:], in0=cs[:], scalar=DEC[:, t, dr:dr + 1], in1=dps[:],
                                                             op0=ALU.mult, op1=ALU.add), reads=[cs, DEC, dps], writes=[cn])
                cs = cn
    k.stage_end()


def st_fourier(k, g, L, ctx_out):
    k.stage_begin()

    def load(t):
        s = k.sb(list(t.a.shape))
        k.dma("sp", s[:], t[:], reads=[t], writes=[s])
        return s
    fws = load(L.fwblk); ccbs = load(g.ccb); cs128s = load(g.cs128); tws = load(g.tw); cs64s = load(g.cs64)
    AB = k.sb([128, 256])
    yps = [k.ps([128, 512]) for _ in range(2)]
    pab = yps[0]
    for i in range(2):
        k.op("pe", lambda e: e.matmul(pab[:, i * 128:(i + 1) * 128], ccbs[:, i * 128:(i + 1) * 128], fws[:],
                                      start=True, stop=True), reads=[ccbs, fws], writes=[pab])
    k.op("dve", lambda e: e.tensor_copy(out=AB[:], in_=pab[:, 0:256]), reads=[pab], writes=[AB])
    xs = k.sb([128, S])
    for i in range(4):
        k.dma("sp", xs[:, i * 2048:(i + 1) * 2048], g.FT_[:, NCTX + i * 2048:NCTX + (i + 1) * 2048], reads=[g.FT_], writes=[xs])
    Y = k.sb([128, 64, 256])
    xv = xs.a.rearrange("p (n2 n1) -> p n1 n2", n1=64)
    for i in range(32):
        yp = yps[i % 2]
        for j in range(2):
            n1 = 2 * i + j
            k.op("pe", lambda e: e.matmul(yp[:, j * 256:(j + 1) * 256], xv[:, n1, :], AB[:], start=True, stop=True),
                 reads=[xs, AB], writes=[yp])
        k.evac(Y, Y[:, 2 * i:2 * i + 2, :].rearrange("p a b -> p (a b)"), yp, yp[:])
    zps = [k.ps([64, 512]) for _ in range(4)]
    zr = [k.sb([64, 4, 128]) for _ in range(2)]
    zi = [k.sb([64, 4, 128]) for _ in range(2)]
    t1 = k.sb([64, 2, 128]); t2 = k.sb([64, 2, 128])
    ops_ = [k.ps([128, 256]) for _ in range(2)]
    FTM = k.sb([128, 64, 128])
    tcb = tws.a[:, 0:128].unsqueeze(1).broadcast_to([64, 2, 128])
    tsb = tws.a[:, 128:256].unsqueeze(1).broadcast_to([64, 2, 128])
    for blk in range(32):
        zrb, zib = zr[blk % 2], zi[blk % 2]
        for hb in range(2):
            zp = zps[(2 * blk + hb) % 4]
            for j in range(2):
                d = blk * 4 + hb * 2 + j
                k.op("pe", lambda e: e.matmul(zp[:, j * 256:(j + 1) * 256], Y[:, :, d], cs128s[:, 0:256],
                                              start=True, stop=False), reads=[Y, cs128s], writes=[zp])
                k.op("pe", lambda e: e.matmul(zp[:, j * 256:(j + 1) * 256], Y[:, :, 128 + d], cs128s[:, 256:512],
                                              start=False, stop=True), reads=[Y, cs128s], writes=[zp])
            zv = zp.a.rearrange("p (d c m) -> p d c m", d=2, c=2)
            sl = slice(hb * 2, hb * 2 + 2)
            k.op("dve", lambda e: e.tensor_tensor(out=t1[:], in0=zv[:, :, 0, :], in1=tcb, op=ALU.mult),
                 reads=[zp, tws], writes=[t1])
            k.op("dve", lambda e: e.tensor_tensor(out=t2[:], in0=zv[:, :, 1, :], in1=tsb, op=ALU.mult),
                 reads=[zp, tws], writes=[t2])
            k.op("pool", lambda e: e.tensor_tensor(out=zrb[:, sl, :], in0=t1[:], in1=t2[:], op=ALU.subtract),
                 reads=[t1, t2], writes=[zrb])
            k.op("dve", lambda e: e.tensor_tensor(out=t1[:], in0=zv[:, :, 0, :], in1=tsb, op=ALU.mult),
                 reads=[zp, tws], writes=[t1])
            k.op("dve", lambda e: e.tensor_tensor(out=t2[:], in0=zv[:, :, 1, :], in1=tcb, op=ALU.mult),
                 reads=[zp, tws], writes=[t2])
            k.op("pool", lambda e: e.tensor_tensor(out=zib[:, sl, :], in0=t1[:], in1=t2[:], op=ALU.add),
                 reads=[t1, t2], writes=[zib])
        op_ = ops_[blk % 2]
        for j in range(4):
            k.op("pe", lambda e: e.matmul(op_[:, j * 64:(j + 1) * 64], zrb[:, j, :], cs64s[:, 0:64], start=True, stop=False),
                 reads=[cs64s, zrb], writes=[op_])
            k.op("pe", lambda e: e.matmul(op_[:, j * 64:(j + 1) * 64], zib[:, j, :], cs64s[:, 64:128], start=False, stop=True),
                 reads=[cs64s, zib], writes=[op_])
        k.evac(FTM, FTM[:, :, blk * 4:(blk + 1) * 4], op_, op_.a.rearrange("p (d m) -> p m d", d=4))
    sv = g.SEND.a[:, 0:128].rearrange("(m1 m2) d -> m2 m1 d", m2=128)
    for q4 in range(4):
        k.dma("pool", sv[:, q4 * 16:(q4 + 1) * 16, :], FTM[:, q4 * 16:(q4 + 1) * 16, :], reads=[FTM], acc=[g.SEND])
    if ctx_out:
        xcs = k.sb([128, NCTX])
        k.dma("sp", xcs[:], g.FT_[:, 0:NCTX], reads=[g.FT_], writes=[xcs])
        dfs = load(g.dftc)
        Yc = k.sb([128, 2, 256])
        for t in range(2):
            yp = yps[t % 2]
            k.op("pe", lambda e: e.matmul(yp[:, 0:256], xcs[:, t * 128:(t + 1) * 128], AB[:], start=True, stop=True),
                 reads=[xcs, AB], writes=[yp])
            k.evac(Yc, Yc[:, t, :], yp, yp[:, 0:256])
        for mt in range(2):
            pv = yps[mt % 2]
            i = 0
            for t in range(2):
                for c in range(2):
                    k.op("pe", lambda e: e.matmul(pv[:, 0:128], dfs[:, t, c * 256 + mt * 128:c * 256 + (mt + 1) * 128],
                                                  Yc[:, t, c * 128:(c + 1) * 128], start=(i == 0), stop=(i == 3)),
                         reads=[dfs, Yc], writes=[pv])
                    i += 1
            oc = k.sb([128, 128])
            k.evac(oc, oc[:], pv, pv[:, 0:128])
            k.dma("pool", g.CMINE[mt * 128:(mt + 1) * 128, 0:128], oc[:], reads=[oc], acc=[g.CMINE])
    k.stage_end()


def st_p3(k, g, L, ctx_out):
    k.stage_begin()

    def load(t):
        s = k.sb(list(t.a.shape))
        k.dma("sp", s[:], t[:], reads=[t], writes=[s])
        return s
    modT = g.modT
    g2s = load(L.g2); wrs = load(L.wr); ident = load(g.idn); hs = load(g.hsel)
    wos = k.sb([128, 8, 8, 128])
    for j in range(8):
        k.dma("sp", wos[:, j], L.wo[j], reads=[L.wo], writes=[wos])
    ones = k.sb([128, 128])
    k.op("dve", lambda e: e.memset(ones[:], 1.0), writes=[ones])
    epsb = k.sb([128, 1])
    k.op("dve", lambda e: e.memset(epsb[:], EPS), writes=[epsb])
    a2 = k.sb([128, 8, 2])
    k.op("dve", lambda e: e.tensor_scalar(out=a2[:], in0=modT[:, 32:40, :], scalar1=1.0, scalar2=None, op0=ALU.add),
         reads=[modT], writes=[a2])
    k.op("dve", lambda e: e.tensor_tensor(out=a2[:], in0=a2[:], in1=g2s[:].unsqueeze(2).broadcast_to([128, 8, 2]),
                                          op=ALU.mult), reads=[a2, g2s], writes=[a2])
    chunks = [(i * 512, 512, 0) for i in range(TL // 512)]
    if ctx_out:
        chunks.append((TL, NCTX, 1))
    mb = k.sb([128, 8, 512])
    xb = [k.sb([128, 8, 512]) for _ in range(2)]
    x1 = k.sb([128, 8, 512])
    h2 = k.sb([128, 8, 512])
    sq = k.sb([128, 8, 512])
    TA = [k.sb([128, 2, 320]) for _ in range(2)]
    TB = [k.sb([128, 2, 320]) for _ in range(2)]
    MT = [k.sb([128, D]) for _ in range(2)]
    H2T = [k.sb([128, D]) for _ in range(2)]
    pps = [k.ps([128, 512]) for _ in range(2)]
    tps = [k.ps([128, 512]) for _ in range(2)]
    ssp = k.ps([128, 512])
    rs = k.sb([128, 512])
    lps = [k.ps([128, 16]) for _ in range(2)]
    mx = k.sb([128, 1]); sm = k.sb([128, 1]); ex = k.sb([128, 16])
    pb = [k.sb([128, 16]) for _ in range(2)]
    xov = g.xown.a.rearrange("(f p) t -> p f t", p=128)
    xcv = g.xc.a.rearrange("(f p) t -> p f t", p=128)
    x1v = g.X1T.a.rearrange("(f p) t -> p f t", p=128)
    cnt = {"s": 0, "p": 0, "t": 0}
    pieces = [(0, 0, 128, 0), (1, 0, 128, 128), (0, 128, 320, 640), (1, 128, 320, 832)]
    for ci, (t0, n, col) in enumerate(chunks):
        x_ = xb[ci % 2]
        k.dma("sp", x_[:, :, :n], (xov[:, :, t0:t0 + n] if col == 0 else xcv[:, :, :]), reads=[g.xown if col == 0 else g.xc],
              writes=[x_])
        for sub in range(n // 128):
            i = cnt["s"] % 2
            cnt["s"] += 1
            ta, tb, mt = TA[i], TB[i], MT[i]
            r0 = t0 + sub * 128
            if col == 0:
                for r in range(2):
                    ra, rb = r0, TL + r0
                    k.dma("sp", ta[:, r, :], g.MG[ra // 1024, r, ra % 1024:ra % 1024 + 128, :], reads=[g.MG], writes=[ta] if r == 0 else [], acc=[] if r == 0 else [ta])
                    k.dma("sp", tb[:, r, :], g.MG[rb // 1024, r, rb % 1024:rb % 1024 + 128, :], reads=[g.MG], writes=[tb] if r == 0 else [], acc=[] if r == 0 else [tb])
                k.dma("sp", mt[:, 256:640], g.DA[r0:r0 + 128, :], reads=[g.DA], writes=[mt])
                k.op("pool", lambda e: e.tensor_tensor(out=tb[:], in0=tb[:], in1=ta[:], op=ALU.subtract), reads=[ta, tb], writes=[tb])
                for (r, c0, c1, dc) in pieces:
                    k.op("dve", lambda e: e.scalar_tensor_tensor(out=mt[:, dc:dc + c1 - c0], in0=tb[:, r, c0:c1], scalar=hs[:],
                                                                 in1=ta[:, r, c0:c1], op0=ALU.mult, op1=ALU.add),
                         reads=[tb, hs, ta], writes=[mt])
            else:
                rr = sub * 128
                for (r, c0, c1, dc) in pieces:
                    k.dma("sp", mt[:, dc:dc + c1 - c0], g.CG[r, rr:rr + 128, c0:c1], reads=[g.CG], writes=[mt])
                k.dma("sp", mt[:, 256:640], g.DAC[rr:rr + 128, :], reads=[g.DAC], writes=[mt])
            for hf in range(2):
                tp = tps[cnt["t"] % 2]
                cnt["t"] += 1
                for j in range(4):
                    c = hf * 4 + j
                    k.op("pe", lambda e: e.transpose(tp[:, j * 128:(j + 1) * 128], mt[:, c * 128:(c + 1) * 128], ident[:]),
                         reads=[mt, ident], writes=[tp])
                k.evac(mb, mb[:, hf * 4:hf * 4 + 4, sub * 128:(sub + 1) * 128], tp, tp[:].rearrange("p (a b) -> p a b", a=4))
        for j in range(8):
            pp = pps[cnt["p"] % 2]
            cnt["p"] += 1
            for f in range(8):
                k.op("pe", lambda e: e.matmul(pp[:, :n], wos[:, j, f, :], mb[:, f, :n], start=(f == 0), stop=(f == 7)),
                     reads=[wos, mb], writes=[pp])
            k.op("dve", lambda e: e.scalar_tensor_tensor(out=x1[:, j, :n], in0=pp[:, :n], scalar=modT[:, 16 + j, col:col + 1],
                                                         in1=x_[:, j, :n], op0=ALU.mult, op1=ALU.add),
                 reads=[pp, modT, x_], writes=[x1])
        k.dma("pool", x1v[:, :, t0:t0 + n], x1[:, :, :n], reads=[x1], acc=[g.X1T])
        k.op("act", lambda e: e.activation(out=sq[:, :, :n], in_=x1[:, :, :n], func=AF.Square), reads=[x1], writes=[sq])
        for f in range(8):
            k.op("pe", lambda e: e.matmul(ssp[:, :n], ones[:], sq[:, f, :n], start=(f == 0), stop=(f == 7)),
                 reads=[ones, sq], writes=[ssp])
        k.op("act", lambda e: e.activation(out=rs[:, :n], in_=ssp[:, :n], func=AF.Sqrt, scale=1.0 / D, bias=epsb[:]),
             reads=[ssp, epsb], writes=[rs])
        k.op("dve", lambda e: e.reciprocal(out=rs[:, :n], in_=rs[:, :n]), reads=[rs], writes=[rs])
        for f in range(8):
            k.op("dve", lambda e: e.scalar_tensor_tensor(out=h2[:, f, :n], in0=x1[:, f, :n], scalar=a2[:, f, col:col + 1],
                                                         in1=rs[:, :n], op0=ALU.mult, op1=ALU.mult),
                 reads=[x1, a2, rs], writes=[h2])
            k.op("act", lambda e: e.activation(out=h2[:, f, :n], in_=h2[:, f, :n], func=AF.Identity,
                                               bias=modT[:, 24 + f, col:col + 1]), reads=[h2, modT], writes=[h2])
        for st in range(n // 128):
            ht = H2T[st % 2]
            for hf in range(2):
                tp = tps[cnt["t"] % 2]
                cnt["t"] += 1
                for j in range(4):
                    f = hf * 4 + j
                    k.op("pe", lambda e: e.transpose(tp[:, j * 128:(j + 1) * 128], h2[:, f, st * 128:(st + 1) * 128], ident[:]),
                         reads=[h2, ident], writes=[tp])
                k.evac(ht, ht[:, hf * 512:(hf + 1) * 512], tp, tp[:])
            if col == 0:
                k.dma("pool", g.H2SEND[t0 + st * 128:t0 + (st + 1) * 128, :], ht[:], reads=[ht], acc=[g.H2SEND])
            else:
                k.dma("pool", g.H2C[st * 128:(st + 1) * 128, :], ht[:], reads=[ht], acc=[g.H2C])
            lp = lps[st % 2]
            p_ = pb[st % 2]
            for f in range(8):
                k.op("pe", lambda e: e.matmul(lp[:], h2[:, f, st * 128:(st + 1) * 128], wrs[:, f, :], start=(f == 0), stop=(f == 7)),
                     reads=[h2, wrs], writes=[lp])
            k.op("dve", lambda e: e.tensor_reduce(out=mx[:], in_=lp[:], op=ALU.max, axis=AX.X, negate=True),
                 reads=[lp], writes=[mx])
            k.op("act", lambda e: e.activation(out=ex[:], in_=lp[:], func=AF.Exp, bias=mx[:], accum_out=sm[:]),
                 reads=[lp, mx], writes=[ex, sm])
            k.op("dve", lambda e: e.reciprocal(out=sm[:], in_=sm[:]), reads=[sm], writes=[sm])
            k.op("dve", lambda e: e.tensor_scalar(out=p_[:], in0=ex[:], scalar1=sm[:], scalar2=None, op0=ALU.mult),
                 reads=[ex, sm], writes=[p_])
            if col == 0:
                k.dma("pool", g.PRSEND[t0 + st * 128:t0 + (st + 1) * 128, :], p_[:], reads=[p_], acc=[g.PRSEND])
            else:
                k.dma("pool", g.PRC[st * 128:(st + 1) * 128, :], p_[:], reads=[p_], acc=[g.PRC])
    k.stage_end()


def st_moe(k, g, L, ctx_out):
    k.stage_begin()
    NL, NC_ = S, (NCTX if ctx_out else 0)
    capL, capC = 2 * NL // NEXP, 2 * NC_ // NEXP
    NSLOT = capL + (128 if NC_ else 0)
    NST = NSLOT // 128
    xs, ys = g.xs, g.ys

    def load(t):
        s = k.sb(list(t.a.shape))
        k.dma("sp", s[:], t[:], reads=[t], writes=[s])
        return s
    lsts = load(g.lst); ident = load(g.idn); hs = load(g.hsel)
    ones = k.sb([128, 128])
    k.op("dve", lambda e: e.memset(ones[:], 1.0), writes=[ones])
    banks = [k.ps([128, 512]) for _ in range(8)]
    breg = g.breg[NSLOT]

    def route(prt, N, cap, base):
        ntl = N // 128
        pr16 = k.sb([128, ntl, 16])
        prs = k.sb([128, ntl, 8]); cmp_ = k.sb([128, ntl, 8]); mask = k.sb([128, ntl, 8]); gate = k.sb([128, ntl, 8])
        off = k.sb([128, ntl, 8]); idxf = k.sb([128, ntl, 8]); idx = k.sb([128, ntl, 8], I32)
        lo = k.sb([128, 8]); mid = k.sb([128, 8]); cpart = k.sb([128, 8]); sel = k.sb([128, 8])
        k.dma("sp", pr16[:], prt.a.rearrange("(t p) e -> p t e", p=128), reads=[prt], writes=[pr16])
        k.op("dve", lambda e: e.tensor_tensor(out=cmp_[:], in0=pr16[:, :, 8:16], in1=pr16[:, :, 0:8], op=ALU.subtract),
             reads=[pr16], writes=[cmp_])
        k.op("dve", lambda e: e.scalar_tensor_tensor(out=prs[:], in0=cmp_[:], scalar=hs[:], in1=pr16[:, :, 0:8],
                                                     op0=ALU.mult, op1=ALU.add), reads=[cmp_, hs, pr16], writes=[prs])
        k.op("dve", lambda e: e.memset(lo[:], 0.0), writes=[lo])
        tot = banks[6]
        for it in range(NBIS):
            w = 2.0 ** -(it + 1)
            k.op("dve", lambda e: e.tensor_scalar(out=mid[:], in0=lo[:], scalar1=w, scalar2=None, op0=ALU.add),
                 reads=[lo], writes=[mid])
            k.op("dve", lambda e: e.tensor_tensor(out=cmp_[:], in0=prs[:], in1=mid[:].unsqueeze(1).broadcast_to([128, ntl, 8]),
                                                  op=ALU.is_gt), reads=[prs, mid], writes=[cmp_])
            k.op("dve", lambda e: e.tensor_reduce(out=cpart[:], in_=cmp_[:].rearrange("p t e -> p e t"), op=ALU.add, axis=AX.X),
                 reads=[cmp_], writes=[cpart])
            k.op("pe", lambda e: e.matmul(tot[:, 0:8], ones[:], cpart[:], start=True, stop=True), reads=[ones, cpart], writes=[tot])
            k.op("dve", lambda e: e.tensor_scalar(out=sel[:], in0=tot[:, 0:8], scalar1=cap - 0.5, scalar2=w, op0=ALU.is_ge,
                                                  op1=ALU.mult), reads=[tot], writes=[sel])
            k.op("dve", lambda e: e.tensor_tensor(out=lo[:], in0=lo[:], in1=sel[:], op=ALU.add), reads=[lo, sel], writes=[lo])
        k.op("dve", lambda e: e.tensor_tensor(out=mask[:], in0=prs[:], in1=lo[:].unsqueeze(1).broadcast_to([128, ntl, 8]),
                                              op=ALU.is_gt), reads=[prs, lo], writes=[mask])
        k.op("dve", lambda e: e.tensor_tensor(out=gate[:], in0=prs[:], in1=mask[:], op=ALU.mult), reads=[prs, mask], writes=[gate])
        m2 = mask.a.rearrange("p t e -> p (t e)")
        pre, cnt = banks[6], banks[7]
        k.op("pe", lambda e: e.matmul(pre[:, 0:ntl * 8], lsts[:], m2, start=True, stop=True), reads=[lsts, mask], writes=[pre])
        k.op("pe", lambda e: e.matmul(cnt[:, 0:ntl * 8], ones[:], m2, start=True, stop=True), reads=[ones, mask], writes=[cnt])
        cv = cnt.a[:, 0:ntl * 8].rearrange("p (t e) -> p t e", e=8)
        pv = pre.a[:, 0:ntl * 8].rearrange("p (t e) -> p t e", e=8)
        k.op("dve", lambda e: e.memset(off[:, 0, :], float(base)), writes=[off])
        for i in range(ntl - 1):
            k.op("dve", lambda e: e.tensor_tensor(out=off[:, i + 1, :], in0=cv[:, i, :], in1=off[:, i, :], op=ALU.add),
                 reads=[cnt, off], writes=[off])
        k.op("dve", lambda e: e.tensor_tensor(out=off[:], in0=pv, in1=off[:], op=ALU.add), reads=[pre, off], writes=[off])
        k.op("dve", lambda e: e.tensor_scalar(out=idxf[:], in0=mask[:], scalar1=-8192.0, scalar2=8192.0, op0=ALU.mult, op1=ALU.add),
             reads=[mask], writes=[idxf])
        k.op("dve", lambda e: e.tensor_tensor(out=idxf[:], in0=idxf[:], in1=off[:], op=ALU.add), reads=[idxf, off], writes=[idxf])
        k.op("dve", lambda e: e.tensor_copy(out=idx[:], in_=idxf[:]), reads=[idxf], writes=[idx])
        return idx, gate, ntl

    sets = [(g.H2ALL, g.PRALL, NL, capL, 0, g.PART)]
    if NC_:
        sets.append((g.H2C, g.PRC, NC_, capC, capL, g.PARTC))
    stage = [k.sb([128, D]) for _ in range(2)]
    routed = []
    ns = 0
    for (ht, prt, N, cap, base, po) in sets:
        idx, gate, ntl = route(prt, N, cap, base)
        routed.append((idx, gate, ntl, po))
        for i in range(ntl):
            st_ = stage[ns % 2]
            ns += 1
            if ht is g.H2ALL:
                rr_, tt_ = (i * 128) // TL, (i * 128) % TL
                k.dma("sp", st_[:], ht[tt_ // 256, rr_, tt_ % 256:tt_ % 256 + 128, :], reads=[ht], writes=[st_])
            else:
                k.dma("sp", st_[:], ht[i * 128:(i + 1) * 128, :], reads=[ht], writes=[st_])
            for e_ in range(8):
                k.dma("pool", None, None, reads=[st_, idx], acc=[xs[e_]],
                      fn=lambda eng: eng.indirect_dma_start(
                          out=xs[e_][0:NSLOT, :], out_offset=bass.IndirectOffsetOnAxis(ap=idx[:, i, e_:e_ + 1], axis=0),
                          in_=st_[:, :], in_offset=None, bounds_check=breg, oob_is_err=False))

    xsT = k.sb([128, 8, NSLOT]); hid = k.sb([128, 16, NSLOT])
    w1b = [k.sb([128, 8, 128]) for _ in range(2)]; w3b = [k.sb([128, 8, 128]) for _ in range(2)]
    w2b = [k.sb([128, 16, 512]) for _ in range(1)]
    sil = [k.sb([128, 512]) for _ in range(2)]
    yb = [k.sb([128, 512]) for _ in range(2)]
    pieces = [(c0, min(512, NSLOT - c0)) for c0 in range(0, NSLOT, 512)]
    cnt = {"w": 0, "p": 0, "y": 0}
    for e_ in range(8):
        for st in range(NST):
            sg = stage[ns % 2]
            ns += 1
            k.dma("sp", sg[:], xs[e_][st * 128:(st + 1) * 128, :], reads=[xs[e_]], writes=[sg])
            for hf in range(2):
                tp = banks[hf]
                for j in range(4):
                    kc = hf * 4 + j
                    k.op("pe", lambda e: e.transpose(tp[:, j * 128:(j + 1) * 128], sg[:, kc * 128:(kc + 1) * 128], ident[:]),
                         reads=[sg, ident], writes=[tp])
                k.evac(xsT, xsT[:, hf * 4:hf * 4 + 4, st * 128:(st + 1) * 128], tp, tp[:].rearrange("p (a b) -> p a b", a=4))
        for fg in range(16):
            wb1, wb3 = w1b[cnt["w"] % 2], w3b[cnt["w"] % 2]
            cnt["w"] += 1
            k.dma("sp", wb1[:], L.w1c[e_, fg], reads=[L.w1c], writes=[wb1])
            k.dma("sp", wb3[:], L.w3c[e_, fg], reads=[L.w3c], writes=[wb3])
            for (c0, n) in pieces:
                p1, p3 = banks[2 + cnt["p"] % 2], banks[4 + cnt["p"] % 2]
                sl_ = sil[cnt["p"] % 2]
                cnt["p"] += 1
                for kc in range(8):
                    k.op("pe", lambda e: e.matmul(p1[:, :n], wb1[:, kc, :], xsT[:, kc, c0:c0 + n], start=(kc == 0), stop=(kc == 7)),
                         reads=[wb1, xsT], writes=[p1])
                for kc in range(8):
                    k.op("pe", lambda e: e.matmul(p3[:, :n], wb3[:, kc, :], xsT[:, kc, c0:c0 + n], start=(kc == 0), stop=(kc == 7)),
                         reads=[wb3, xsT], writes=[p3])
                k.op("act", lambda e: e.activation(out=sl_[:, :n], in_=p1[:, :n], func=AF.Silu), reads=[p1], writes=[sl_])
                k.op("dve", lambda e: e.tensor_tensor(out=hid[:, fg, c0:c0 + n], in0=p3[:, :n], in1=sl_[:, :n], op=ALU.mult),
                     reads=[p3, sl_], writes=[hid])
        for dh in range(2):
            wb2 = w2b[0]
            for q8 in range(8):
                dq, q4 = 2 * dh + q8 // 4, q8 % 4
                k.dma("sp", wb2[:, q4 * 4:(q4 + 1) * 4, (q8 // 4) * 256:(q8 // 4 + 1) * 256],
                      L.w2c[e_, dq, :, q4 * 4:(q4 + 1) * 4, :], reads=[L.w2c], writes=[wb2] if q8 == 0 else [],
                      acc=[] if q8 == 0 else [wb2])
            for st in range(NST):
                po_ = banks[cnt["y"] % 2]
                y_ = yb[cnt["y"] % 2]
                cnt["y"] += 1
                for fg in range(16):
                    k.op("pe", lambda e: e.matmul(po_[:, 0:512], hid[:, fg, st * 128:(st + 1) * 128], wb2[:, fg, :],
                                                  start=(fg == 0), stop=(fg == 15)), reads=[hid, wb2], writes=[po_])
                k.evac(y_, y_[:], po_, po_[:, 0:512])
                k.dma("pool", ys[e_][st * 128:(st + 1) * 128, dh * 512:(dh + 1) * 512], y_[:], reads=[y_], acc=[ys[e_]])

    accb = [k.sb([128, D]) for _ in range(2)]
    stage = stage + [k.sb([128, D]) for _ in range(1)]
    for s_ in stage:
        k.op("dve", lambda e: e.memset(s_[:], 0.0), writes=[s_])
    na = 0
    for (idx, gate, ntl, po) in routed:
        for i in range(ntl):
            ac = accb[na % 2]
            na += 1
            for e_ in range(8):
                g_ = stage[ns % 3]
                ns += 1
                k.dma("pool", None, None, reads=[ys[e_], idx], writes=[g_],
                      fn=lambda eng: eng.indirect_dma_start(
                          out=g_[:, :], out_offset=None, in_=ys[e_][0:NSLOT, :],
                          in_offset=bass.IndirectOffsetOnAxis(ap=idx[:, i, e_:e_ + 1], axis=0),
                          bounds_check=breg, oob_is_err=False))
                if e_ == 0:
                    k.op("dve", lambda e: e.tensor_scalar(out=ac[:], in0=g_[:], scalar1=gate[:, i, 0:1], scalar2=None, op0=ALU.mult),
                         reads=[g_, gate], writes=[ac])
                else:
                    k.op("dve", lambda e: e.scalar_tensor_tensor(out=ac[:], in0=g_[:], scalar=gate[:, i, e_:e_ + 1], in1=ac[:],
                                                                 op0=ALU.mult, op1=ALU.add), reads=[g_, gate, ac], writes=[ac])
            k.dma("sp", po[i * 128:(i + 1) * 128, :], ac[:], reads=[ac], acc=[po])
    k.stage_end()


def st_p5(k, g, L, ctx_out, final):
    k.stage_begin()
    modT = g.modT
    ident = k.sb([128, 128])
    k.dma("sp", ident[:], g.idn[:], reads=[g.idn], writes=[ident])
    if final:
        fgs = k.sb([128, 8])
        k.dma("sp", fgs[:], g.fg[:], reads=[g.fg], writes=[fgs])
        ones = k.sb([128, 128])
        k.op("dve", lambda e: e.memset(ones[:], 1.0), writes=[ones])
        epsb = k.sb([128, 1])
        k.op("dve", lambda e: e.memset(epsb[:], EPS), writes=[epsb])
        sq = k.sb([128, 8, 512]); ssp = k.ps([128, 512]); rs = k.sb([128, 512])
    chunks = [(i * 512, 512, 0) for i in range(TL // 512)]
    if ctx_out:
        chunks.append((TL, NCTX, 1))
    xb = [k.sb([128, 8, 512]) for _ in range(2)]
    mo = k.sb([128, 8, 512])
    mtile = [k.sb([128, D]) for _ in range(2)]
    tps = [k.ps([128, 512]) for _ in range(2)]
    x1v = g.X1T.a.rearrange("(f p) t -> p f t", p=128)
    cnt = {"t": 0, "m": 0}
    for ci, (t0, n, col) in enumerate(chunks):
        x_ = xb[ci % 2]
        k.dma("sp", x_[:, :, :n], x1v[:, :, t0:t0 + n], reads=[g.X1T], writes=[x_])
        for sub in range(n // 128):
            mt = mtile[cnt["m"] % 2]
            cnt["m"] += 1
            if col == 0:
                k.dma("sp", mt[:], g.MOE[t0 + sub * 128:t0 + (sub + 1) * 128, :], reads=[g.MOE], writes=[mt])
            else:
                k.dma("sp", mt[:], g.MOEC[sub * 128:(sub + 1) * 128, :], reads=[g.MOEC], writes=[mt])
            for hf in range(2):
                tp = tps[cnt["t"] % 2]
                cnt["t"] += 1
                for j in range(4):
                    c = hf * 4 + j
                    k.op("pe", lambda e: e.transpose(tp[:, j * 128:(j + 1) * 128], mt[:, c * 128:(c + 1) * 128], ident[:]),
                         reads=[mt, ident], writes=[tp])
                k.evac(mo, mo[:, hf * 4:hf * 4 + 4, sub * 128:(sub + 1) * 128], tp, tp[:].rearrange("p (a b) -> p a b", a=4))
        for f in range(8):
            k.op("dve", lambda e: e.scalar_tensor_tensor(out=x_[:, f, :n], in0=mo[:, f, :n], scalar=modT[:, 40 + f, col:col + 1],
                                                         in1=x_[:, f, :n], op0=ALU.mult, op1=ALU.add),
                 reads=[mo, modT, x_], writes=[x_])
        if final:
            k.op("act", lambda e: e.activation(out=sq[:, :, :n], in_=x_[:, :, :n], func=AF.Square), reads=[x_], writes=[sq])
            for f in range(8):
                k.op("pe", lambda e: e.matmul(ssp[:, :n], ones[:], sq[:, f, :n], start=(f == 0), stop=(f == 7)),
                     reads=[ones, sq], writes=[ssp])
            k.op("act", lambda e: e.activation(out=rs[:, :n], in_=ssp[:, :n], func=AF.Sqrt, scale=1.0 / D, bias=epsb[:]),
                 reads=[ssp, epsb], writes=[rs])
            k.op("dve", lambda e: e.reciprocal(out=rs[:, :n], in_=rs[:, :n]), reads=[rs], writes=[rs])
            for f in range(8):
                k.op("dve", lambda e: e.scalar_tensor_tensor(out=x_[:, f, :n], in0=x_[:, f, :n], scalar=fgs[:, f:f + 1],
                                                             in1=rs[:, :n], op0=ALU.mult, op1=ALU.mult),
                     reads=[x_, fgs, rs], writes=[x_])
            ov = g.out.a.rearrange("(f p) t -> p f t", p=128)
            k.dma("pool", ov[:, :, t0:t0 + n], x_[:, :, :n], reads=[x_], writes=[g.out], is_output=True)
        else:
            if col == 0:
                ov = g.xown_next.a.rearrange("(f p) t -> p f t", p=128)
                k.dma("pool", ov[:, :, t0:t0 + n], x_[:, :, :n], reads=[x_], acc=[g.xown_next])
            else:
                ov = g.xc_next.a.rearrange("(f p) t -> p f t", p=128)
                k.dma("pool", ov[:, :, :], x_[:, :, :n], reads=[x_], acc=[g.xc_next])
    k.stage_end()


def build_fused(nlayer, stop=None):
    k = KB()
    g = Ctx()
    step = [0]

    def chk(exports):
        step[0] += 1
        if stop is not None and step[0] == stop:
            for nm, t in exports:
                o = k.outp('dbg_' + nm, list(t.a.shape))
                k.dma('sp', o[:], t[:], reads=[t], writes=[o], is_output=True)
            return True
        return False
    g.xall = k.inp("xall", [16, 2, 64, TL])
    g.xown = k.inp("xown", [D, TL]); g.xc = k.inp("xc", [D, NCTX])
    g.cv = k.inp("cv", [128, 8, 2])
    g.cosk = k.inp("cosk", [128, NKALL]); g.sink = k.inp("sink", [128, NKALL])
    g.cosq = k.inp("cosq", [128, TL]); g.sinq = k.inp("sinq", [128, TL])
    g.msk = k.inp("msk", [128, 6, 128]); g.idn = k.inp("idn", [128, 128]); g.lst = k.inp("lst", [128, 128])
    g.ccb = k.inp("ccb", [128, 256]); g.cs128 = k.inp("cs128", [128, 512]); g.tw = k.inp("tw", [64, 256])
    g.cs64 = k.inp("cs64", [64, 128]); g.dftc = k.inp("dftc", [128, 2, 512])
    g.hsel = k.inp("hsel", [128, 1]); g.fg = k.inp("fg", [128, 8])
    g.out = k.outp("outT", [D, TL])
    Ls = []
    for l in range(nlayer):
        L = Ctx()
        sfx = "_%d" % l
        L.adaw = k.inp("adaw" + sfx, [12, 128, 8, 512]); L.adab = k.inp("adab" + sfx, [128, 48]); L.g1 = k.inp("g1" + sfx, [128, 8])
        L.wAfm = k.inp("wAfm" + sfx, [NFM, 128, 8, 128]); L.wAtm = k.inp("wAtm" + sfx, [128, 8, TMW])
        L.wQ = k.inp("wQ" + sfx, [6, 128, 8, 128])
        L.dlam = k.inp("dlam" + sfx, [128]); L.dng = k.inp("dng" + sfx, [64])
        L.cw = k.inp("cw" + sfx, [2, 96, 8]); L.gb = k.inp("gb" + sfx, [8]); L.mng = k.inp("mng" + sfx, [192])
        L.fwblk = k.inp("fwblk" + sfx, [128, 128])
        L.wo = k.inp("wo" + sfx, [8, 128, 8, 128]); L.g2 = k.inp("g2" + sfx, [128, 8]); L.wr = k.inp("wr" + sfx, [128, 8, 16])
        Ls.append(L)
    g.FT_ = k.dram("FT_", [128, NKALL]); g.KT_ = k.dram("KT_", [384, NKALL]); g.KST_ = k.dram("KST_", [384, NKALL])
    g.MQP = k.dram("MQP", [2, 96, XPW]); g.MKP = k.dram("MKP", [2, 96, XPW])
    g.TM = k.dram("TM", [NKALL, TMW])
    g.QT_ = k.dram("QT_", [384, TL]); g.QST_ = k.dram("QST_", [384, TL]); g.QCT = k.dram("QCT", [384, NCTX])
    g.DA = k.dram("DA", [TL, 384]); g.DAC = k.dram("DAC", [NCTX, 384])
    g.SEND = k.dram("SEND", [S, 320]); g.MG = k.dram("MGc", [8, 2, 1024, 320])
    g.CMINE = k.dram("CMINE", [NCTX, 320]); CG2 = k.dram("CG2", [2 * NCTX, 320])
    g.CG = T(CG2.a.rearrange("(r t) c -> r t c", r=2)); g.CG2 = CG2
    g.X1T = k.dram("X1T", [D, TL + NCTX])
    g.H2SEND = k.dram("H2SEND", [TL, D]); g.H2ALL = k.dram("H2ALLc", [16, 2, 256, D]); g.H2C = k.dram("H2C", [NCTX, D])
    g.PRSEND = k.dram("PRSEND", [TL, NEXP]); g.PRALL = k.dram("PRALL", [S, NEXP]); g.PRC = k.dram("PRC", [NCTX, NEXP])
    g.xs = [k.dram("xs%d" % e, [1152, D]) for e in range(8)]
    g.ys = [k.dram("ys%d" % e, [1152, D]) for e in range(8)]
    g.PART = k.dram("PART", [S, D]); g.PARTC = k.dram("PARTC", [NCTX, D])
    g.MOE = k.dram("MOE", [TL, D]); g.MOEC = k.dram("MOEC", [NCTX, D])
    xown1 = k.dram("xown1", [D, TL]); xall1 = k.dram("xall1", [16, 2, 64, TL]); xc1 = k.dram("xc1", [D, NCTX])
    g.modT = k.sbp([128, 48, 2])
    g.breg = {}
    for ns in (1152, 1024):
        r = k.nc.gpsimd.alloc_register("bchk%d" % ns)
        k.nc.gpsimd.reg_mov(r, ns - 1)
        g.breg[ns] = r
    for l in range(nlayer):
        L = Ls[l]
        ctx_out = l < nlayer - 1
        final = not ctx_out
        lam_init = 0.8 - 0.6 * math.exp(-0.3 * l)
        st_proj(k, g, L, ctx_out)
        if chk([("TM", g.TM), ("KT", g.KT_), ("KST", g.KST_), ("QT", g.QT_), ("QST", g.QST_), ("QCT", g.QCT), ("MQP", g.MQP),
                ("MKP", g.MKP), ("FT", g.FT_)]):
            return k
        st_attn(k, g, L, lam_init, ctx_out)
        if chk([("DA", g.DA), ("DAC", g.DAC)]):
            return k
        st_mlstm(k, g, L)
        if chk([("SEND", g.SEND), ("CMINE", g.CMINE)]):
            return k
        st_fourier(k, g, L, ctx_out)
        if chk([("SEND", g.SEND), ("CMINE", g.CMINE)]):
            return k
        for i in range(8):
            k.coll("AllGather", ALU.bypass, g.SEND, g.MG, RG, in_ap=g.SEND.a[i * 1024:(i + 1) * 1024, :],
                   out_ap=g.MG.a[i].rearrange("r t c -> (r t) c"))
        if ctx_out:
            k.coll("AllGather", ALU.bypass, g.CMINE, g.CG2, RG)
            g.CG.w = g.CG2.w
        if chk([("MG", g.MG), ("CG2", g.CG2), ("DA", g.DA), ("DAC", g.DAC)]):
            return k
        st_p3(k, g, L, ctx_out)
        if chk([("X1T", g.X1T), ("H2SEND", g.H2SEND), ("PRSEND", g.PRSEND), ("H2C", g.H2C), ("PRC", g.PRC)]):
            return k
        for i in range(16):
            k.coll("AllGather", ALU.bypass, g.H2SEND, g.H2ALL, RG, in_ap=g.H2SEND.a[i * 256:(i + 1) * 256, :],
                   out_ap=g.H2ALL.a[i].rearrange("r t c -> (r t) c"))
        k.coll("AllGather", ALU.bypass, g.PRSEND, g.PRALL, RG)
        if chk([("H2ALL", g.H2ALL), ("PRALL", g.PRALL)]):
            return k
        sfx = "_%d" % l
        L.w1c = k.inp("w1c" + sfx, [8, 16, 128, 8, 128]); L.w3c = k.inp("w3c" + sfx, [8, 16, 128, 8, 128])
        L.w2c = k.inp("w2c" + sfx, [8, 4, 128, 16, 256])
        st_moe(k, g, L, ctx_out)
        if chk([("PART", g.PART), ("PARTC", g.PARTC)]):
            return k
        k.coll("ReduceScatter", ALU.add, g.PART, g.MOE, RG)
        if ctx_out:
            k.coll("AllReduce", ALU.add, g.PARTC, g.MOEC, RG)
        if chk([("MOE", g.MOE), ("MOEC", g.MOEC)]):
            return k
        g.xown_next, g.xc_next = xown1, xc1
        st_p5(k, g, L, ctx_out, final)
        if not final:
            for i in range(16):
                k.coll("AllGather", ALU.bypass, xown1, xall1, RG, in_ap=xown1.a[i * 64:(i + 1) * 64, :],
                       out_ap=xall1.a[i].rearrange("r w t -> (r w) t"))
            g.xall = xall1
            g.xown, g.xc = xown1, xc1
            if chk([("xown1", xown1), ("xall1", xall1), ("xc1", xc1)]):
                return k
    return k


OFF_F, OFF_DQ, OFF_MO, OFF_MQ, OFF_MK, OFF_DK, OFF_DV, OFF_MV, OFF_G = 0, 256, 640, 1024, 1408, 1792, 2176, 2560, 2944


def _swap_perm():
    p = np.zeros(32, np.int64)
    for a in range(2):
        for pp in range(2):
            for j in range(8):
                p[a * 16 + pp * 8 + j] = a * 16 + (1 - pp) * 8 + j
    return np.concatenate([gi * 32 + p for gi in range(12)])


def chunk_groups(wm, groups):
    out = np.zeros((len(groups), 128, 8, 128), np.float32)
    for i, cols in enumerate(groups):
        blk = wm[:, cols]
        out[i, :, :, :len(cols)] = blk.reshape(8, 128, len(cols)).transpose(1, 0, 2)
    return out


def chunk_w(wm, nj):
    K_, n = wm.shape
    out = np.zeros((K_, nj * 128), np.float32)
    out[:, :n] = wm
    return np.ascontiguousarray(out.reshape(K_ // 128, 128, nj, 128).transpose(2, 1, 0, 3))


def vecT(v, nchunk):
    return np.ascontiguousarray(v.reshape(nchunk, 128).T)


def rope_tables(nrows_lat):
    t_row = np.repeat(np.arange(nrows_lat), 64).astype(np.float32)
    t_col = np.tile(np.arange(64), nrows_lat).astype(np.float32)
    inv = (np.float32(10000.0) ** (-np.arange(8, dtype=np.float32) / np.float32(8))).astype(np.float32)
    ar = t_row[:, None] * inv
    ac = t_col[:, None] * inv
    ang = np.concatenate([ar, ar, ac, ac], -1)
    cos = np.cos(ang).astype(np.float32)
    sin = np.sin(ang).astype(np.float32)
    sgn = np.tile(np.concatenate([-np.ones(8), np.ones(8)]), 2).astype(np.float32)
    ssin = sin * sgn
    cosk = np.concatenate([np.ones((NCTX, 32), np.float32), cos], 0).T
    sink = np.concatenate([np.zeros((NCTX, 32), np.float32), ssin], 0).T
    return (np.ascontiguousarray(np.tile(cosk, (4, 1))), np.ascontiguousarray(np.tile(sink, (4, 1))))


def mlstm_masks():
    j = np.arange(128)[:, None]
    s = np.arange(128)[None, :]
    UF = (j > s); TRF = (j <= s)
    MF = np.where(s < j, -30000.0, 0.0)
    UB = (j < s); TRB = (j >= s)
    MB = np.where(j < s, -30000.0, 0.0)
    return np.ascontiguousarray(np.stack([UF, TRF, MF, UB, TRB, MB], 1).astype(np.float32))


def fourier_tables():
    def cs(n, m, N):
        ang = 2.0 * np.pi * np.outer(np.arange(n), np.arange(m)) / N
        return np.cos(ang), np.sin(ang)
    c64, s64 = cs(64, 64, 64)
    z = np.zeros((64, 64))
    ccb = np.concatenate([np.block([[c64, z], [z, c64]]), np.block([[s64, z], [z, s64]])], 1)
    c128, s128 = cs(128, 128, 128)
    cs128 = np.concatenate([c128, s128, -s128, c128], 1)
    tc, ts = cs(64, 128, S)
    tw = np.concatenate([tc, ts], 1) / np.sqrt(64.0 * S)
    cs64 = np.concatenate([c64, -s64], 1)
    cn, sn = cs(NCTX, NCTX, NCTX)
    dft = np.concatenate([cn, -sn], 1) / np.sqrt(64.0 * NCTX)
    dftc = dft.reshape(2, 128, 512).transpose(1, 0, 2)
    f = lambda a: np.ascontiguousarray(a.astype(np.float32))
    return {"ccb": f(ccb), "cs128": f(cs128), "tw": f(tw), "cs64": f(cs64), "dftc": f(dftc)}


def host_inputs(inp):
    nlayer = inp["w_in"].shape[0]
    cosk, sink = rope_tables(S // 64)
    tabs = fourier_tables()
    sw = _swap_perm()
    ar = np.arange
    shared = {"cosk": cosk, "sink": sink, "msk": mlstm_masks(), "idn": np.eye(128, dtype=np.float32),
              "lst": np.ascontiguousarray(np.triu(np.ones((128, 128), np.float32), 1)),
              "fg": vecT(inp["final_g"], 8)}
    shared.update(tabs)
    for l in range(nlayer):
        sfx = "_%d" % l
        shared["adaw" + sfx] = np.ascontiguousarray(inp["ada_w"][l].reshape(8, 128, 12, 512).transpose(2, 1, 0, 3))
        shared["adab" + sfx] = vecT(inp["ada_b"][l], 48)
        shared["g1" + sfx] = vecT(inp["norm1_g"][l], 8)
        shared["wQ" + sfx] = chunk_groups(inp["w_in"][l], [OFF_DQ + ar(j * 128, (j + 1) * 128) for j in range(3)]
                                          + [OFF_DQ + sw[j * 128:(j + 1) * 128] for j in range(3)])
        shared["dlam" + sfx] = np.ascontiguousarray(inp["d_lam"][l].reshape(128))
        shared["dng" + sfx] = inp["d_norm_g"][l]
        shared["wo" + sfx] = chunk_w(inp["w_out"][l], 8)
        shared["g2" + sfx] = vecT(inp["norm2_g"][l], 8)
        shared["wr" + sfx] = np.ascontiguousarray(inp["router_w"][l].reshape(8, 128, 16).transpose(1, 0, 2))
    maps = []
    for c in range(NCORE):
        b, h = c // 2, c % 2
        m = dict(shared)
        m["xall"] = np.ascontiguousarray(inp["x"][b].reshape(2, TL, 16, 64).transpose(2, 0, 3, 1))
        m["xown"] = np.ascontiguousarray(inp["x"][b, h * TL:(h + 1) * TL].T)
        m["xc"] = np.ascontiguousarray(inp["ctx"][b].T)
        cv = np.stack([inp["c"][b], inp["c_ctx"]], axis=-1)
        m["cv"] = np.ascontiguousarray(cv.reshape(8, 128, 2).transpose(1, 0, 2))
        q0 = NCTX + h * TL
        m["cosq"] = np.ascontiguousarray(cosk[:, q0:q0 + TL]); m["sinq"] = np.ascontiguousarray(sink[:, q0:q0 + TL])
        m["hsel"] = np.full((128, 1), float(h), np.float32)
        heads = [2 * h, 2 * h + 1]
        es = slice(8 * h, 8 * h + 8)
        for l in range(nlayer):
            sfx = "_%d" % l
            w = inp["w_in"][l]
            groups = [OFF_F + h * 128 + ar(128)]
            groups += [OFF_DK + ar(j * 128, (j + 1) * 128) for j in range(3)]
            groups += [OFF_DK + sw[j * 128:(j + 1) * 128] for j in range(3)]
            groups += [OFF_MQ + hh * 96 + ar(96) for hh in heads]
            groups += [OFF_MK + hh * 96 + ar(96) for hh in heads]
            m["wAfm" + sfx] = chunk_groups(w, groups)
            gcols = [[hh, 4 + hh, 8 + hh, 12 + hh] for hh in heads]
            tmc = np.concatenate([OFF_DV + ar(384)] + [OFF_MV + hh * 96 + ar(96) for hh in heads]
                                 + [OFF_MO + hh * 96 + ar(96) for hh in heads] + [OFF_G + np.array(gc) for gc in gcols])
            m["wAtm" + sfx] = np.ascontiguousarray(w[:, tmc].reshape(8, 128, TMW).transpose(1, 0, 2))
            cwl, cbl = inp["m_conv_w"][l], inp["m_conv_b"][l]
            cw = np.zeros((2, 96, 8), np.float32)
            for i, hh in enumerate(heads):
                cw[i, :, 0:3] = cwl[:, hh * 96:(hh + 1) * 96].T
                cw[i, :, 3] = cbl[hh * 96:(hh + 1) * 96]
                cw[i, :, 4:7] = cwl[:, 384 + hh * 96:384 + (hh + 1) * 96].T
                cw[i, :, 7] = cbl[384 + hh * 96:384 + (hh + 1) * 96]
            m["cw" + sfx] = cw
            m["gb" + sfx] = np.ascontiguousarray(np.stack([inp["m_gate_b"][l][gc] for gc in gcols]).reshape(8))
            m["mng" + sfx] = np.ascontiguousarray(inp["m_norm_g"][l][heads[0] * 96:(heads[1] + 1) * 96])
            fw = inp["four_w"][l]
            fwblk = np.zeros((128, 128), np.float32)
            fwblk[0:64, 0:64] = fw[2 * h]
            fwblk[64:128, 64:128] = fw[2 * h + 1]
            m["fwblk" + sfx] = fwblk
            w1 = inp["exp_w1"][l][es]; w3 = inp["exp_w3"][l][es]; w2 = inp["exp_w2"][l][es]
            m["w1c" + sfx] = np.ascontiguousarray(w1.reshape(8, 8, 128, 16, 128).transpose(0, 3, 2, 1, 4))
            m["w3c" + sfx] = np.ascontiguousarray(w3.reshape(8, 8, 128, 16, 128).transpose(0, 3, 2, 1, 4))
            m["w2c" + sfx] = np.ascontiguousarray(w2.reshape(8, 16, 128, 4, 256).transpose(0, 3, 2, 1, 4))
        maps.append(m)
    return maps


def kernel(**inp):
    inp = {k_: np.asarray(v_) for k_, v_ in inp.items()}
    nlayer = inp["w_in"].shape[0]
    kb = build_fused(nlayer)
    res = run(kb, host_inputs(inp))
    out = np.zeros((B, S, D), np.float32)
    for c in range(NCORE):
        b, h = c // 2, c % 2
        out[b, h * TL:(h + 1) * TL] = res[c]["outT"].T
    return out
```
